# Optimizing a Trainium2 kernel written in Bass

```python
import math
import jax, jax.numpy as jnp
from jax import lax
import numpy as np

D_MODEL = 1024
BATCH = 8
SEQ = 4096
DEPTH = 2

MIX_WIDTH = 1024
D_FF = 2816
EPS = 1e-6
N_EVEN = (DEPTH + 1) // 2
N_ODD = DEPTH // 2

GLA_HEADS = 4
GLA_DK = 64
GLA_DV = 128
GLA_KEY_WIDTH = GLA_HEADS * GLA_DK
GLA_WIDTH = GLA_HEADS * GLA_DV
GLA_GATE_RANK = 16
GLA_GATE_NORMALIZER = 16.0
GLA_CHUNK = 64

POOL_WINDOWS = (2, 4, 8, 16)
POOL_GROUPS = 4
POOL_GROUP_DIM = 128
POOL_WIDTH = POOL_GROUPS * POOL_GROUP_DIM

EVEN_PROJ = 2 * GLA_KEY_WIDTH + 2 * GLA_WIDTH + GLA_GATE_RANK + POOL_WIDTH
EVEN_SPLITS = (GLA_KEY_WIDTH, 2 * GLA_KEY_WIDTH, 2 * GLA_KEY_WIDTH + GLA_WIDTH,
               2 * GLA_KEY_WIDTH + 2 * GLA_WIDTH,
               2 * GLA_KEY_WIDTH + 2 * GLA_WIDTH + GLA_GATE_RANK)

SSD_HEADS = 8
SSD_HEADDIM = 64
SSD_WIDTH = SSD_HEADS * SSD_HEADDIM
SSD_GROUPS = 2
SSD_STATE = 128
SSD_CONV = 4
SSD_CHUNK = 128
SSD_CONV_DIM = SSD_WIDTH + 2 * SSD_GROUPS * SSD_STATE

DIFF_HEADS = 4
DIFF_HEAD_DIM = 64
DIFF_V_DIM = 128
DIFF_WIDTH = DIFF_HEADS * DIFF_V_DIM
Q_BLOCK = 128
REL_BUCKETS = 32
REL_MAX_DIST = 128
NEG_INF = -1e30

ODD_PROJ = SSD_WIDTH + SSD_CONV_DIM + SSD_HEADS + 3 * DIFF_WIDTH
ODD_SPLITS = (SSD_WIDTH, SSD_WIDTH + SSD_CONV_DIM, SSD_WIDTH + SSD_CONV_DIM + SSD_HEADS,
              SSD_WIDTH + SSD_CONV_DIM + SSD_HEADS + DIFF_WIDTH,
              SSD_WIDTH + SSD_CONV_DIM + SSD_HEADS + 2 * DIFF_WIDTH)

kernel_name = "hybrid_gla_pool_ssd_diffattn_macaron"

F32 = jnp.float32


def rmsnorm(x, w, eps=EPS):
    xf = x.astype(F32)
    y = xf * lax.rsqrt(jnp.mean(xf * xf, axis=-1, keepdims=True) + eps)
    return (y * w.astype(F32)).astype(x.dtype)


def swiglu(x, w_gate, w_up, w_down):
    return (jax.nn.silu(x @ w_gate) * (x @ w_up)) @ w_down


def gla_mixer(q, k, v, g_out, gate_lr, w_gk_up, b_gk, w_norm):
    Bsz, T, _ = q.shape
    N = T // GLA_CHUNK

    def heads(t, d):
        return t.astype(F32).reshape(Bsz, N, GLA_CHUNK, GLA_HEADS, d).transpose(0, 3, 1, 2, 4)

    log_a = jax.nn.log_sigmoid((gate_lr @ w_gk_up + b_gk).astype(F32)) / GLA_GATE_NORMALIZER
    qh = heads(q, GLA_DK) * (GLA_DK ** -0.5)
    kh = heads(k, GLA_DK)
    vh = heads(v, GLA_DV)
    G = jnp.cumsum(heads(log_a, GLA_DK), axis=3)
    G_last = G[:, :, :, -1:, :]
    q_dec = qh * jnp.exp(G)
    k_dec = kh * jnp.exp(-G)
    causal = jnp.tril(jnp.ones((GLA_CHUNK, GLA_CHUNK), bool))
    A = jnp.where(causal, jnp.einsum('bhnik,bhnjk->bhnij', q_dec, k_dec), 0.0)
    o_intra = jnp.einsum('bhnij,bhnjv->bhniv', A, vh)
    chunk_state = jnp.einsum('bhnck,bhncv->nbhkv', kh * jnp.exp(G_last - G), vh)
    chunk_decay = jnp.exp(G_last[:, :, :, 0, :]).transpose(2, 0, 1, 3)

    def step(S, inp):
        dec, st = inp
        return dec[..., None] * S + st, S

    S0 = jnp.zeros((Bsz, GLA_HEADS, GLA_DK, GLA_DV), F32)
    _, S_prev = lax.scan(step, S0, (chunk_decay, chunk_state))
    o_inter = jnp.einsum('bhnck,nbhkv->bhncv', q_dec, S_prev)
    o = (o_intra + o_inter).transpose(0, 2, 3, 1, 4).reshape(Bsz, T, GLA_HEADS, GLA_DV)
    o = rmsnorm(o, w_norm).reshape(Bsz, T, GLA_WIDTH)
    return (o * jax.nn.silu(g_out.astype(F32))).astype(q.dtype)


def pool_mixer(u, w_pool, pool_scale):
    Bsz, T, _ = u.shape
    uf = u.astype(F32).reshape(Bsz, T, POOL_GROUPS, POOL_GROUP_DIM)
    cs = jnp.concatenate([jnp.zeros_like(uf[:, :1]), jnp.cumsum(uf, axis=1)], axis=1)
    t = jnp.arange(T)[:, None]
    win = jnp.array(POOL_WINDOWS, jnp.int32)[None, :]
    lo = jnp.maximum(t + 1 - win, 0)
    grp = jnp.arange(POOL_GROUPS)[None, :]
    window_sum = cs[:, 1:] - cs[:, lo, grp]
    count = (t + 1 - lo).astype(F32)[..., None]
    pooled = window_sum / count - uf
    y = jnp.einsum('btgc,gcd->btgd', pooled, w_pool.astype(F32)).reshape(Bsz, T, POOL_WIDTH)
    return (y * pool_scale.astype(F32)).astype(u.dtype)


def segsum_exp(a):
    L = a.shape[-1]
    cs = jnp.cumsum(a, axis=-1)
    diff = cs[..., :, None] - cs[..., None, :]
    mask = jnp.tril(jnp.ones((L, L), bool))
    return jnp.where(mask, jnp.exp(jnp.where(mask, diff, 0.0)), 0.0)


def ssd_mixer(z, xbc, dt, conv_w, conv_b, dt_bias, a_log, d_skip, w_norm):
    Bsz, T, _ = z.shape
    N, L, G, Hg = T // SSD_CHUNK, SSD_CHUNK, SSD_GROUPS, SSD_HEADS // SSD_GROUPS
    xbc = lax.conv_general_dilated(xbc, conv_w[:, None, :], window_strides=(1,),
                                   padding=[(SSD_CONV - 1, 0)],
                                   dimension_numbers=('NWC', 'WIO', 'NWC'),
                                   feature_group_count=SSD_CONV_DIM)
    xbc = jax.nn.silu(xbc + conv_b).astype(F32)
    x, Bm, Cm = jnp.split(xbc, [SSD_WIDTH, SSD_WIDTH + G * SSD_STATE], axis=-1)
    x = x.reshape(Bsz, N, L, G, Hg, SSD_HEADDIM)
    Bm = Bm.reshape(Bsz, N, L, G, SSD_STATE)
    Cm = Cm.reshape(Bsz, N, L, G, SSD_STATE)
    dt = jax.nn.softplus(dt.astype(F32) + dt_bias.astype(F32))
    A = -jnp.exp(a_log.astype(F32))
    a = (dt * A).reshape(Bsz, N, L, G, Hg).transpose(0, 3, 4, 1, 2)
    xdt = x * dt.reshape(Bsz, N, L, G, Hg)[..., None]
    a_cs = jnp.cumsum(a, axis=-1)
    Lmat = segsum_exp(a)
    CB = jnp.einsum('bclgn,bcsgn->bgcls', Cm, Bm)
    y_diag = jnp.einsum('bgcls,bghcls,bcsghp->bclghp', CB, Lmat, xdt)
    decay_to_end = jnp.exp(a_cs[..., -1:] - a_cs)
    states = jnp.einsum('bcsgn,bghcs,bcsghp->cbghpn', Bm, decay_to_end, xdt)
    chunk_decay = jnp.exp(a_cs[..., -1]).transpose(3, 0, 1, 2)

    def step(S, inp):
        dec, st = inp
        return dec[..., None, None] * S + st, S

    S0 = jnp.zeros(states.shape[1:], F32)
    _, S_prev = lax.scan(step, S0, (chunk_decay, states))
    y_off = jnp.einsum('bclgn,cbghpn,bghcl->bclghp', Cm, S_prev, jnp.exp(a_cs))
    y = y_diag + y_off + x * d_skip.astype(F32).reshape(G, Hg)[:, :, None]
    y = y.reshape(Bsz, T, SSD_WIDTH) * jax.nn.silu(z.astype(F32))
    y = rmsnorm(y.reshape(Bsz, T, G, SSD_WIDTH // G), w_norm.reshape(G, SSD_WIDTH // G))
    return y.reshape(Bsz, T, SSD_WIDTH).astype(z.dtype)


def t5_bucket(q_pos, k_pos):
    n = jnp.maximum(q_pos[:, None] - k_pos[None, :], 0)
    max_exact = REL_BUCKETS // 2
    nf = jnp.maximum(n, 1).astype(F32)
    large = max_exact + (jnp.log(nf / max_exact) / math.log(REL_MAX_DIST / max_exact)
                         * (REL_BUCKETS - max_exact)).astype(jnp.int32)
    large = jnp.minimum(large, REL_BUCKETS - 1)
    return jnp.where(n < max_exact, n, large)


def diff_attention(q, k, v, rel_bias, q_norm, k_norm, lq1, lk1, lq2, lk2, subln, lambda_init):
    Bsz, T, _ = q.shape
    nb = T // Q_BLOCK
    qh = rmsnorm(q.reshape(Bsz, T, DIFF_HEADS, 2, DIFF_HEAD_DIM), q_norm).astype(F32) * (DIFF_HEAD_DIM ** -0.5)
    kh = rmsnorm(k.reshape(Bsz, T, DIFF_HEADS, 2, DIFF_HEAD_DIM), k_norm).astype(F32)
    vh = v.reshape(Bsz, T, DIFF_HEADS, DIFF_V_DIM).astype(F32)
    lam = (jnp.exp(jnp.sum(lq1.astype(F32) * lk1.astype(F32)))
           - jnp.exp(jnp.sum(lq2.astype(F32) * lk2.astype(F32))) + lambda_init)
    q_blocks = qh.reshape(Bsz, nb, Q_BLOCK, DIFF_HEADS, 2, DIFF_HEAD_DIM).transpose(1, 0, 2, 3, 4, 5)
    k_pos = jnp.arange(T)
    table = rel_bias.astype(F32)

    def one_block(inp):
        qb, blk = inp
        q_pos = blk * Q_BLOCK + jnp.arange(Q_BLOCK)
        bias = table[t5_bucket(q_pos, k_pos)].transpose(2, 0, 1)
        s = jnp.einsum('bqhmd,bkhmd->bhmqk', qb, kh) + bias[None, :, None]
        s = jnp.where(k_pos[None, :] <= q_pos[:, None], s, NEG_INF)
        p = jax.nn.softmax(s, axis=-1)
        w = p[:, :, 0] - lam * p[:, :, 1]
        return jnp.einsum('bhqk,bkhv->bqhv', w, vh)

    o = lax.map(one_block, (q_blocks, jnp.arange(nb)))
    o = o.transpose(1, 0, 2, 3, 4).reshape(Bsz, T, DIFF_HEADS, DIFF_V_DIM)
    o = rmsnorm(o, subln) * (1.0 - lambda_init)
    return o.reshape(Bsz, T, DIFF_WIDTH).astype(q.dtype)


def setup_inputs(seed: int = 0) -> dict:
    key = jax.random.key(seed)
    ks = iter(jax.random.split(key, 40))
    nrm = lambda shape, scale: jax.random.normal(next(ks), shape, F32) * scale
    gain = lambda shape: 1.0 + 0.1 * jax.random.normal(next(ks), shape, F32)
    x = jax.random.normal(next(ks), (BATCH, SEQ, D_MODEL), F32)
    dt0 = jnp.exp(jax.random.uniform(next(ks), (N_ODD, SSD_HEADS), F32)
                  * (math.log(0.1) - math.log(1e-3)) + math.log(1e-3))
    return {
        "x": x,
        "ffn_norm": gain((DEPTH, 2, D_MODEL)),
        "ffn_w_gate": nrm((DEPTH, 2, D_MODEL, D_FF), D_MODEL ** -0.5),
        "ffn_w_up": nrm((DEPTH, 2, D_MODEL, D_FF), D_MODEL ** -0.5),
        "ffn_w_down": nrm((DEPTH, 2, D_FF, D_MODEL), D_FF ** -0.5),
        "mix_norm": gain((DEPTH, D_MODEL)),
        "ev_w_in": nrm((N_EVEN, D_MODEL, EVEN_PROJ), D_MODEL ** -0.5),
        "ev_w_gk_up": nrm((N_EVEN, GLA_GATE_RANK, GLA_KEY_WIDTH), GLA_GATE_RANK ** -0.5),
        "ev_b_gk": nrm((N_EVEN, GLA_KEY_WIDTH), 0.1),
        "ev_gla_norm": gain((N_EVEN, GLA_DV)),
        "ev_w_pool": nrm((N_EVEN, POOL_GROUPS, POOL_GROUP_DIM, POOL_GROUP_DIM), POOL_GROUP_DIM ** -0.5),
        "ev_pool_scale": gain((N_EVEN, POOL_WIDTH)),
        "ev_w_out": nrm((N_EVEN, MIX_WIDTH, D_MODEL), MIX_WIDTH ** -0.5),
        "od_w_in": nrm((N_ODD, D_MODEL, ODD_PROJ), D_MODEL ** -0.5),
        "od_conv_w": nrm((N_ODD, SSD_CONV, SSD_CONV_DIM), SSD_CONV ** -0.5),
        "od_conv_b": nrm((N_ODD, SSD_CONV_DIM), 0.02),
        "od_dt_bias": dt0 + jnp.log(-jnp.expm1(-dt0)),
        "od_a_log": jnp.log(jax.random.uniform(next(ks), (N_ODD, SSD_HEADS), F32, 1.0, 16.0)),
        "od_d_skip": gain((N_ODD, SSD_HEADS)),
        "od_ssd_norm": gain((N_ODD, SSD_WIDTH)),
        "od_q_norm": gain((N_ODD, DIFF_HEAD_DIM)),
        "od_k_norm": gain((N_ODD, DIFF_HEAD_DIM)),
        "od_lambda_q1": nrm((N_ODD, DIFF_HEAD_DIM), 0.1),
        "od_lambda_k1": nrm((N_ODD, DIFF_HEAD_DIM), 0.1),
        "od_lambda_q2": nrm((N_ODD, DIFF_HEAD_DIM), 0.1),
        "od_lambda_k2": nrm((N_ODD, DIFF_HEAD_DIM), 0.1),
        "od_subln": gain((N_ODD, DIFF_V_DIM)),
        "od_w_out": nrm((N_ODD, MIX_WIDTH, D_MODEL), MIX_WIDTH ** -0.5),
        "rel_bias": nrm((REL_BUCKETS, DIFF_HEADS), 0.5),
    }


def reference(x, ffn_norm, ffn_w_gate, ffn_w_up, ffn_w_down, mix_norm,
              ev_w_in, ev_w_gk_up, ev_b_gk, ev_gla_norm, ev_w_pool, ev_pool_scale, ev_w_out,
              od_w_in, od_conv_w, od_conv_b, od_dt_bias, od_a_log, od_d_skip, od_ssd_norm,
              od_q_norm, od_k_norm, od_lambda_q1, od_lambda_k1, od_lambda_q2, od_lambda_k2,
              od_subln, od_w_out, rel_bias):
    for i in range(DEPTH):
        x = x + 0.5 * swiglu(rmsnorm(x, ffn_norm[i, 0]), ffn_w_gate[i, 0], ffn_w_up[i, 0], ffn_w_down[i, 0])
        h = rmsnorm(x, mix_norm[i])
        if i % 2 == 0:
            j = i // 2
            p = h @ ev_w_in[j]
            q, k, v, g, lr, u = jnp.split(p, list(EVEN_SPLITS), axis=-1)
            y_a = gla_mixer(q, k, v, g, lr, ev_w_gk_up[j], ev_b_gk[j], ev_gla_norm[j])
            y_b = pool_mixer(u, ev_w_pool[j], ev_pool_scale[j])
            x = x + jnp.concatenate([y_a, y_b], axis=-1) @ ev_w_out[j]
        else:
            j = i // 2
            lambda_init = 0.8 - 0.6 * math.exp(-0.3 * i)
            p = h @ od_w_in[j]
            z, xbc, dt, q, k, v = jnp.split(p, list(ODD_SPLITS), axis=-1)
            y_c = ssd_mixer(z, xbc, dt, od_conv_w[j], od_conv_b[j], od_dt_bias[j], od_a_log[j],
                            od_d_skip[j], od_ssd_norm[j])
            y_d = diff_attention(q, k, v, rel_bias, od_q_norm[j], od_k_norm[j], od_lambda_q1[j],
                                 od_lambda_k1[j], od_lambda_q2[j], od_lambda_k2[j], od_subln[j],
                                 lambda_init)
            x = x + jnp.concatenate([y_c, y_d], axis=-1) @ od_w_out[j]
        x = x + 0.5 * swiglu(rmsnorm(x, ffn_norm[i, 1]), ffn_w_gate[i, 1], ffn_w_up[i, 1], ffn_w_down[i, 1])
    return x
```

```python
import contextlib
import numpy as np
import concourse.bass as bass
import concourse.mybir as mybir
from concourse.bass_utils import run_bass_kernel_spmd

F32 = mybir.dt.float32
F32R = mybir.dt.float32r
ALU = mybir.AluOpType
AF = mybir.ActivationFunctionType
AX = mybir.AxisListType

T = 4096
D = 1024
DFF = 2816
NFC = DFF // 128
EPS = 1e-6
MM_FAST = False


def mm(ap):
    return ap.bitcast(F32R) if MM_FAST else ap


class Trk:
    __slots__ = ("w", "r", "sem", "name")

    def __init__(self, name=""):
        self.w = None
        self.r = []
        self.sem = None
        self.name = name


class Op:
    __slots__ = ("eng", "fn", "deps", "flag", "sem", "val", "dma", "n")

    def __init__(self, eng, fn, dma=False, n=1):
        self.eng = eng
        self.fn = fn
        self.deps = []
        self.flag = False
        self.sem = None
        self.val = 0
        self.dma = dma
        self.n = n


EPOCH = 6000
ENGS = ("pe", "act", "dve", "pool", "sp")


class FW:
    def __init__(self, nc, stack, n_dma_sems=48):
        self.nc = nc
        self.stack = stack
        self.eng_obj = {"pe": nc.tensor, "act": nc.scalar, "dve": nc.vector,
                        "pool": nc.gpsimd, "sp": nc.sync}
        self.eng_sems = {e: [] for e in ENGS}
        self.eng_cnt = {e: 0 for e in ENGS}
        self.free_dma = []
        self.dma_cnt = {}
        self.n_sem = 0
        self.ops = []
        self.phase_dma_sems = set()
        self.waited = {e: {} for e in ENGS}
        self.phase_stack = None
        self.n_total = 0

    def new_sem(self, name):
        self.n_sem += 1
        return self.stack.enter_context(self.nc.semaphore(f"{name}_{self.n_sem}"))

    def sb(self, name, shape, dtype=F32):
        self.n_sem += 1
        name = f"{name}_{self.n_sem}"
        t = self.phase_stack.enter_context(self.nc.sbuf_tensor(name, list(shape), dtype))
        return t

    def ps(self, name, shape, dtype=F32):
        self.n_sem += 1
        name = f"{name}_{self.n_sem}"
        t = self.phase_stack.enter_context(self.nc.psum_tensor(name, list(shape), dtype))
        return t

    def _dep(self, op, reads, writes):
        deps = op.deps
        for t in reads:
            if t.w is not None:
                deps.append(t.w)
        for t in writes:
            if t.w is not None:
                deps.append(t.w)
            deps.extend(t.r)
        for t in reads:
            if not op.dma:
                t.r = [x for x in t.r if x.dma or x.eng != op.eng]
            t.r.append(op)
        for t in writes:
            t.w = op
            t.r = []

    def op(self, eng, fn, reads=(), writes=(), pe_acc=False):
        o = Op(eng, fn)
        self._dep(o, reads, writes)
        if pe_acc:
            o.deps = [d for d in o.deps if d.eng != "pe" or d.dma]
        self.ops.append(o)
        return o

    def dma(self, q, fns, sbuf_trk, reads=(), writes=()):
        if not isinstance(fns, (list, tuple)):
            fns = [fns]
        o = Op(q, fns, dma=True, n=len(fns))
        self._dep(o, reads, writes)
        if sbuf_trk.sem is None:
            if not self.free_dma:
                s = self.new_sem("dq")
                self.dma_cnt[s] = 0
                self.free_dma.append(s)
            sbuf_trk.sem = self.free_dma.pop()
            self.phase_dma_sems.add(sbuf_trk.sem)
        o.sem = sbuf_trk.sem
        self.dma_cnt[o.sem] += 16 * len(fns)
        o.val = self.dma_cnt[o.sem]
        self.ops.append(o)
        return o

    def emit(self):
        nc = self.nc
        ops = self.ops
        for o in ops:
            for d in o.deps:
                if not d.dma:
                    d.flag = True
        last = {}
        for o in ops:
            if not o.dma:
                last[o.eng] = o
        for o in last.values():
            o.flag = True
        for o in ops:
            if o.dma or not o.flag:
                continue
            c = self.eng_cnt[o.eng]
            ep = c // EPOCH
            sems = self.eng_sems[o.eng]
            while len(sems) <= ep:
                sems.append(self.new_sem("e" + o.eng))
            o.sem = sems[ep]
            o.val = c % EPOCH + 1
            self.eng_cnt[o.eng] = c + 1
        targets = []
        for e, o in last.items():
            targets.append((o.sem, o.val))
        for s in self.phase_dma_sems:
            targets.append((s, self.dma_cnt[s]))
        by_eng = {e: [] for e in ENGS}
        for o in ops:
            by_eng[o.eng].append(o)
        waited = self.waited

        def run(ename):
            def body(eng):
                wd = waited[ename]
                for o in by_eng[ename]:
                    for d in o.deps:
                        if d.sem is None:
                            continue
                        if wd.get(d.sem, 0) >= d.val:
                            continue
                        if d.eng == ename and not d.dma and ename == "pe":
                            pass
                        eng.wait_ge(d.sem, d.val)
                        wd[d.sem] = d.val
                    if o.dma:
                        for f in o.fn:
                            f(eng).then_inc(o.sem, 16)
                    else:
                        ins = o.fn(eng)
                        if o.flag:
                            ins.then_inc(o.sem, 1)
                for (s, v) in targets:
                    if wd.get(s, 0) < v:
                        eng.wait_ge(s, v)
                        wd[s] = v
            return body

        with nc.Block() as block:
            block.sync(run("sp"))
            block.scalar(run("act"))
            block.vector(run("dve"))
            block.gpsimd(run("pool"))
            block.tensor(run("pe"))
        self.n_total += len(ops)
        for s in self.phase_dma_sems:
            if self.dma_cnt[s] < 24000:
                self.free_dma.append(s)
        self.phase_dma_sems = set()
        self.ops = []

    @contextlib.contextmanager
    def phase(self):
        with contextlib.ExitStack() as st:
            self.phase_stack = st
            yield
            self.emit()
        self.phase_stack = None


class Buf:
    def __init__(self, fw, name, shape, dtype=F32, psum=False):
        self.t = fw.ps(name, shape, dtype) if psum else fw.sb(name, shape, dtype)
        self.k = Trk(name)

    def __getitem__(self, idx):
        return self.t[idx]


def rstd_from_ss(fw, out_buf, ss_ps, n, width, epsb):
    fw.op("act", lambda e: e.activation(out_buf[:, :width], ss_ps[:, :width], AF.Sqrt,
                                        bias=epsb[:, 0:1], scale=1.0 / n),
          reads=[ss_ps.k, epsb.k], writes=[out_buf.k])
    fw.op("dve", lambda e: e.reciprocal(out_buf[:, :width], out_buf[:, :width]),
          reads=[out_buf.k], writes=[out_buf.k])


def ffn_phase(fw, xT_in, xT_out, wg, wu, wd, gnorm, NT=512):
    with fw.phase():
        ones = Buf(fw, "ones", [128, 128])
        g_sb = Buf(fw, "g_sb", [128, 8])
        xt = [Buf(fw, f"xt{i}", [128, 8, NT]) for i in range(2)]
        xn = Buf(fw, "xn", [128, 8, NT])
        sq = [Buf(fw, f"sq{i}", [128, NT]) for i in range(2)]
        rstd = Buf(fw, "rstd", [128, NT])
        hid = Buf(fw, "hid", [128, NFC, NT])
        sg = [Buf(fw, f"sg{i}", [128, NT]) for i in range(2)]
        wgb = [Buf(fw, f"wgb{i}", [128, 8, 128]) for i in range(3)]
        wub = [Buf(fw, f"wub{i}", [128, 8, 128]) for i in range(3)]
        wdb = [Buf(fw, f"wdb{i}", [128, NFC, 128]) for i in range(2)]
        xo = [Buf(fw, f"xo{i}", [128, NT]) for i in range(2)]
        ss_ps = Buf(fw, "ss_ps", [128, NT], psum=True)
        gp = [Buf(fw, f"gp{i}", [128, NT], psum=True) for i in range(2)]
        up = [Buf(fw, f"up{i}", [128, NT], psum=True) for i in range(2)]
        op_ = [Buf(fw, f"op{i}", [128, NT], psum=True) for i in range(2)]

        fw.op("pool", lambda e: e.memset(ones[:, :], 1.0), writes=[ones.k])
        epsb = Buf(fw, "epsb", [128, 1])
        fw.op("pool", lambda e: e.memset(epsb[:, :], EPS), writes=[epsb.k])
        fw.dma("sp", lambda e: e.dma_start(out=g_sb[:, :], in_=gnorm), g_sb.k, writes=[g_sb.k])
        xin = xT_in.rearrange("(c p) t -> p c t", p=128)
        xout = xT_out.rearrange("(c p) t -> p c t", p=128)
        ntiles = T // NT
        wi = 0
        di = 0
        for it in range(ntiles):
            X = xt[it % 2]
            ts = slice(it * NT, (it + 1) * NT)
            fw.dma("sp", lambda e, X=X, ts=ts: e.dma_start(out=X[:, :, :], in_=xin[:, :, ts]),
                   X.k, writes=[X.k])
            for c in range(8):
                S = sq[c % 2]
                fw.op("act", lambda e, S=S, X=X, c=c: e.activation(S[:, :], X[:, c, :], AF.Square),
                      reads=[X.k], writes=[S.k])
                fw.op("pe", lambda e, S=S, c=c: e.matmul(ss_ps[:, :], ones[:, :], S[:, :],
                                                          start=(c == 0), stop=(c == 7)),
                      reads=[ones.k, S.k], writes=[ss_ps.k], pe_acc=(c > 0))
            rstd_from_ss(fw, rstd, ss_ps, D, NT, epsb)
            for c in range(8):
                fw.op("dve", lambda e, X=X, c=c: e.scalar_tensor_tensor(
                    xn[:, c, :], X[:, c, :], g_sb[:, c:c + 1], rstd[:, :], ALU.mult, ALU.mult),
                    reads=[X.k, g_sb.k, rstd.k], writes=[xn.k])
            for f in range(NFC):
                WG = wgb[wi % 3]
                WU = wub[wi % 3]
                wi += 1
                fw.dma("sp", lambda e, WG=WG, f=f: e.dma_start(out=WG[:, :, :], in_=wg[f]),
                       WG.k, writes=[WG.k])
                fw.dma("pool", lambda e, WU=WU, f=f: e.dma_start(out=WU[:, :, :], in_=wu[f]),
                       WU.k, writes=[WU.k])
                G = gp[f % 2]
                U = up[f % 2]
                for c in range(8):
                    fw.op("pe", lambda e, W=WG, G=G, c=c: e.matmul(
                        G[:, :], mm(W[:, c, :]), mm(xn[:, c, :]), start=(c == 0), stop=(c == 7)),
                        reads=[WG.k, xn.k], writes=[G.k], pe_acc=(c > 0))
                for c in range(8):
                    fw.op("pe", lambda e, W=WU, U=U, c=c: e.matmul(
                        U[:, :], mm(W[:, c, :]), mm(xn[:, c, :]), start=(c == 0), stop=(c == 7)),
                        reads=[WU.k, xn.k], writes=[U.k], pe_acc=(c > 0))
                SG = sg[f % 2]
                fw.op("act", lambda e, SG=SG, G=G: e.activation(SG[:, :], G[:, :], AF.Silu),
                      reads=[G.k], writes=[SG.k])
                fw.op("dve", lambda e, SG=SG, U=U, f=f: e.tensor_tensor(
                    hid[:, f, :], SG[:, :], U[:, :], ALU.mult),
                    reads=[SG.k, U.k], writes=[hid.k])
            for dc in range(8):
                W = wdb[di % 2]
                di += 1
                fw.dma("sp" if dc % 2 == 0 else "pool",
                       lambda e, W=W, dc=dc: e.dma_start(out=W[:, :, :], in_=wd[dc]),
                       W.k, writes=[W.k])
                O = op_[dc % 2]
                for f in range(NFC):
                    fw.op("pe", lambda e, W=W, O=O, f=f: e.matmul(
                        O[:, :], mm(W[:, f, :]), mm(hid[:, f, :]), start=(f == 0), stop=(f == NFC - 1)),
                        reads=[W.k, hid.k], writes=[O.k], pe_acc=(f > 0))
                XO = xo[dc % 2]
                fw.op("dve", lambda e, XO=XO, O=O, X=X, dc=dc: e.scalar_tensor_tensor(
                    XO[:, :], O[:, :], 0.5, X[:, dc, :], ALU.mult, ALU.add),
                    reads=[O.k, X.k], writes=[XO.k])
                fw.dma("sp", lambda e, XO=XO, dc=dc, ts=ts: e.dma_start(out=xout[:, dc, ts], in_=XO[:, :]),
                       XO.k, reads=[XO.k])


def mm_group(fw, O, out_ap, pairs, reads):
    n = len(pairs)
    for i, (l, r) in enumerate(pairs):
        fw.op("pe", lambda e, l=l, r=r, i=i: e.matmul(out_ap, mm(l), mm(r), start=(i == 0),
                                                       stop=(i == n - 1)),
              reads=reads, writes=[O.k], pe_acc=(i > 0))


def load(fw, q, B, out_ap, in_ap):
    fw.dma(q, lambda e: e.dma_start(out=out_ap, in_=in_ap), B.k, writes=[B.k])


def store(fw, q, B, out_ap, in_ap):
    fw.dma(q, lambda e: e.dma_start(out=out_ap, in_=in_ap), B.k, reads=[B.k])


def norm_tile(fw, X, hT, sq, ss_ps, rstd, ones, g_sb, epsb, NT):
    for c in range(8):
        S = sq[c % 2]
        fw.op("act", lambda e, S=S, c=c: e.activation(S[:, :], X[:, c, :], AF.Square),
              reads=[X.k], writes=[S.k])
        fw.op("pe", lambda e, S=S, c=c: e.matmul(ss_ps[:, :NT], ones[:, :], S[:, :],
                                                  start=(c == 0), stop=(c == 7)),
              reads=[ones.k, S.k], writes=[ss_ps.k], pe_acc=(c > 0))
    rstd_from_ss(fw, rstd, ss_ps, D, NT, epsb)
    for c in range(8):
        fw.op("dve", lambda e, c=c: e.scalar_tensor_tensor(
            hT[:, c, :], X[:, c, :], g_sb[:, c:c + 1], rstd[:, :NT], ALU.mult, ALU.mult),
            reads=[X.k, g_sb.k, rstd.k], writes=[hT.k])


def inproj_phase(fw, xT_in, w_in, ncols, gnorm, fm_groups, tm_groups, NT=512):
    with fw.phase():
        ones = Buf(fw, "ones", [128, 128])
        epsb = Buf(fw, "epsb", [128, 1])
        g_sb = Buf(fw, "g_sb", [128, 8])
        W = Buf(fw, "W", [128, 8, ncols])
        xt = [Buf(fw, f"xt{i}", [128, 8, NT]) for i in range(2)]
        hT = Buf(fw, "hT", [128, 8, NT])
        sq = [Buf(fw, f"sq{i}", [128, NT]) for i in range(2)]
        rstd = Buf(fw, "rstd", [128, NT])
        stf = [Buf(fw, f"stf{i}", [128, NT]) for i in range(3)]
        stt = [Buf(fw, f"stt{i}", [128, 512]) for i in range(3)]
        ss_ps = Buf(fw, "ss_ps", [128, NT], psum=True)
        fp = [Buf(fw, f"fp{i}", [128, NT], psum=True) for i in range(3)]
        tp = [Buf(fw, f"tp{i}", [128, 512], psum=True) for i in range(3)]
        fw.op("pool", lambda e: e.memset(ones[:, :], 1.0), writes=[ones.k])
        fw.op("pool", lambda e: e.memset(epsb[:, :], EPS), writes=[epsb.k])
        load(fw, "sp", g_sb, g_sb[:, :], gnorm)
        fw.dma("pool", [lambda e, c=c: e.dma_start(out=W[:, c, :], in_=w_in[:, c, :]) for c in range(8)],
               W.k, writes=[W.k])
        xin = xT_in.rearrange("(c p) t -> p c t", p=128)
        k = 0
        for it in range(T // NT):
            X = xt[it % 2]
            ts = slice(it * NT, (it + 1) * NT)
            load(fw, "sp", X, X[:, :, :], xin[:, :, ts])
            norm_tile(fw, X, hT, sq, ss_ps, rstd, ones, g_sb, epsb, NT)
            for (c0, wd_, dst) in fm_groups:
                P_ = fp[k % 3]
                S_ = stf[k % 3]
                mm_group(fw, P_, P_[:wd_, :], [(W[:, c, c0:c0 + wd_], hT[:, c, :]) for c in range(8)],
                         [W.k, hT.k])
                eng = "act" if k % 2 == 0 else "dve"
                if eng == "act":
                    fw.op("act", lambda e, P_=P_, S_=S_, wd_=wd_: e.copy(S_[:wd_, :], P_[:wd_, :]),
                          reads=[P_.k], writes=[S_.k])
                else:
                    fw.op("dve", lambda e, P_=P_, S_=S_, wd_=wd_: e.tensor_copy(S_[:wd_, :], P_[:wd_, :]),
                          reads=[P_.k], writes=[S_.k])
                store(fw, "sp", S_, dst[:, ts], S_[:wd_, :])
                k += 1
            for sub in range(NT // 128):
                t0 = it * NT + sub * 128
                for (c0, wd_, dst) in tm_groups:
                    P_ = tp[k % 3]
                    S_ = stt[k % 3]
                    mm_group(fw, P_, P_[:, :wd_],
                             [(hT[:, c, sub * 128:(sub + 1) * 128], W[:, c, c0:c0 + wd_]) for c in range(8)],
                             [W.k, hT.k])
                    if k % 2 == 0:
                        fw.op("act", lambda e, P_=P_, S_=S_, wd_=wd_: e.copy(S_[:, :wd_], P_[:, :wd_]),
                              reads=[P_.k], writes=[S_.k])
                    else:
                        fw.op("dve", lambda e, P_=P_, S_=S_, wd_=wd_: e.tensor_copy(S_[:, :wd_], P_[:, :wd_]),
                              reads=[P_.k], writes=[S_.k])
                    store(fw, "pool", S_, dst[t0:t0 + 128, :], S_[:, :wd_])
                    k += 1


def outproj_phase(fw, xT_in, xT_out, yT, w_out, NT=512):
    with fw.phase():
        W = Buf(fw, "W", [128, 8, 1024])
        xt = [Buf(fw, f"xt{i}", [128, 8, NT]) for i in range(2)]
        yt = [Buf(fw, f"yt{i}", [128, 8, NT]) for i in range(2)]
        xo = [Buf(fw, f"xo{i}", [128, 8, NT]) for i in range(2)]
        op_ = [Buf(fw, f"op{i}", [128, NT], psum=True) for i in range(3)]
        fw.dma("pool", [lambda e, c=c: e.dma_start(out=W[:, c, :], in_=w_out[:, c, :]) for c in range(8)],
               W.k, writes=[W.k])
        xin = xT_in.rearrange("(c p) t -> p c t", p=128)
        yin = yT.rearrange("(c p) t -> p c t", p=128)
        xout = xT_out.rearrange("(c p) t -> p c t", p=128)
        k = 0
        for it in range(T // NT):
            X = xt[it % 2]
            Y = yt[it % 2]
            XO = xo[it % 2]
            ts = slice(it * NT, (it + 1) * NT)
            load(fw, "sp", X, X[:, :, :], xin[:, :, ts])
            load(fw, "pool", Y, Y[:, :, :], yin[:, :, ts])
            for dc in range(8):
                O = op_[k % 3]
                k += 1
                mm_group(fw, O, O[:, :], [(W[:, c, dc * 128:(dc + 1) * 128], Y[:, c, :]) for c in range(8)],
                         [W.k, Y.k])
                fw.op("dve", lambda e, O=O, X=X, XO=XO, dc=dc: e.tensor_tensor(
                    XO[:, dc, :], O[:, :], X[:, dc, :], ALU.add),
                    reads=[O.k, X.k], writes=[XO.k])
            store(fw, "sp", XO, xout[:, :, ts], XO[:, :, :])


def gla_consts():
    s = np.arange(128)[:, None]
    t = np.arange(128)[None, :]
    same = (s // 64) == (t // 64)
    Mc = np.zeros((128, 130), np.float32)
    Mc[:, :128] = np.where(same & (s <= t), -1.0 / 16, 0.0)
    Mc[:64, 128] = -1.0 / 16
    Mc[64:, 129] = -1.0 / 16
    M3 = np.where(same & (s > t), -1.0 / 16, 0.0).astype(np.float32)
    maskA = np.where(same & (s <= t), 1.0, 0.0).astype(np.float32)
    return Mc, M3, maskA


def gla_phase(fw, qT, kT, gT, lrT, k_tm, v_tm, yT, Mc_d, M3_d, maskA_d, wgk1_d, gnorm_d):
    with fw.phase():
        ones = Buf(fw, "ones", [128, 128])
        epsb = Buf(fw, "epsb", [128, 1])
        Mc = Buf(fw, "Mc", [128, 130])
        M3 = Buf(fw, "M3", [128, 128])
        mA = Buf(fw, "mA", [128, 128])
        wgk = Buf(fw, "wgk", [32, 256])
        wn = Buf(fw, "wn", [128, 1])
        qt = [Buf(fw, f"qt{i}", [128, 2, 128]) for i in range(2)]
        kt = [Buf(fw, f"kt{i}", [128, 2, 128]) for i in range(2)]
        gt = [Buf(fw, f"gt{i}", [128, 4, 128]) for i in range(2)]
        lr = [Buf(fw, f"lr{i}", [32, 128]) for i in range(2)]
        ktm = [Buf(fw, f"ktm{i}", [128, 256]) for i in range(2)]
        vtm = [Buf(fw, f"vtm{i}", [128, 512]) for i in range(2)]
        e1 = Buf(fw, "e1", [128, 256])
        sp_ = Buf(fw, "sp_", [128, 256])
        eG = Buf(fw, "eG", [128, 2, 130])
        enG = Buf(fw, "enG", [128, 2, 128])
        eD = Buf(fw, "eD", [128, 256])
        qd = Buf(fw, "qd", [128, 2, 128])
        kd = Buf(fw, "kd", [128, 2, 128])
        kk = Buf(fw, "kk", [128, 256])
        ATm = Buf(fw, "ATm", [128, 4, 128])
        oxs = Buf(fw, "oxs", [128, 4, 128])
        oT = Buf(fw, "oT", [128, 4, 128])
        sqo = Buf(fw, "sqo", [128, 4, 128])
        rstd = Buf(fw, "rstd", [128, 512])
        sg = Buf(fw, "sg", [128, 4, 128])
        ya = [Buf(fw, f"ya{i}", [128, 4, 128]) for i in range(2)]
        S = [Buf(fw, f"S{i}", [128, 2, 128]) for i in range(2)]
        zd_ps = Buf(fw, "zd_ps", [128, 512], psum=True)
        d_ps = Buf(fw, "d_ps", [128, 512], psum=True)
        gt_ps = Buf(fw, "gt_ps", [128, 2, 256], psum=True)
        at_ps = Buf(fw, "at_ps", [128, 4, 128], psum=True)
        oi_ps = Buf(fw, "oi_ps", [128, 4, 128], psum=True)
        ox_ps = Buf(fw, "ox_ps", [128, 4, 128], psum=True)
        st_ps = [Buf(fw, f"st_ps{i}", [128, 2, 256], psum=True) for i in range(2)]
        fw.op("pool", lambda e: e.memset(ones[:, :], 1.0), writes=[ones.k])
        fw.op("pool", lambda e: e.memset(epsb[:, :], EPS), writes=[epsb.k])
        for i in range(2):
            fw.op("pool", lambda e, i=i: e.memset(lr[i][:, :], 1.0), writes=[lr[i].k])
            fw.op("pool", lambda e, i=i: e.memset(S[i][:, :, :], 0.0), writes=[S[i].k])
        load(fw, "sp", Mc, Mc[:, :], Mc_d)
        load(fw, "sp", M3, M3[:, :], M3_d)
        load(fw, "sp", mA, mA[:, :], maskA_d)
        load(fw, "sp", wgk, wgk[0:17, :], wgk1_d)
        load(fw, "sp", wn, wn[:, :], gnorm_d)
        qTr = qT.rearrange("(c p) t -> p c t", p=128)
        kTr = kT.rearrange("(c p) t -> p c t", p=128)
        gTr = gT.rearrange("(c p) t -> p c t", p=128)
        yTr = yT.rearrange("(c p) t -> p c t", p=128)
        for it in range(T // 128):
            ts = slice(it * 128, (it + 1) * 128)
            b = it % 2
            Q, K_, G_, L, KT, VT = qt[b], kt[b], gt[b], lr[b], ktm[b], vtm[b]
            load(fw, "sp", Q, Q[:, :, :], qTr[:, :, ts])
            load(fw, "sp", K_, K_[:, :, :], kTr[:, :, ts])
            load(fw, "sp", G_, G_[:, :, :], gTr[:, :, ts])
            load(fw, "pool", L, L[0:16, :], lrT[:, ts])
            load(fw, "pool", KT, KT[:, :], k_tm[ts, :])
            load(fw, "pool", VT, VT[:, :], v_tm[ts, :])
            mm_group(fw, zd_ps, zd_ps[:, 0:256], [(L[0:17, :], wgk[0:17, :])], [L.k, wgk.k])
            fw.op("act", lambda e: e.activation(e1[:, :], zd_ps[:, 0:256], AF.Exp, scale=-1.0),
                  reads=[zd_ps.k], writes=[e1.k])
            fw.op("act", lambda e: e.activation(sp_[:, :], e1[:, :], AF.Ln, bias=1.0),
                  reads=[e1.k], writes=[sp_.k])
            for c in range(2):
                mm_group(fw, gt_ps, gt_ps[:, c, 0:130], [(sp_[:, c * 128:(c + 1) * 128], Mc[:, :])],
                         [sp_.k, Mc.k])
            mm_group(fw, d_ps, d_ps[:, 0:256], [(M3[:, :], sp_[:, :])], [M3.k, sp_.k])
            fw.op("act", lambda e: e.activation(eG[:, :, :], gt_ps[:, :, 0:130], AF.Exp),
                  reads=[gt_ps.k], writes=[eG.k])
            fw.op("act", lambda e: e.activation(enG[:, :, :], gt_ps[:, :, 0:128], AF.Exp, scale=-1.0),
                  reads=[gt_ps.k], writes=[enG.k])
            fw.op("act", lambda e: e.activation(eD[:, :], d_ps[:, 0:256], AF.Exp),
                  reads=[d_ps.k], writes=[eD.k])
            fw.op("dve", lambda e, Q=Q: e.scalar_tensor_tensor(
                qd[:, :, :], Q[:, :, :], 0.125, eG[:, :, 0:128], ALU.mult, ALU.mult),
                reads=[Q.k, eG.k], writes=[qd.k])
            fw.op("dve", lambda e, K_=K_: e.tensor_tensor(kd[:, :, :], K_[:, :, :], enG[:, :, :], ALU.mult),
                  reads=[K_.k, enG.k], writes=[kd.k])
            fw.op("dve", lambda e, KT=KT: e.tensor_tensor(kk[:, :], KT[:, :], eD[:, :], ALU.mult),
                  reads=[KT.k, eD.k], writes=[kk.k])
            for h in range(4):
                c, pb = h // 2, (h % 2) * 64
                mm_group(fw, at_ps, at_ps[:, h, :], [(kd[pb:pb + 64, c, :], qd[pb:pb + 64, c, :])],
                         [kd.k, qd.k])
            fw.op("dve", lambda e: e.tensor_tensor(
                ATm[:, :, :], at_ps[:, :, :], mA[:, :].unsqueeze(1).to_broadcast([128, 4, 128]), ALU.mult),
                reads=[at_ps.k, mA.k], writes=[ATm.k])
            for h in range(4):
                mm_group(fw, oi_ps, oi_ps[:, h, :], [(VT[:, h * 128:(h + 1) * 128], ATm[:, h, :])],
                         [VT.k, ATm.k])
            for cc in range(2):
                Sc, Sn = S[cc], S[1 - cc]
                for h in range(4):
                    c, pb = h // 2, (h % 2) * 64
                    mm_group(fw, ox_ps, ox_ps[:, h, cc * 64:(cc + 1) * 64],
                             [(Sc[pb:pb + 64, c, :], qd[pb:pb + 64, c, cc * 64:(cc + 1) * 64])],
                             [Sc.k, qd.k])
                STP = st_ps[cc]
                for c in range(2):
                    mm_group(fw, STP, STP[:, c, :],
                             [(kk[cc * 64:(cc + 1) * 64, c * 128:(c + 1) * 128],
                               VT[cc * 64:(cc + 1) * 64, c * 256:(c + 1) * 256])], [kk.k, VT.k])
                for h in range(4):
                    c, pb = h // 2, (h % 2) * 64
                    fw.op("dve", lambda e, Sc=Sc, Sn=Sn, STP=STP, c=c, pb=pb, h=h, cc=cc:
                          e.scalar_tensor_tensor(
                              Sn[pb:pb + 64, c, :], Sc[pb:pb + 64, c, :], eG[pb:pb + 64, c, 128 + cc:129 + cc],
                              STP[pb:pb + 64, c, (h % 2) * 128:(h % 2) * 128 + 128], ALU.mult, ALU.add),
                          reads=[Sc.k, eG.k, STP.k], writes=[Sn.k])
            fw.op("act", lambda e: e.copy(oxs[:, :, :], ox_ps[:, :, :]), reads=[ox_ps.k], writes=[oxs.k])
            fw.op("dve", lambda e: e.tensor_tensor(oT[:, :, :], oi_ps[:, :, :], oxs[:, :, :], ALU.add),
                  reads=[oi_ps.k, oxs.k], writes=[oT.k])
            fw.op("act", lambda e: e.activation(sqo[:, :, :], oT[:, :, :], AF.Square),
                  reads=[oT.k], writes=[sqo.k])
            mm_group(fw, zd_ps, zd_ps[:, :], [(ones[:, :], sqo[:, :, :].rearrange("p h t -> p (h t)"))],
                     [ones.k, sqo.k])
            rstd_from_ss(fw, rstd, zd_ps, 128, 512, epsb)
            fw.op("act", lambda e, G_=G_: e.activation(sg[:, :, :], G_[:, :, :], AF.Silu),
                  reads=[G_.k], writes=[sg.k])
            YA = ya[b]
            fw.op("dve", lambda e, YA=YA: e.scalar_tensor_tensor(
                YA[:, :, :].rearrange("p h t -> p (h t)"), oT[:, :, :].rearrange("p h t -> p (h t)"),
                wn[:, 0:1], rstd[:, :], ALU.mult, ALU.mult),
                reads=[oT.k, wn.k, rstd.k], writes=[YA.k])
            fw.op("dve", lambda e, YA=YA: e.tensor_tensor(YA[:, :, :], YA[:, :, :], sg[:, :, :], ALU.mult),
                  reads=[YA.k, sg.k], writes=[YA.k])
            store(fw, "sp", YA, yTr[:, :, ts], YA[:, :, :])


def pool_consts():
    s = np.arange(128)[:, None]
    t = np.arange(128)[None, :]
    cur = np.zeros((4, 128, 128), np.float32)
    prev = np.zeros((4, 128, 128), np.float32)
    cur0 = np.zeros((4, 128, 128), np.float32)
    for g, w in enumerate((2, 4, 8, 16)):
        d = t - s
        cur[g] = np.where((d >= 0) & (d < w), 1.0 / w, 0.0) - np.eye(128)
        cnt = np.minimum(t + 1, w).astype(np.float64)
        cur0[g] = np.where((d >= 0) & (d < w), 1.0 / cnt, 0.0) - np.eye(128)
        d2 = t + 128 - s
        prev[g] = np.where((d2 >= 0) & (d2 < w), 1.0 / w, 0.0)
    return cur.astype(np.float32), prev.astype(np.float32), cur0.astype(np.float32)


def pool_phase(fw, u_tm, yT, wpool_d, pscale_d, cur_d, prev_d, cur0_d):
    with fw.phase():
        wp = Buf(fw, "wp", [128, 4, 128])
        psc = Buf(fw, "psc", [128, 4])
        Pc = Buf(fw, "Pc", [128, 4, 128])
        Pp = Buf(fw, "Pp", [128, 4, 128])
        P0 = Buf(fw, "P0", [128, 4, 128])
        ut = [Buf(fw, f"ut{i}", [128, 512]) for i in range(3)]
        pl = Buf(fw, "pl", [128, 4, 128])
        yb = [Buf(fw, f"yb{i}", [128, 4, 128]) for i in range(2)]
        pt_ps = [Buf(fw, f"pt_ps{i}", [128, 4, 128], psum=True) for i in range(2)]
        y_ps = [Buf(fw, f"y_ps{i}", [128, 4, 128], psum=True) for i in range(2)]
        load(fw, "sp", wp, wp[:, :, :], wpool_d)
        load(fw, "sp", psc, psc[:, :], pscale_d)
        load(fw, "sp", Pc, Pc[:, :, :], cur_d)
        load(fw, "sp", Pp, Pp[:, :, :], prev_d)
        load(fw, "sp", P0, P0[:, :, :], cur0_d)
        yTr = yT.rearrange("(c p) t -> p c t", p=128)
        for it in range(T // 128):
            ts = slice(it * 128, (it + 1) * 128)
            U = ut[it % 3]
            Uprev = ut[(it - 1) % 3]
            load(fw, "sp", U, U[:, :], u_tm[ts, :])
            PT = pt_ps[it % 2]
            for g in range(4):
                gs = slice(g * 128, (g + 1) * 128)
                if it == 0:
                    mm_group(fw, PT, PT[:, g, :], [(U[:, gs], P0[:, g, :])], [U.k, P0.k])
                else:
                    mm_group(fw, PT, PT[:, g, :], [(U[:, gs], Pc[:, g, :]), (Uprev[:, gs], Pp[:, g, :])],
                             [U.k, Uprev.k, Pc.k, Pp.k])
            fw.op("act", lambda e, PT=PT: e.copy(pl[:, :, :], PT[:, :, :]), reads=[PT.k], writes=[pl.k])
            YP = y_ps[it % 2]
            for g in range(4):
                mm_group(fw, YP, YP[:, g, :], [(wp[:, g, :], pl[:, g, :])], [wp.k, pl.k])
            YB = yb[it % 2]
            fw.op("dve", lambda e, YB=YB, YP=YP: e.tensor_tensor(
                YB[:, :, :], YP[:, :, :], psc[:, :].unsqueeze(2).to_broadcast([128, 4, 128]), ALU.mult),
                reads=[YP.k, psc.k], writes=[YB.k])
            store(fw, "pool", YB, yTr[:, :, ts], YB[:, :, :])


def conv_phase(fw, xbcT, xcT, xB_tm, cw_d, cb_d, ident_d, NT=512):
    with fw.phase():
        cw = Buf(fw, "cw", [128, 8, 4])
        cb = Buf(fw, "cb", [128, 8])
        idn = Buf(fw, "idn", [128, 128])
        xt = [Buf(fw, f"xt{i}", [128, NT + 3]) for i in range(3)]
        acc = [Buf(fw, f"acc{i}", [128, NT]) for i in range(2)]
        xc = [Buf(fw, f"xc{i}", [128, NT]) for i in range(3)]
        s2 = [Buf(fw, f"s2{i}", [128, 4, 128]) for i in range(3)]
        tr_ps = [Buf(fw, f"tr_ps{i}", [128, 4, 128], psum=True) for i in range(2)]
        load(fw, "sp", cw, cw[:, :, :], cw_d)
        load(fw, "sp", cb, cb[:, :], cb_d)
        load(fw, "sp", idn, idn[:, :], ident_d)
        k = 0
        for it in range(T // NT):
            t0 = it * NT
            for c in range(8):
                X = xt[k % 3]
                A = acc[k % 2]
                XC = xc[k % 3]
                rows = slice(c * 128, (c + 1) * 128)
                if it == 0:
                    fw.op("pool", lambda e, X=X: e.memset(X[:, 0:3], 0.0), writes=[X.k])
                    load(fw, "sp", X, X[:, 3:NT + 3], xbcT[rows, 0:NT])
                else:
                    load(fw, "sp", X, X[:, :], xbcT[rows, t0 - 3:t0 + NT])
                fw.op("dve", lambda e, X=X, A=A, c=c: e.tensor_scalar(
                    A[:, :], X[:, 0:NT], cw[:, c, 0:1], None, ALU.mult), reads=[X.k, cw.k], writes=[A.k])
                for j in range(1, 4):
                    fw.op("dve", lambda e, X=X, A=A, c=c, j=j: e.scalar_tensor_tensor(
                        A[:, :], X[:, j:j + NT], cw[:, c, j:j + 1], A[:, :], ALU.mult, ALU.add),
                        reads=[X.k, cw.k, A.k], writes=[A.k])
                fw.op("act", lambda e, A=A, XC=XC, c=c: e.activation(
                    XC[:, :], A[:, :], AF.Silu, bias=cb[:, c:c + 1]), reads=[A.k, cb.k], writes=[XC.k])
                store(fw, "pool", XC, xcT[rows, t0:t0 + NT], XC[:, :])
                if c < 6:
                    TP = tr_ps[k % 2]
                    for sub in range(4):
                        fw.op("pe", lambda e, TP=TP, XC=XC, sub=sub: e.transpose(
                            TP[:, sub, :], XC[:, sub * 128:(sub + 1) * 128], idn[:, :]),
                            reads=[XC.k, idn.k], writes=[TP.k])
                    S2 = s2[k % 3]
                    fw.op("act", lambda e, TP=TP, S2=S2: e.copy(S2[:, :, :], TP[:, :, :]),
                          reads=[TP.k], writes=[S2.k])
                    store(fw, "sp", S2, xB_tm[t0:t0 + NT, c * 128:(c + 1) * 128].rearrange(
                        "(s p) v -> p s v", p=128), S2[:, :, :])
                k += 1


def ssd_consts():
    t = np.arange(128)[:, None]
    s = np.arange(128)[None, :]
    U = (t > s).astype(np.float32)
    Tri = (t <= s).astype(np.float32)
    NEG = np.where(s >= t, 0.0, -30000.0).astype(np.float32)
    return U, Tri, NEG


def ssd_phase(fw, xcT, xB_tm, z_tm, dt_tm, yT, U_d, Tri_d, NEG_d, ident_d, dtb_d, alog_d, dsk_d, wn_d):
    with fw.phase():
        ones = Buf(fw, "ones", [128, 128])
        epsb = Buf(fw, "epsb", [128, 1])
        U = Buf(fw, "U", [128, 128])
        Tri = Buf(fw, "Tri", [128, 128])
        NEG = Buf(fw, "NEG", [128, 128])
        idn = Buf(fw, "idn", [128, 128])
        dtb = Buf(fw, "dtb", [128, 8])
        An = Buf(fw, "An", [128, 8])
        dsk = Buf(fw, "dsk", [128, 8])
        wn = Buf(fw, "wn", [128, 512])
        BT = [Buf(fw, f"BT{i}", [128, 2, 128]) for i in range(2)]
        CT = [Buf(fw, f"CT{i}", [128, 2, 128]) for i in range(2)]
        XB = [Buf(fw, f"XB{i}", [128, 768]) for i in range(2)]
        Z = [Buf(fw, f"Z{i}", [128, 512]) for i in range(2)]
        DT = [Buf(fw, f"DT{i}", [128, 8]) for i in range(2)]
        dt = Buf(fw, "dt", [128, 8])
        a = Buf(fw, "a", [128, 8])
        e3 = Buf(fw, "e3", [128, 3, 8])
        xdt = Buf(fw, "xdt", [128, 8, 64])
        xdt2 = Buf(fw, "xdt2", [128, 8, 64])
        CBT = Buf(fw, "CBT", [128, 2, 128])
        lh = [Buf(fw, f"lh{i}", [128, 128]) for i in range(2)]
        Lh = [Buf(fw, f"Lh{i}", [128, 128]) for i in range(2)]
        Wh = [Buf(fw, f"Wh{i}", [128, 128]) for i in range(2)]
        yo = Buf(fw, "yo", [128, 8, 64])
        y = Buf(fw, "y", [128, 8, 64])
        sz = Buf(fw, "sz", [128, 512])
        junk = Buf(fw, "junk", [128, 256])
        ssq = Buf(fw, "ssq", [128, 2])
        rs = Buf(fw, "rs", [128, 2])
        yc = [Buf(fw, f"yc{i}", [128, 4, 128]) for i in range(2)]
        ST = [Buf(fw, f"ST{i}", [128, 4, 64]) for i in range(2)]
        sm_ps = Buf(fw, "sm_ps", [128, 3, 8], psum=True)
        cb_ps = Buf(fw, "cb_ps", [128, 2, 128], psum=True)
        dm_ps = [Buf(fw, f"dm_ps{i}", [128, 512], psum=True) for i in range(2)]
        yd_ps = Buf(fw, "yd_ps", [128, 8, 64], psum=True)
        yo_ps = Buf(fw, "yo_ps", [128, 8, 64], psum=True)
        st_ps = Buf(fw, "st_ps", [128, 2, 256], psum=True)
        tr_ps = Buf(fw, "tr_ps", [128, 4, 128], psum=True)
        fw.op("pool", lambda e: e.memset(ones[:, :], 1.0), writes=[ones.k])
        fw.op("pool", lambda e: e.memset(epsb[:, :], EPS), writes=[epsb.k])
        for i in range(2):
            fw.op("pool", lambda e, i=i: e.memset(ST[i][:, :, :], 0.0), writes=[ST[i].k])
        load(fw, "sp", U, U[:, :], U_d)
        load(fw, "sp", Tri, Tri[:, :], Tri_d)
        load(fw, "sp", NEG, NEG[:, :], NEG_d)
        load(fw, "sp", idn, idn[:, :], ident_d)
        load(fw, "sp", dtb, dtb[:, :], dtb_d.partition_broadcast(128))
        load(fw, "sp", An, An[:, :], alog_d.partition_broadcast(128))
        load(fw, "sp", dsk, dsk[:, :], dsk_d.partition_broadcast(128))
        load(fw, "sp", wn, wn[:, :], wn_d.partition_broadcast(128))
        fw.op("act", lambda e: e.activation(An[:, :], An[:, :], AF.Exp), reads=[An.k], writes=[An.k])
        fw.op("dve", lambda e: e.tensor_scalar(An[:, :], An[:, :], -1.0, None, ALU.mult),
              reads=[An.k], writes=[An.k])
        BTr = xcT[512:768].rearrange("(g p) t -> p g t", p=128)
        CTr = xcT[768:1024].rearrange("(g p) t -> p g t", p=128)
        yTr = yT.rearrange("(c p) t -> p c t", p=128)

        def bc8(ap):
            return ap.unsqueeze(2).to_broadcast([128, 8, 64])

        for n in range(T // 128):
            ts = slice(n * 128, (n + 1) * 128)
            b = n % 2
            B_, C_, X_, Z_, D_ = BT[b], CT[b], XB[b], Z[b], DT[b]
            load(fw, "sp", B_, B_[:, :, :], BTr[:, :, ts])
            load(fw, "sp", C_, C_[:, :, :], CTr[:, :, ts])
            load(fw, "pool", X_, X_[:, :], xB_tm[ts, :])
            load(fw, "pool", Z_, Z_[:, :], z_tm[ts, :])
            load(fw, "pool", D_, D_[:, :], dt_tm[ts, :])
            x3 = X_[:, 0:512].rearrange("p (h d) -> p h d", h=8)
            fw.op("dve", lambda e, D_=D_: e.tensor_tensor(dt[:, :], D_[:, :], dtb[:, :], ALU.add),
                  reads=[D_.k, dtb.k], writes=[dt.k])
            fw.op("act", lambda e: e.activation(dt[:, :], dt[:, :], AF.Exp), reads=[dt.k], writes=[dt.k])
            fw.op("act", lambda e: e.activation(dt[:, :], dt[:, :], AF.Ln, bias=1.0), reads=[dt.k], writes=[dt.k])
            fw.op("dve", lambda e: e.tensor_tensor(a[:, :], dt[:, :], An[:, :], ALU.mult),
                  reads=[dt.k, An.k], writes=[a.k])
            mm_group(fw, sm_ps, sm_ps[:, 0, :], [(Tri[:, :], a[:, :])], [Tri.k, a.k])
            mm_group(fw, sm_ps, sm_ps[:, 1, :], [(ones[:, :], a[:, :])], [ones.k, a.k])
            mm_group(fw, sm_ps, sm_ps[:, 2, :], [(U[:, :], a[:, :])], [U.k, a.k])
            fw.op("act", lambda e: e.activation(e3[:, :, :], sm_ps[:, :, :], AF.Exp),
                  reads=[sm_ps.k], writes=[e3.k])
            fw.op("dve", lambda e, x3=x3, X_=X_: e.tensor_tensor(xdt[:, :, :], x3, bc8(dt[:, :]), ALU.mult),
                  reads=[X_.k, dt.k], writes=[xdt.k])
            fw.op("dve", lambda e: e.tensor_tensor(xdt2[:, :, :], xdt[:, :, :], bc8(e3[:, 2, :]), ALU.mult),
                  reads=[xdt.k, e3.k], writes=[xdt2.k])
            for g in range(2):
                mm_group(fw, cb_ps, cb_ps[:, g, :], [(B_[:, g, :], C_[:, g, :])], [B_.k, C_.k])
            fw.op("act", lambda e: e.copy(CBT[:, :, :], cb_ps[:, :, :]), reads=[cb_ps.k], writes=[CBT.k])
            for h in range(8):
                g = h // 4
                LH, LL, WW = lh[h % 2], Lh[h % 2], Wh[h % 2]
                DM = dm_ps[(h // 4) % 2]
                dm = DM[:, (h % 4) * 128:(h % 4 + 1) * 128]
                fw.op("dve", lambda e, LH=LH, h=h: e.tensor_scalar(
                    LH[:, :], U[:, :], a[:, h:h + 1], None, ALU.mult), reads=[U.k, a.k], writes=[LH.k])
                mm_group(fw, DM, dm, [(LH[:, :], Tri[:, :]), (idn[:, :], NEG[:, :])],
                         [LH.k, Tri.k, idn.k, NEG.k])
                fw.op("act", lambda e, LL=LL, dm=dm: e.activation(LL[:, :], dm, AF.Exp),
                      reads=[DM.k], writes=[LL.k])
                fw.op("dve", lambda e, LL=LL, WW=WW, g=g: e.tensor_tensor(
                    WW[:, :], LL[:, :], CBT[:, g, :], ALU.mult), reads=[LL.k, CBT.k], writes=[WW.k])
                mm_group(fw, yd_ps, yd_ps[:, h, :], [(WW[:, :], xdt[:, h, :])], [WW.k, xdt.k])
            for g in range(2):
                mm_group(fw, yo_ps, yo_ps[:, g * 4:(g + 1) * 4, :].rearrange("p h d -> p (h d)"),
                         [(C_[:, g, :], ST[g][:, :, :].rearrange("p h d -> p (h d)"))], [C_.k, ST[g].k])
            fw.op("dve", lambda e: e.tensor_tensor(yo[:, :, :], yo_ps[:, :, :], bc8(e3[:, 0, :]), ALU.mult),
                  reads=[yo_ps.k, e3.k], writes=[yo.k])
            fw.op("dve", lambda e: e.tensor_tensor(y[:, :, :], yd_ps[:, :, :], yo[:, :, :], ALU.add),
                  reads=[yd_ps.k, yo.k], writes=[y.k])
            fw.op("dve", lambda e, x3=x3, X_=X_: e.tensor_tensor(yo[:, :, :], x3, bc8(dsk[:, :]), ALU.mult),
                  reads=[X_.k, dsk.k], writes=[yo.k])
            fw.op("dve", lambda e: e.tensor_tensor(y[:, :, :], y[:, :, :], yo[:, :, :], ALU.add),
                  reads=[y.k, yo.k], writes=[y.k])
            fw.op("act", lambda e, Z_=Z_: e.activation(sz[:, :], Z_[:, :], AF.Silu), reads=[Z_.k], writes=[sz.k])
            y2 = y[:, :, :].rearrange("p h d -> p (h d)")
            fw.op("dve", lambda e, y2=y2: e.tensor_tensor(y2, y2, sz[:, :], ALU.mult),
                  reads=[y.k, sz.k], writes=[y.k])
            fw.op("pool", lambda e: e.memset(ssq[:, :], 0.0), writes=[ssq.k])
            for g in range(2):
                fw.op("act", lambda e, g=g, y2=y2: e.activation(
                    junk[:, :], y2[:, g * 256:(g + 1) * 256], AF.Square, accum_out=ssq[:, g:g + 1]),
                    reads=[y.k], writes=[junk.k, ssq.k])
            rstd_from_ss(fw, rs, ssq, 256, 2, epsb)
            for g in range(2):
                fw.op("dve", lambda e, g=g, y2=y2: e.scalar_tensor_tensor(
                    y2[:, g * 256:(g + 1) * 256], y2[:, g * 256:(g + 1) * 256], rs[:, g:g + 1],
                    wn[:, g * 256:(g + 1) * 256], ALU.mult, ALU.mult),
                    reads=[y.k, rs.k, wn.k], writes=[y.k])
            for c in range(4):
                fw.op("pe", lambda e, c=c, y2=y2: e.transpose(tr_ps[:, c, :], y2[:, c * 128:(c + 1) * 128],
                                                               idn[:, :]),
                      reads=[y.k, idn.k], writes=[tr_ps.k])
            YC = yc[b]
            fw.op("act", lambda e, YC=YC: e.copy(YC[:, :, :], tr_ps[:, :, :]), reads=[tr_ps.k], writes=[YC.k])
            store(fw, "sp", YC, yTr[:, :, ts], YC[:, :, :])
            for g in range(2):
                mm_group(fw, st_ps, st_ps[:, g, :],
                         [(X_[:, 512 + g * 128:512 + (g + 1) * 128],
                           xdt2[:, g * 4:(g + 1) * 4, :].rearrange("p h d -> p (h d)"))], [X_.k, xdt2.k])
            for g in range(2):
                fw.op("dve", lambda e, g=g: e.tensor_tensor(
                    ST[g][:, :, :], ST[g][:, :, :],
                    e3[:, 1, g * 4:(g + 1) * 4].unsqueeze(2).to_broadcast([128, 4, 64]), ALU.mult),
                    reads=[ST[g].k, e3.k], writes=[ST[g].k])
                fw.op("dve", lambda e, g=g: e.tensor_tensor(
                    ST[g][:, :, :].rearrange("p h d -> p (h d)"),
                    ST[g][:, :, :].rearrange("p h d -> p (h d)"), st_ps[:, g, :], ALU.add),
                    reads=[ST[g].k, st_ps.k], writes=[ST[g].k])


def qknorm_phase(fw, qT, kT, qnT, knT, wq_d, wk_d, bones_d, NT=512):
    with fw.phase():
        bo = Buf(fw, "bo", [128, 128])
        epsb = Buf(fw, "epsb", [128, 1])
        wq = Buf(fw, "wq", [128, 1])
        wk = Buf(fw, "wk", [128, 1])
        xt = [Buf(fw, f"xt{i}", [128, NT]) for i in range(3)]
        sq = [Buf(fw, f"sq{i}", [128, NT]) for i in range(2)]
        rstd = [Buf(fw, f"rstd{i}", [128, NT]) for i in range(2)]
        xo = [Buf(fw, f"xo{i}", [128, NT]) for i in range(3)]
        ss_ps = [Buf(fw, f"ss_ps{i}", [128, NT], psum=True) for i in range(2)]
        fw.op("pool", lambda e: e.memset(epsb[:, :], EPS), writes=[epsb.k])
        load(fw, "sp", bo, bo[:, :], bones_d)
        load(fw, "sp", wq, wq[:, :], wq_d)
        load(fw, "sp", wk, wk[:, :], wk_d)
        fw.op("dve", lambda e: e.tensor_scalar(wq[:, :], wq[:, :], 0.125, None, ALU.mult),
              reads=[wq.k], writes=[wq.k])
        k = 0
        for (src, dst, w) in ((qT, qnT, wq), (kT, knT, wk)):
            for it in range(T // NT):
                ts = slice(it * NT, (it + 1) * NT)
                for c in range(4):
                    X, S_, R_, XO, SS = xt[k % 3], sq[k % 2], rstd[k % 2], xo[k % 3], ss_ps[k % 2]
                    k += 1
                    rows = slice(c * 128, (c + 1) * 128)
                    load(fw, "sp", X, X[:, :], src[rows, ts])
                    fw.op("act", lambda e, X=X, S_=S_: e.activation(S_[:, :], X[:, :], AF.Square),
                          reads=[X.k], writes=[S_.k])
                    mm_group(fw, SS, SS[:, :], [(bo[:, :], S_[:, :])], [bo.k, S_.k])
                    rstd_from_ss(fw, R_, SS, 64, NT, epsb)
                    fw.op("dve", lambda e, X=X, XO=XO, R_=R_, w=w: e.scalar_tensor_tensor(
                        XO[:, :], X[:, :], w[:, 0:1], R_[:, :], ALU.mult, ALU.mult),
                        reads=[X.k, w.k, R_.k], writes=[XO.k])
                    store(fw, "pool", XO, dst[rows, ts], XO[:, :])


def t5_consts():
    k = np.arange(128)[:, None]
    q = np.arange(128)[None, :]
    E = np.zeros((33, 2, 128, 128), np.float32)
    for blk, off in ((0, 0), (1, 128)):
        n = q - k + off
        valid = n >= 0
        nn = np.maximum(n, 0)
        nf = np.maximum(nn, 1).astype(np.float32)
        large = 16 + (np.log(nf / np.float32(16)) / np.float32(np.log(128 / 16)) * np.float32(16)).astype(np.int32)
        large = np.minimum(large, 31)
        bucket = np.where(nn < 16, nn, large)
        for b in range(32):
            E[b, blk] = ((bucket == b) & valid).astype(np.float32)
        E[32, blk] = (~valid).astype(np.float32)
    return E.reshape(33, 2 * 16384)


def bias_phase(fw, rel_bias_d, Ecat_d, BnT):
    with fw.phase():
        tab = Buf(fw, "tab", [64, 4])
        t31 = Buf(fw, "t31", [32, 4])
        Ec = [Buf(fw, f"Ec{i}", [33, 4096]) for i in range(2)]
        out = [Buf(fw, f"out{i}", [4, 4096]) for i in range(2)]
        b_ps = [Buf(fw, f"b_ps{i}", [4, 512], psum=True) for i in range(2)]
        fw.op("pool", lambda e: e.memset(tab[:, :], -30000.0), writes=[tab.k])
        load(fw, "sp", tab, tab[0:32, :], rel_bias_d)
        load(fw, "sp", t31, t31[:, :], rel_bias_d[31:32, :].partition_broadcast(32))
        fw.op("dve", lambda e: e.tensor_tensor(tab[0:32, :], tab[0:32, :], t31[:, :], ALU.subtract),
              reads=[tab.k, t31.k], writes=[tab.k])
        k = 0
        for pc in range(8):
            E_, O_ = Ec[pc % 2], out[pc % 2]
            load(fw, "sp", E_, E_[:, :], Ecat_d[:, pc * 4096:(pc + 1) * 4096])
            for j in range(8):
                P_ = b_ps[k % 2]
                k += 1
                mm_group(fw, P_, P_[:, :], [(tab[0:33, :], E_[0:33, j * 512:(j + 1) * 512])], [tab.k, E_.k])
                fw.op("dve", lambda e, P_=P_, O_=O_, j=j: e.tensor_copy(O_[:, j * 512:(j + 1) * 512], P_[:, :]),
                      reads=[P_.k], writes=[O_.k])
            store(fw, "pool", O_, BnT[:, pc * 4096:(pc + 1) * 4096], O_[:, :])


def attn_phase(fw, qnT, knT, v_tm, BnT, yT, ident_d, lq1, lk1, lq2, lk2, subln_d, lambda_init):
    NQ = 512
    with fw.phase():
        idn = Buf(fw, "idn", [128, 128])
        epsb = Buf(fw, "epsb", [128, 1])
        lv = Buf(fw, "lv", [128, 4, 64])
        lt = Buf(fw, "lt", [128, 2, 64])
        ls = Buf(fw, "ls", [128, 2])
        lam = Buf(fw, "lam", [128, 1])
        subw = Buf(fw, "subw", [128, 128])
        qn = Buf(fw, "qn", [128, T])
        kn = Buf(fw, "kn", [128, T])
        va = Buf(fw, "va", [128, 32, 129])
        Bn = Buf(fw, "Bn", [128, 2, 128])
        tmp = [Buf(fw, f"tmp{i}", [128, 128]) for i in range(2)]
        PT = [Buf(fw, f"PT{i}", [128, NQ]) for i in range(3)]
        om = [Buf(fw, f"om{i}", [128, 4, 129]) for i in range(2)]
        rr = Buf(fw, "rr", [128, 4, 2])
        t1 = Buf(fw, "t1", [128, 128])
        o = Buf(fw, "o", [128, 4, 128])
        junk = Buf(fw, "junk", [128, 128])
        ssq = Buf(fw, "ssq", [128, 4])
        rs = Buf(fw, "rs", [128, 4])
        yd = [Buf(fw, f"yd{i}", [128, 4, 128]) for i in range(2)]
        s_ps = [Buf(fw, f"s_ps{i}", [128, NQ], psum=True) for i in range(2)]
        o_ps = [Buf(fw, f"o_ps{i}", [128, 512], psum=True) for i in range(4)]
        tr_ps = Buf(fw, "tr_ps", [128, 4, 128], psum=True)
        fw.op("pool", lambda e: e.memset(epsb[:, :], EPS), writes=[epsb.k])
        load(fw, "sp", idn, idn[:, :], ident_d)
        for i, l in enumerate((lq1, lk1, lq2, lk2)):
            load(fw, "sp", lv, lv[:, i, :], l.partition_broadcast(128))
        load(fw, "sp", subw, subw[:, :], subln_d.partition_broadcast(128))
        fw.op("dve", lambda e: e.tensor_scalar(subw[:, :], subw[:, :], 1.0 - lambda_init, None, ALU.mult),
              reads=[subw.k], writes=[subw.k])
        fw.op("dve", lambda e: e.tensor_tensor(lt[:, 0, :], lv[:, 0, :], lv[:, 1, :], ALU.mult),
              reads=[lv.k], writes=[lt.k])
        fw.op("dve", lambda e: e.tensor_tensor(lt[:, 1, :], lv[:, 2, :], lv[:, 3, :], ALU.mult),
              reads=[lv.k, lt.k], writes=[lt.k])
        fw.op("dve", lambda e: e.reduce_sum(ls[:, :], lt[:, :, :], axis=AX.X), reads=[lt.k], writes=[ls.k])
        fw.op("act", lambda e: e.activation(ls[:, :], ls[:, :], AF.Exp), reads=[ls.k], writes=[ls.k])
        fw.op("dve", lambda e: e.tensor_tensor(lam[:, :], ls[:, 0:1], ls[:, 1:2], ALU.subtract),
              reads=[ls.k], writes=[lam.k])
        fw.op("dve", lambda e: e.tensor_scalar(lam[:, :], lam[:, :], float(lambda_init), None, ALU.add),
              reads=[lam.k], writes=[lam.k])
        fw.op("pool", lambda e: e.memset(va[:, :, 128:129], 1.0), writes=[va.k])
        yTr = yT.rearrange("(c p) t -> p c t", p=128)
        pk = 0
        sk = 0
        for h in range(4):
            rows = slice(h * 128, (h + 1) * 128)
            load(fw, "sp", qn, qn[:, :], qnT[rows, :])
            load(fw, "sp", kn, kn[:, :], knT[rows, :])
            load(fw, "pool", va, va[:, :, 0:128],
                 v_tm[:, rows].rearrange("(b p) v -> p b v", p=128))
            load(fw, "pool", Bn, Bn[:, :, :], BnT[h].rearrange("(b k q) -> k b q", b=2, k=128))
            for qg in range(T // NQ):
                YD = yd[qg % 2]
                for m in range(2):
                    mp = slice(m * 64, (m + 1) * 64)
                    nkb = 4 * qg + 4
                    for kb in range(nkb):
                        SP = s_ps[sk % 2]
                        sk += 1
                        mm_group(fw, SP, SP[:, :], [(kn[mp, kb * 128:(kb + 1) * 128],
                                                     qn[mp, qg * NQ:(qg + 1) * NQ])], [kn.k, qn.k])
                        P_ = PT[pk % 3]
                        pk += 1
                        if kb <= 4 * qg - 2:
                            fw.op("act", lambda e, P_=P_, SP=SP: e.activation(P_[:, :], SP[:, :], AF.Exp),
                                  reads=[SP.k], writes=[P_.k])
                            js = range(4)
                        else:
                            js = []
                            for j in range(4):
                                qb = 4 * qg + j
                                cs = slice(j * 128, (j + 1) * 128)
                                if kb > qb:
                                    continue
                                js.append(j)
                                if kb < qb - 1:
                                    fw.op("act", lambda e, P_=P_, SP=SP, cs=cs: e.activation(
                                        P_[:, cs], SP[:, cs], AF.Exp), reads=[SP.k], writes=[P_.k])
                                else:
                                    TM = tmp[j % 2]
                                    bi = 0 if kb == qb else 1
                                    fw.op("dve", lambda e, TM=TM, SP=SP, cs=cs, bi=bi: e.tensor_tensor(
                                        TM[:, :], SP[:, cs], Bn[:, bi, :], ALU.add),
                                        reads=[SP.k, Bn.k], writes=[TM.k])
                                    fw.op("act", lambda e, P_=P_, TM=TM, cs=cs: e.activation(
                                        P_[:, cs], TM[:, :], AF.Exp), reads=[TM.k], writes=[P_.k])
                        for j in js:
                            qb = 4 * qg + j
                            OP = o_ps[j]
                            fw.op("pe", lambda e, OP=OP, P_=P_, j=j, kb=kb, qb=qb: e.matmul(
                                OP[:, 0:129], mm(P_[:, j * 128:(j + 1) * 128]), mm(va[:, kb, :]),
                                start=(kb == 0), stop=(kb == qb)),
                                reads=[P_.k, va.k], writes=[OP.k], pe_acc=(kb > 0))
                    OM = om[m]
                    for j in range(4):
                        if j % 2 == 0:
                            fw.op("act", lambda e, OM=OM, j=j: e.copy(OM[:, j, :], o_ps[j][:, 0:129]),
                                  reads=[o_ps[j].k], writes=[OM.k])
                        else:
                            fw.op("dve", lambda e, OM=OM, j=j: e.tensor_copy(OM[:, j, :], o_ps[j][:, 0:129]),
                                  reads=[o_ps[j].k], writes=[OM.k])
                fw.op("dve", lambda e: e.reciprocal(rr[:, :, 0:1], om[0][:, :, 128:129]),
                      reads=[om[0].k], writes=[rr.k])
                fw.op("dve", lambda e: e.reciprocal(rr[:, :, 1:2], om[1][:, :, 128:129]),
                      reads=[om[1].k, rr.k], writes=[rr.k])
                fw.op("pool", lambda e: e.memset(ssq[:, :], 0.0), writes=[ssq.k])
                for j in range(4):
                    fw.op("dve", lambda e, j=j: e.tensor_scalar(
                        t1[:, :], om[1][:, j, 0:128], rr[:, j, 1:2], lam[:, 0:1], ALU.mult, ALU.mult),
                        reads=[om[1].k, rr.k, lam.k], writes=[t1.k])
                    fw.op("dve", lambda e, j=j: e.scalar_tensor_tensor(
                        o[:, j, :], om[0][:, j, 0:128], rr[:, j, 0:1], t1[:, :], ALU.mult, ALU.subtract),
                        reads=[om[0].k, rr.k, t1.k], writes=[o.k])
                    fw.op("act", lambda e, j=j: e.activation(junk[:, :], o[:, j, :], AF.Square,
                                                             accum_out=ssq[:, j:j + 1]),
                          reads=[o.k], writes=[junk.k, ssq.k])
                rstd_from_ss(fw, rs, ssq, 128, 4, epsb)
                for j in range(4):
                    fw.op("dve", lambda e, j=j: e.scalar_tensor_tensor(
                        o[:, j, :], o[:, j, :], rs[:, j:j + 1], subw[:, :], ALU.mult, ALU.mult),
                        reads=[o.k, rs.k, subw.k], writes=[o.k])
                    fw.op("pe", lambda e, j=j: e.transpose(tr_ps[:, j, :], o[:, j, :], idn[:, :]),
                          reads=[o.k, idn.k], writes=[tr_ps.k])
                fw.op("act", lambda e, YD=YD: e.copy(YD[:, :, :], tr_ps[:, :, :]), reads=[tr_ps.k], writes=[YD.k])
                store(fw, "sp", YD, yTr[:, h, qg * NQ:(qg + 1) * NQ].rearrange("p (j q) -> p j q", j=4),
                      YD[:, :, :])


def _lay_w(W):
    n = W.shape[1]
    return np.ascontiguousarray(W.reshape(8, 128, n).transpose(1, 0, 2))


def _lay_g(g):
    return np.ascontiguousarray(g.reshape(8, 128).T)


def _lay_gu(W):
    return np.ascontiguousarray(W.reshape(8, 128, NFC, 128).transpose(2, 1, 0, 3))


def _lay_d(W):
    return np.ascontiguousarray(W.reshape(NFC, 128, 8, 128).transpose(2, 1, 0, 3))


def host_layout(inputs):
    f = lambda a: np.ascontiguousarray(np.asarray(a, dtype=np.float32))
    sh = {}
    for i in range(2):
        for j in range(2):
            sh[f"wg{i}{j}"] = _lay_gu(f(inputs["ffn_w_gate"][i, j]))
            sh[f"wu{i}{j}"] = _lay_gu(f(inputs["ffn_w_up"][i, j]))
            sh[f"wd{i}{j}"] = _lay_d(f(inputs["ffn_w_down"][i, j]))
            sh[f"fg{i}{j}"] = _lay_g(f(inputs["ffn_norm"][i, j]))
        sh[f"mg{i}"] = _lay_g(f(inputs["mix_norm"][i]))
    Mc, M3, maskA = gla_consts()
    cur, prev, cur0 = pool_consts()
    U, Tri, NEG = ssd_consts()
    sh["ev_w_in"] = _lay_w(f(inputs["ev_w_in"][0]))
    sh["Mc"], sh["M3"], sh["mA"] = Mc, M3, maskA
    sh["wgk1"] = f(np.concatenate([inputs["ev_w_gk_up"][0], inputs["ev_b_gk"][0][None]], 0))
    sh["gnorm"] = f(inputs["ev_gla_norm"][0][:, None])
    sh["wpool"] = f(np.asarray(inputs["ev_w_pool"][0]).transpose(1, 0, 2))
    sh["pscale"] = f(np.asarray(inputs["ev_pool_scale"][0]).reshape(4, 128).T)
    sh["cur"] = f(cur.transpose(1, 0, 2))
    sh["prev"] = f(prev.transpose(1, 0, 2))
    sh["cur0"] = f(cur0.transpose(1, 0, 2))
    sh["ev_w_out"] = _lay_w(f(inputs["ev_w_out"][0]))
    sh["od_w_in"] = _lay_w(f(inputs["od_w_in"][0]))
    sh["U"], sh["Tri"], sh["NEG"] = U, Tri, NEG
    sh["ident"] = np.eye(128, dtype=np.float32)
    sh["cw"] = f(np.asarray(inputs["od_conv_w"][0]).reshape(4, 8, 128).transpose(2, 1, 0))
    sh["cb"] = f(np.asarray(inputs["od_conv_b"][0]).reshape(8, 128).T)
    sh["dtb"] = f(inputs["od_dt_bias"])
    sh["alog"] = f(inputs["od_a_log"])
    sh["dsk"] = f(inputs["od_d_skip"])
    sh["wn"] = f(inputs["od_ssd_norm"])
    sh["wq"] = f(np.tile(np.asarray(inputs["od_q_norm"][0]), 2)[:, None])
    sh["wk"] = f(np.tile(np.asarray(inputs["od_k_norm"][0]), 2)[:, None])
    sh["bones"] = np.kron(np.eye(2), np.ones((64, 64))).astype(np.float32)
    sh["relb"] = f(inputs["rel_bias"])
    sh["Ecat"] = t5_consts()
    for nm in ("q1", "k1", "q2", "k2"):
        sh["l" + nm] = f(inputs["od_lambda_" + nm])
    sh["subln"] = f(inputs["od_subln"])
    sh["od_w_out"] = _lay_w(f(inputs["od_w_out"][0]))
    return sh


def build_program(shared_shapes):
    import math
    nc = bass.Bass("TRN2", target_bir_lowering=False)
    A = {}
    A["xin"] = nc.dram_tensor("xin", [D, T], F32, kind="ExternalInput").ap()
    for k, shp in shared_shapes.items():
        A[k] = nc.dram_tensor(k, list(shp), F32, kind="ExternalInput").ap()
    xout = nc.dram_tensor("xout", [D, T], F32, kind="ExternalOutput").ap()

    def Sx(name, shape):
        return nc.dram_tensor(name, list(shape), F32).ap()

    xa, xb = Sx("xa", [D, T]), Sx("xb", [D, T])
    yT = Sx("yT", [1024, T])
    qT, kT = Sx("qT", [512, T]), Sx("kT", [512, T])
    gT, lrT = Sx("gT", [512, T]), Sx("lrT", [16, T])
    k_tm, v_tm, u_tm = Sx("k_tm", [T, 256]), Sx("v_tm", [T, 512]), Sx("u_tm", [T, 512])
    xbcT, xcT, xB_tm = Sx("xbcT", [1024, T]), Sx("xcT", [1024, T]), Sx("xB_tm", [T, 768])
    qnT, knT = Sx("qnT", [512, T]), Sx("knT", [512, T])
    z_tm, dt_tm, BnT = Sx("z_tm", [T, 512]), Sx("dt_tm", [T, 8]), Sx("BnT", [4, 32768])
    with contextlib.ExitStack() as st:
        fw = FW(nc, st)
        ffn_phase(fw, A["xin"], xa, A["wg00"], A["wu00"], A["wd00"], A["fg00"])
        fm = [(0, 128, qT[0:128]), (128, 128, qT[128:256]), (256, 128, kT[0:128]), (384, 128, kT[128:256])]
        fm += [(1024 + i * 128, 128, gT[i * 128:(i + 1) * 128]) for i in range(4)]
        fm += [(1536, 16, lrT)]
        tm = [(256, 256, k_tm), (512, 512, v_tm), (1552, 512, u_tm)]
        inproj_phase(fw, xa, A["ev_w_in"], 2064, A["mg0"], fm, tm)
        gla_phase(fw, qT[0:256], kT[0:256], gT, lrT, k_tm, v_tm, yT[0:512], A["Mc"], A["M3"], A["mA"],
                  A["wgk1"], A["gnorm"])
        pool_phase(fw, u_tm, yT[512:1024], A["wpool"], A["pscale"], A["cur"], A["prev"], A["cur0"])
        outproj_phase(fw, xa, xb, yT, A["ev_w_out"])
        ffn_phase(fw, xb, xa, A["wg01"], A["wu01"], A["wd01"], A["fg01"])
        ffn_phase(fw, xa, xb, A["wg10"], A["wu10"], A["wd10"], A["fg10"])
        fm = [(512 + i * 128, 128, xbcT[i * 128:(i + 1) * 128]) for i in range(8)]
        fm += [(1544 + i * 128, 128, qT[i * 128:(i + 1) * 128]) for i in range(4)]
        fm += [(2056 + i * 128, 128, kT[i * 128:(i + 1) * 128]) for i in range(4)]
        tm = [(0, 512, z_tm), (1536, 8, dt_tm), (2568, 512, v_tm)]
        inproj_phase(fw, xb, A["od_w_in"], 3080, A["mg1"], fm, tm)
        conv_phase(fw, xbcT, xcT, xB_tm, A["cw"], A["cb"], A["ident"])
        ssd_phase(fw, xcT, xB_tm, z_tm, dt_tm, yT[0:512], A["U"], A["Tri"], A["NEG"], A["ident"],
                  A["dtb"], A["alog"], A["dsk"], A["wn"])
        qknorm_phase(fw, qT, kT, qnT, knT, A["wq"], A["wk"], A["bones"])
        bias_phase(fw, A["relb"], A["Ecat"], BnT)
        attn_phase(fw, qnT, knT, v_tm, BnT, yT[512:1024], A["ident"], A["lq1"], A["lk1"], A["lq2"],
                   A["lk2"], A["subln"], 0.8 - 0.6 * math.exp(-0.3 * 1))
        outproj_phase(fw, xb, xa, yT, A["od_w_out"])
        ffn_phase(fw, xa, xout, A["wg11"], A["wu11"], A["wd11"], A["fg11"])
    return nc


def kernel(**inputs):
    x = np.asarray(inputs["x"], dtype=np.float32)
    sh = host_layout(inputs)
    nc = build_program({k: v.shape for k, v in sh.items()})
    in_maps = []
    for b in range(8):
        m = dict(sh)
        m["xin"] = np.ascontiguousarray(x[b].T)
        in_maps.append(m)
    res = run_bass_kernel_spmd(nc, in_maps, core_ids=list(range(8)))
    out = np.stack([np.ascontiguousarray(res.results[b]["xout"].T) for b in range(8)], 0)
    return out.astype(np.float32)
```

```python
import contextlib
import numpy as np
import concourse.bass as bass
import concourse.mybir as mybir
from concourse.bass_utils import run_bass_kernel_spmd

F32 = mybir.dt.float32
F32R = mybir.dt.float32r
ALU = mybir.AluOpType
AF = mybir.ActivationFunctionType
AX = mybir.AxisListType

T = 4096
D = 1024
DFF = 2816
NFC = DFF // 128
EPS = 1e-6
MM_FAST = False


def mm(ap):
    return ap.bitcast(F32R) if MM_FAST else ap


class Trk:
    __slots__ = ("w", "r", "sem", "name")

    def __init__(self, name=""):
        self.w = None
        self.r = []
        self.sem = None
        self.name = name


class Op:
    __slots__ = ("eng", "fn", "deps", "flag", "sem", "val", "dma", "n")

    def __init__(self, eng, fn, dma=False, n=1):
        self.eng = eng
        self.fn = fn
        self.deps = []
        self.flag = False
        self.sem = None
        self.val = 0
        self.dma = dma
        self.n = n


EPOCH = 6000
ENGS = ("pe", "act", "dve", "pool", "sp")


class FW:
    def __init__(self, nc, stack, n_dma_sems=48):
        self.nc = nc
        self.stack = stack
        self.eng_obj = {"pe": nc.tensor, "act": nc.scalar, "dve": nc.vector,
                        "pool": nc.gpsimd, "sp": nc.sync}
        self.eng_sems = {e: [] for e in ENGS}
        self.eng_cnt = {e: 0 for e in ENGS}
        self.free_dma = []
        self.dma_cnt = {}
        self.n_sem = 0
        self.ops = []
        self.phase_dma_sems = set()
        self.waited = {e: {} for e in ENGS}
        self.phase_stack = None
        self.n_total = 0

    def new_sem(self, name):
        self.n_sem += 1
        return self.stack.enter_context(self.nc.semaphore(f"{name}_{self.n_sem}"))

    def sb(self, name, shape, dtype=F32):
        self.n_sem += 1
        name = f"{name}_{self.n_sem}"
        t = self.phase_stack.enter_context(self.nc.sbuf_tensor(name, list(shape), dtype))
        return t

    def ps(self, name, shape, dtype=F32):
        self.n_sem += 1
        name = f"{name}_{self.n_sem}"
        t = self.phase_stack.enter_context(self.nc.psum_tensor(name, list(shape), dtype))
        return t

    def _dep(self, op, reads, writes):
        deps = op.deps
        for t in reads:
            if t.w is not None:
                deps.append(t.w)
        for t in writes:
            if t.w is not None:
                deps.append(t.w)
            deps.extend(t.r)
        for t in reads:
            if not op.dma:
                t.r = [x for x in t.r if x.dma or x.eng != op.eng]
            t.r.append(op)
        for t in writes:
            t.w = op
            t.r = []

    def op(self, eng, fn, reads=(), writes=(), pe_acc=False):
        o = Op(eng, fn)
        self._dep(o, reads, writes)
        if pe_acc:
            o.deps = [d for d in o.deps if d.eng != "pe" or d.dma]
        self.ops.append(o)
        return o

    def dma(self, q, fns, sbuf_trk, reads=(), writes=()):
        if not isinstance(fns, (list, tuple)):
            fns = [fns]
        o = Op(q, fns, dma=True, n=len(fns))
        self._dep(o, reads, writes)
        if sbuf_trk.sem is None:
            if not self.free_dma:
                s = self.new_sem("dq")
                self.dma_cnt[s] = 0
                self.free_dma.append(s)
            sbuf_trk.sem = self.free_dma.pop()
            self.phase_dma_sems.add(sbuf_trk.sem)
        o.sem = sbuf_trk.sem
        self.dma_cnt[o.sem] += 16 * len(fns)
        o.val = self.dma_cnt[o.sem]
        self.ops.append(o)
        return o

    def emit(self):
        nc = self.nc
        ops = self.ops
        for o in ops:
            for d in o.deps:
                if not d.dma:
                    d.flag = True
        last = {}
        for o in ops:
            if not o.dma:
                last[o.eng] = o
        for o in last.values():
            o.flag = True
        for o in ops:
            if o.dma or not o.flag:
                continue
            c = self.eng_cnt[o.eng]
            ep = c // EPOCH
            sems = self.eng_sems[o.eng]
            while len(sems) <= ep:
                sems.append(self.new_sem("e" + o.eng))
            o.sem = sems[ep]
            o.val = c % EPOCH + 1
            self.eng_cnt[o.eng] = c + 1
        targets = []
        for e, o in last.items():
            targets.append((o.sem, o.val))
        for s in self.phase_dma_sems:
            targets.append((s, self.dma_cnt[s]))
        by_eng = {e: [] for e in ENGS}
        for o in ops:
            by_eng[o.eng].append(o)
        waited = self.waited

        def run(ename):
            def body(eng):
                wd = waited[ename]
                for o in by_eng[ename]:
                    for d in o.deps:
                        if d.sem is None:
                            continue
                        if wd.get(d.sem, 0) >= d.val:
                            continue
                        if d.eng == ename and not d.dma and ename == "pe":
                            pass
                        eng.wait_ge(d.sem, d.val)
                        wd[d.sem] = d.val
                    if o.dma:
                        for f in o.fn:
                            f(eng).then_inc(o.sem, 16)
                    else:
                        ins = o.fn(eng)
                        if o.flag:
                            ins.then_inc(o.sem, 1)
                for (s, v) in targets:
                    if wd.get(s, 0) < v:
                        eng.wait_ge(s, v)
                        wd[s] = v
            return body

        with nc.Block() as block:
            block.sync(run("sp"))
            block.scalar(run("act"))
            block.vector(run("dve"))
            block.gpsimd(run("pool"))
            block.tensor(run("pe"))
        self.n_total += len(ops)
        for s in self.phase_dma_sems:
            if self.dma_cnt[s] < 24000:
                self.free_dma.append(s)
        self.phase_dma_sems = set()
        self.ops = []

    @contextlib.contextmanager
    def phase(self):
        with contextlib.ExitStack() as st:
            self.phase_stack = st
            yield
            self.emit()
        self.phase_stack = None


class Buf:
    def __init__(self, fw, name, shape, dtype=F32, psum=False):
        self.t = fw.ps(name, shape, dtype) if psum else fw.sb(name, shape, dtype)
        self.k = Trk(name)

    def __getitem__(self, idx):
        return self.t[idx]


def rstd_from_ss(fw, out_buf, ss_ps, n, width, epsb):
    fw.op("act", lambda e: e.activation(out_buf[:, :width], ss_ps[:, :width], AF.Sqrt,
                                        bias=epsb[:, 0:1], scale=1.0 / n),
          reads=[ss_ps.k, epsb.k], writes=[out_buf.k])
    fw.op("dve", lambda e: e.reciprocal(out_buf[:, :width], out_buf[:, :width]),
          reads=[out_buf.k], writes=[out_buf.k])


def ffn_phase(fw, xT_in, xT_out, wg, wu, wd, gnorm, NT=512):
    with fw.phase():
        ones = Buf(fw, "ones", [128, 128])
        g_sb = Buf(fw, "g_sb", [128, 8])
        xt = [Buf(fw, f"xt{i}", [128, 8, NT]) for i in range(2)]
        xn = Buf(fw, "xn", [128, 8, NT])
        sq = [Buf(fw, f"sq{i}", [128, NT]) for i in range(2)]
        rstd = Buf(fw, "rstd", [128, NT])
        hid = Buf(fw, "hid", [128, NFC, NT])
        sg = [Buf(fw, f"sg{i}", [128, NT]) for i in range(2)]
        wgb = [Buf(fw, f"wgb{i}", [128, 8, 128]) for i in range(3)]
        wub = [Buf(fw, f"wub{i}", [128, 8, 128]) for i in range(3)]
        wdb = [Buf(fw, f"wdb{i}", [128, NFC, 128]) for i in range(2)]
        xo = [Buf(fw, f"xo{i}", [128, NT]) for i in range(2)]
        ss_ps = Buf(fw, "ss_ps", [128, NT], psum=True)
        gp = [Buf(fw, f"gp{i}", [128, NT], psum=True) for i in range(2)]
        up = [Buf(fw, f"up{i}", [128, NT], psum=True) for i in range(2)]
        op_ = [Buf(fw, f"op{i}", [128, NT], psum=True) for i in range(2)]

        fw.op("pool", lambda e: e.memset(ones[:, :], 1.0), writes=[ones.k])
        epsb = Buf(fw, "epsb", [128, 1])
        fw.op("pool", lambda e: e.memset(epsb[:, :], EPS), writes=[epsb.k])
        fw.dma("sp", lambda e: e.dma_start(out=g_sb[:, :], in_=gnorm), g_sb.k, writes=[g_sb.k])
        xin = xT_in.rearrange("(c p) t -> p c t", p=128)
        xout = xT_out.rearrange("(c p) t -> p c t", p=128)
        ntiles = T // NT
        wi = 0
        di = 0
        for it in range(ntiles):
            X = xt[it % 2]
            ts = slice(it * NT, (it + 1) * NT)
            fw.dma("sp", lambda e, X=X, ts=ts: e.dma_start(out=X[:, :, :], in_=xin[:, :, ts]),
                   X.k, writes=[X.k])
            for c in range(8):
                S = sq[c % 2]
                fw.op("act", lambda e, S=S, X=X, c=c: e.activation(S[:, :], X[:, c, :], AF.Square),
                      reads=[X.k], writes=[S.k])
                fw.op("pe", lambda e, S=S, c=c: e.matmul(ss_ps[:, :], ones[:, :], S[:, :],
                                                          start=(c == 0), stop=(c == 7)),
                      reads=[ones.k, S.k], writes=[ss_ps.k], pe_acc=(c > 0))
            rstd_from_ss(fw, rstd, ss_ps, D, NT, epsb)
            for c in range(8):
                fw.op("dve", lambda e, X=X, c=c: e.scalar_tensor_tensor(
                    xn[:, c, :], X[:, c, :], g_sb[:, c:c + 1], rstd[:, :], ALU.mult, ALU.mult),
                    reads=[X.k, g_sb.k, rstd.k], writes=[xn.k])
            for f in range(NFC):
                WG = wgb[wi % 3]
                WU = wub[wi % 3]
                wi += 1
                fw.dma("sp", lambda e, WG=WG, f=f: e.dma_start(out=WG[:, :, :], in_=wg[f]),
                       WG.k, writes=[WG.k])
                fw.dma("pool", lambda e, WU=WU, f=f: e.dma_start(out=WU[:, :, :], in_=wu[f]),
                       WU.k, writes=[WU.k])
                G = gp[f % 2]
                U = up[f % 2]
                for c in range(8):
                    fw.op("pe", lambda e, W=WG, G=G, c=c: e.matmul(
                        G[:, :], mm(W[:, c, :]), mm(xn[:, c, :]), start=(c == 0), stop=(c == 7)),
                        reads=[WG.k, xn.k], writes=[G.k], pe_acc=(c > 0))
                for c in range(8):
                    fw.op("pe", lambda e, W=WU, U=U, c=c: e.matmul(
                        U[:, :], mm(W[:, c, :]), mm(xn[:, c, :]), start=(c == 0), stop=(c == 7)),
                        reads=[WU.k, xn.k], writes=[U.k], pe_acc=(c > 0))
                SG = sg[f % 2]
                fw.op("act", lambda e, SG=SG, G=G: e.activation(SG[:, :], G[:, :], AF.Silu),
                      reads=[G.k], writes=[SG.k])
                fw.op("dve", lambda e, SG=SG, U=U, f=f: e.tensor_tensor(
                    hid[:, f, :], SG[:, :], U[:, :], ALU.mult),
                    reads=[SG.k, U.k], writes=[hid.k])
            for dc in range(8):
                W = wdb[di % 2]
                di += 1
                fw.dma("sp" if dc % 2 == 0 else "pool",
                       lambda e, W=W, dc=dc: e.dma_start(out=W[:, :, :], in_=wd[dc]),
                       W.k, writes=[W.k])
                O = op_[dc % 2]
                for f in range(NFC):
                    fw.op("pe", lambda e, W=W, O=O, f=f: e.matmul(
                        O[:, :], mm(W[:, f, :]), mm(hid[:, f, :]), start=(f == 0), stop=(f == NFC - 1)),
                        reads=[W.k, hid.k], writes=[O.k], pe_acc=(f > 0))
                XO = xo[dc % 2]
                fw.op("dve", lambda e, XO=XO, O=O, X=X, dc=dc: e.scalar_tensor_tensor(
                    XO[:, :], O[:, :], 0.5, X[:, dc, :], ALU.mult, ALU.add),
                    reads=[O.k, X.k], writes=[XO.k])
                fw.dma("sp", lambda e, XO=XO, dc=dc, ts=ts: e.dma_start(out=xout[:, dc, ts], in_=XO[:, :]),
                       XO.k, reads=[XO.k])


def mm_group(fw, O, out_ap, pairs, reads):
    n = len(pairs)
    for i, (l, r) in enumerate(pairs):
        fw.op("pe", lambda e, l=l, r=r, i=i: e.matmul(out_ap, mm(l), mm(r), start=(i == 0),
                                                       stop=(i == n - 1)),
              reads=reads, writes=[O.k], pe_acc=(i > 0))


def load(fw, q, B, out_ap, in_ap):
    fw.dma(q, lambda e: e.dma_start(out=out_ap, in_=in_ap), B.k, writes=[B.k])


def store(fw, q, B, out_ap, in_ap):
    fw.dma(q, lambda e: e.dma_start(out=out_ap, in_=in_ap), B.k, reads=[B.k])


def norm_tile(fw, X, hT, sq, ss_ps, rstd, ones, g_sb, epsb, NT):
    for c in range(8):
        S = sq[c % 2]
        fw.op("act", lambda e, S=S, c=c: e.activation(S[:, :], X[:, c, :], AF.Square),
              reads=[X.k], writes=[S.k])
        fw.op("pe", lambda e, S=S, c=c: e.matmul(ss_ps[:, :NT], ones[:, :], S[:, :],
                                                  start=(c == 0), stop=(c == 7)),
              reads=[ones.k, S.k], writes=[ss_ps.k], pe_acc=(c > 0))
    rstd_from_ss(fw, rstd, ss_ps, D, NT, epsb)
    for c in range(8):
        fw.op("dve", lambda e, c=c: e.scalar_tensor_tensor(
            hT[:, c, :], X[:, c, :], g_sb[:, c:c + 1], rstd[:, :NT], ALU.mult, ALU.mult),
            reads=[X.k, g_sb.k, rstd.k], writes=[hT.k])


def inproj_phase(fw, xT_in, w_in, ncols, gnorm, fm_groups, tm_groups, NT=512):
    with fw.phase():
        ones = Buf(fw, "ones", [128, 128])
        epsb = Buf(fw, "epsb", [128, 1])
        g_sb = Buf(fw, "g_sb", [128, 8])
        W = Buf(fw, "W", [128, 8, ncols])
        xt = [Buf(fw, f"xt{i}", [128, 8, NT]) for i in range(2)]
        hT = Buf(fw, "hT", [128, 8, NT])
        sq = [Buf(fw, f"sq{i}", [128, NT]) for i in range(2)]
        rstd = Buf(fw, "rstd", [128, NT])
        stf = [Buf(fw, f"stf{i}", [128, NT]) for i in range(3)]
        stt = [Buf(fw, f"stt{i}", [128, 512]) for i in range(3)]
        ss_ps = Buf(fw, "ss_ps", [128, NT], psum=True)
        fp = [Buf(fw, f"fp{i}", [128, NT], psum=True) for i in range(3)]
        tp = [Buf(fw, f"tp{i}", [128, 512], psum=True) for i in range(3)]
        fw.op("pool", lambda e: e.memset(ones[:, :], 1.0), writes=[ones.k])
        fw.op("pool", lambda e: e.memset(epsb[:, :], EPS), writes=[epsb.k])
        load(fw, "sp", g_sb, g_sb[:, :], gnorm)
        fw.dma("pool", [lambda e, c=c: e.dma_start(out=W[:, c, :], in_=w_in[:, c, :]) for c in range(8)],
               W.k, writes=[W.k])
        xin = xT_in.rearrange("(c p) t -> p c t", p=128)
        k = 0
        for it in range(T // NT):
            X = xt[it % 2]
            ts = slice(it * NT, (it + 1) * NT)
            load(fw, "sp", X, X[:, :, :], xin[:, :, ts])
            norm_tile(fw, X, hT, sq, ss_ps, rstd, ones, g_sb, epsb, NT)
            for (c0, wd_, dst) in fm_groups:
                P_ = fp[k % 3]
                S_ = stf[k % 3]
                mm_group(fw, P_, P_[:wd_, :], [(W[:, c, c0:c0 + wd_], hT[:, c, :]) for c in range(8)],
                         [W.k, hT.k])
                eng = "act" if k % 2 == 0 else "dve"
                if eng == "act":
                    fw.op("act", lambda e, P_=P_, S_=S_, wd_=wd_: e.copy(S_[:wd_, :], P_[:wd_, :]),
                          reads=[P_.k], writes=[S_.k])
                else:
                    fw.op("dve", lambda e, P_=P_, S_=S_, wd_=wd_: e.tensor_copy(S_[:wd_, :], P_[:wd_, :]),
                          reads=[P_.k], writes=[S_.k])
                store(fw, "sp", S_, dst[:, ts], S_[:wd_, :])
                k += 1
            for sub in range(NT // 128):
                t0 = it * NT + sub * 128
                for (c0, wd_, dst) in tm_groups:
                    P_ = tp[k % 3]
                    S_ = stt[k % 3]
                    mm_group(fw, P_, P_[:, :wd_],
                             [(hT[:, c, sub * 128:(sub + 1) * 128], W[:, c, c0:c0 + wd_]) for c in range(8)],
                             [W.k, hT.k])
                    if k % 2 == 0:
                        fw.op("act", lambda e, P_=P_, S_=S_, wd_=wd_: e.copy(S_[:, :wd_], P_[:, :wd_]),
                              reads=[P_.k], writes=[S_.k])
                    else:
                        fw.op("dve", lambda e, P_=P_, S_=S_, wd_=wd_: e.tensor_copy(S_[:, :wd_], P_[:, :wd_]),
                              reads=[P_.k], writes=[S_.k])
                    store(fw, "pool", S_, dst[t0:t0 + 128, :], S_[:, :wd_])
                    k += 1


def outproj_phase(fw, xT_in, xT_out, yT, w_out, NT=512):
    with fw.phase():
        W = Buf(fw, "W", [128, 8, 1024])
        xt = [Buf(fw, f"xt{i}", [128, 8, NT]) for i in range(2)]
        yt = [Buf(fw, f"yt{i}", [128, 8, NT]) for i in range(2)]
        xo = [Buf(fw, f"xo{i}", [128, 8, NT]) for i in range(2)]
        op_ = [Buf(fw, f"op{i}", [128, NT], psum=True) for i in range(3)]
        fw.dma("pool", [lambda e, c=c: e.dma_start(out=W[:, c, :], in_=w_out[:, c, :]) for c in range(8)],
               W.k, writes=[W.k])
        xin = xT_in.rearrange("(c p) t -> p c t", p=128)
        yin = yT.rearrange("(c p) t -> p c t", p=128)
        xout = xT_out.rearrange("(c p) t -> p c t", p=128)
        k = 0
        for it in range(T // NT):
            X = xt[it % 2]
            Y = yt[it % 2]
            XO = xo[it % 2]
            ts = slice(it * NT, (it + 1) * NT)
            load(fw, "sp", X, X[:, :, :], xin[:, :, ts])
            load(fw, "pool", Y, Y[:, :, :], yin[:, :, ts])
            for dc in range(8):
                O = op_[k % 3]
                k += 1
                mm_group(fw, O, O[:, :], [(W[:, c, dc * 128:(dc + 1) * 128], Y[:, c, :]) for c in range(8)],
                         [W.k, Y.k])
                fw.op("dve", lambda e, O=O, X=X, XO=XO, dc=dc: e.tensor_tensor(
                    XO[:, dc, :], O[:, :], X[:, dc, :], ALU.add),
                    reads=[O.k, X.k], writes=[XO.k])
            store(fw, "sp", XO, xout[:, :, ts], XO[:, :, :])


def gla_consts():
    s = np.arange(128)[:, None]
    t = np.arange(128)[None, :]
    same = (s // 64) == (t // 64)
    Mc = np.zeros((128, 130), np.float32)
    Mc[:, :128] = np.where(same & (s <= t), -1.0 / 16, 0.0)
    Mc[:64, 128] = -1.0 / 16
    Mc[64:, 129] = -1.0 / 16
    M3 = np.where(same & (s > t), -1.0 / 16, 0.0).astype(np.float32)
    maskA = np.where(same & (s <= t), 1.0, 0.0).astype(np.float32)
    return Mc, M3, maskA


def gla_phase(fw, qT, kT, gT, lrT, k_tm, v_tm, yT, Mc_d, M3_d, maskA_d, wgk1_d, gnorm_d):
    with fw.phase():
        ones = Buf(fw, "ones", [128, 128])
        epsb = Buf(fw, "epsb", [128, 1])
        Mc = Buf(fw, "Mc", [128, 130])
        M3 = Buf(fw, "M3", [128, 128])
        mA = Buf(fw, "mA", [128, 128])
        wgk = Buf(fw, "wgk", [32, 256])
        wn = Buf(fw, "wn", [128, 1])
        qt = [Buf(fw, f"qt{i}", [128, 2, 128]) for i in range(2)]
        kt = [Buf(fw, f"kt{i}", [128, 2, 128]) for i in range(2)]
        gt = [Buf(fw, f"gt{i}", [128, 4, 128]) for i in range(2)]
        lr = [Buf(fw, f"lr{i}", [32, 128]) for i in range(2)]
        ktm = [Buf(fw, f"ktm{i}", [128, 256]) for i in range(2)]
        vtm = [Buf(fw, f"vtm{i}", [128, 512]) for i in range(2)]
        e1 = Buf(fw, "e1", [128, 256])
        sp_ = Buf(fw, "sp_", [128, 256])
        eG = Buf(fw, "eG", [128, 2, 130])
        enG = Buf(fw, "enG", [128, 2, 128])
        eD = Buf(fw, "eD", [128, 256])
        qd = Buf(fw, "qd", [128, 2, 128])
        kd = Buf(fw, "kd", [128, 2, 128])
        kk = Buf(fw, "kk", [128, 256])
        ATm = Buf(fw, "ATm", [128, 4, 128])
        oxs = Buf(fw, "oxs", [128, 4, 128])
        oT = Buf(fw, "oT", [128, 4, 128])
        sqo = Buf(fw, "sqo", [128, 4, 128])
        rstd = Buf(fw, "rstd", [128, 512])
        sg = Buf(fw, "sg", [128, 4, 128])
        ya = [Buf(fw, f"ya{i}", [128, 4, 128]) for i in range(2)]
        S = [Buf(fw, f"S{i}", [128, 2, 128]) for i in range(2)]
        zd_ps = Buf(fw, "zd_ps", [128, 512], psum=True)
        d_ps = Buf(fw, "d_ps", [128, 512], psum=True)
        gt_ps = Buf(fw, "gt_ps", [128, 2, 256], psum=True)
        at_ps = Buf(fw, "at_ps", [128, 4, 128], psum=True)
        oi_ps = Buf(fw, "oi_ps", [128, 4, 128], psum=True)
        ox_ps = Buf(fw, "ox_ps", [128, 4, 128], psum=True)
        st_ps = [Buf(fw, f"st_ps{i}", [128, 2, 256], psum=True) for i in range(2)]
        fw.op("pool", lambda e: e.memset(ones[:, :], 1.0), writes=[ones.k])
        fw.op("pool", lambda e: e.memset(epsb[:, :], EPS), writes=[epsb.k])
        for i in range(2):
            fw.op("pool", lambda e, i=i: e.memset(lr[i][:, :], 1.0), writes=[lr[i].k])
            fw.op("pool", lambda e, i=i: e.memset(S[i][:, :, :], 0.0), writes=[S[i].k])
        load(fw, "sp", Mc, Mc[:, :], Mc_d)
        load(fw, "sp", M3, M3[:, :], M3_d)
        load(fw, "sp", mA, mA[:, :], maskA_d)
        load(fw, "sp", wgk, wgk[0:17, :], wgk1_d)
        load(fw, "sp", wn, wn[:, :], gnorm_d)
        qTr = qT.rearrange("(c p) t -> p c t", p=128)
        kTr = kT.rearrange("(c p) t -> p c t", p=128)
        gTr = gT.rearrange("(c p) t -> p c t", p=128)
        yTr = yT.rearrange("(c p) t -> p c t", p=128)
        for it in range(T // 128):
            ts = slice(it * 128, (it + 1) * 128)
            b = it % 2
            Q, K_, G_, L, KT, VT = qt[b], kt[b], gt[b], lr[b], ktm[b], vtm[b]
            load(fw, "sp", Q, Q[:, :, :], qTr[:, :, ts])
            load(fw, "sp", K_, K_[:, :, :], kTr[:, :, ts])
            load(fw, "sp", G_, G_[:, :, :], gTr[:, :, ts])
            load(fw, "pool", L, L[0:16, :], lrT[:, ts])
            load(fw, "pool", KT, KT[:, :], k_tm[ts, :])
            load(fw, "pool", VT, VT[:, :], v_tm[ts, :])
            mm_group(fw, zd_ps, zd_ps[:, 0:256], [(L[0:17, :], wgk[0:17, :])], [L.k, wgk.k])
            fw.op("act", lambda e: e.activation(e1[:, :], zd_ps[:, 0:256], AF.Exp, scale=-1.0),
                  reads=[zd_ps.k], writes=[e1.k])
            fw.op("act", lambda e: e.activation(sp_[:, :], e1[:, :], AF.Ln, bias=1.0),
                  reads=[e1.k], writes=[sp_.k])
            for c in range(2):
                mm_group(fw, gt_ps, gt_ps[:, c, 0:130], [(sp_[:, c * 128:(c + 1) * 128], Mc[:, :])],
                         [sp_.k, Mc.k])
            mm_group(fw, d_ps, d_ps[:, 0:256], [(M3[:, :], sp_[:, :])], [M3.k, sp_.k])
            fw.op("act", lambda e: e.activation(eG[:, :, :], gt_ps[:, :, 0:130], AF.Exp),
                  reads=[gt_ps.k], writes=[eG.k])
            fw.op("act", lambda e: e.activation(enG[:, :, :], gt_ps[:, :, 0:128], AF.Exp, scale=-1.0),
                  reads=[gt_ps.k], writes=[enG.k])
            fw.op("act", lambda e: e.activation(eD[:, :], d_ps[:, 0:256], AF.Exp),
                  reads=[d_ps.k], writes=[eD.k])
            fw.op("dve", lambda e, Q=Q: e.scalar_tensor_tensor(
                qd[:, :, :], Q[:, :, :], 0.125, eG[:, :, 0:128], ALU.mult, ALU.mult),
                reads=[Q.k, eG.k], writes=[qd.k])
            fw.op("dve", lambda e, K_=K_: e.tensor_tensor(kd[:, :, :], K_[:, :, :], enG[:, :, :], ALU.mult),
                  reads=[K_.k, enG.k], writes=[kd.k])
            fw.op("dve", lambda e, KT=KT: e.tensor_tensor(kk[:, :], KT[:, :], eD[:, :], ALU.mult),
                  reads=[KT.k, eD.k], writes=[kk.k])
            for h in range(4):
                c, pb = h // 2, (h % 2) * 64
                mm_group(fw, at_ps, at_ps[:, h, :], [(kd[pb:pb + 64, c, :], qd[pb:pb + 64, c, :])],
                         [kd.k, qd.k])
            fw.op("dve", lambda e: e.tensor_tensor(
                ATm[:, :, :], at_ps[:, :, :], mA[:, :].unsqueeze(1).to_broadcast([128, 4, 128]), ALU.mult),
                reads=[at_ps.k, mA.k], writes=[ATm.k])
            for h in range(4):
                mm_group(fw, oi_ps, oi_ps[:, h, :], [(VT[:, h * 128:(h + 1) * 128], ATm[:, h, :])],
                         [VT.k, ATm.k])
            for cc in range(2):
                Sc, Sn = S[cc], S[1 - cc]
                for h in range(4):
                    c, pb = h // 2, (h % 2) * 64
                    mm_group(fw, ox_ps, ox_ps[:, h, cc * 64:(cc + 1) * 64],
                             [(Sc[pb:pb + 64, c, :], qd[pb:pb + 64, c, cc * 64:(cc + 1) * 64])],
                             [Sc.k, qd.k])
                STP = st_ps[cc]
                for c in range(2):
                    mm_group(fw, STP, STP[:, c, :],
                             [(kk[cc * 64:(cc + 1) * 64, c * 128:(c + 1) * 128],
                               VT[cc * 64:(cc + 1) * 64, c * 256:(c + 1) * 256])], [kk.k, VT.k])
                for h in range(4):
                    c, pb = h // 2, (h % 2) * 64
                    fw.op("dve", lambda e, Sc=Sc, Sn=Sn, STP=STP, c=c, pb=pb, h=h, cc=cc:
                          e.scalar_tensor_tensor(
                              Sn[pb:pb + 64, c, :], Sc[pb:pb + 64, c, :], eG[pb:pb + 64, c, 128 + cc:129 + cc],
                              STP[pb:pb + 64, c, (h % 2) * 128:(h % 2) * 128 + 128], ALU.mult, ALU.add),
                          reads=[Sc.k, eG.k, STP.k], writes=[Sn.k])
            fw.op("act", lambda e: e.copy(oxs[:, :, :], ox_ps[:, :, :]), reads=[ox_ps.k], writes=[oxs.k])
            fw.op("dve", lambda e: e.tensor_tensor(oT[:, :, :], oi_ps[:, :, :], oxs[:, :, :], ALU.add),
                  reads=[oi_ps.k, oxs.k], writes=[oT.k])
            fw.op("act", lambda e: e.activation(sqo[:, :, :], oT[:, :, :], AF.Square),
                  reads=[oT.k], writes=[sqo.k])
            mm_group(fw, zd_ps, zd_ps[:, :], [(ones[:, :], sqo[:, :, :].rearrange("p h t -> p (h t)"))],
                     [ones.k, sqo.k])
            rstd_from_ss(fw, rstd, zd_ps, 128, 512, epsb)
            fw.op("act", lambda e, G_=G_: e.activation(sg[:, :, :], G_[:, :, :], AF.Silu),
                  reads=[G_.k], writes=[sg.k])
            YA = ya[b]
            fw.op("dve", lambda e, YA=YA: e.scalar_tensor_tensor(
                YA[:, :, :].rearrange("p h t -> p (h t)"), oT[:, :, :].rearrange("p h t -> p (h t)"),
                wn[:, 0:1], rstd[:, :], ALU.mult, ALU.mult),
                reads=[oT.k, wn.k, rstd.k], writes=[YA.k])
            fw.op("dve", lambda e, YA=YA: e.tensor_tensor(YA[:, :, :], YA[:, :, :], sg[:, :, :], ALU.mult),
                  reads=[YA.k, sg.k], writes=[YA.k])
            store(fw, "sp", YA, yTr[:, :, ts], YA[:, :, :])


def pool_consts():
    s = np.arange(128)[:, None]
    t = np.arange(128)[None, :]
    cur = np.zeros((4, 128, 128), np.float32)
    prev = np.zeros((4, 128, 128), np.float32)
    cur0 = np.zeros((4, 128, 128), np.float32)
    for g, w in enumerate((2, 4, 8, 16)):
        d = t - s
        cur[g] = np.where((d >= 0) & (d < w), 1.0 / w, 0.0) - np.eye(128)
        cnt = np.minimum(t + 1, w).astype(np.float64)
        cur0[g] = np.where((d >= 0) & (d < w), 1.0 / cnt, 0.0) - np.eye(128)
        d2 = t + 128 - s
        prev[g] = np.where((d2 >= 0) & (d2 < w), 1.0 / w, 0.0)
    return cur.astype(np.float32), prev.astype(np.float32), cur0.astype(np.float32)


def pool_phase(fw, u_tm, yT, wpool_d, pscale_d, cur_d, prev_d, cur0_d):
    with fw.phase():
        wp = Buf(fw, "wp", [128, 4, 128])
        psc = Buf(fw, "psc", [128, 4])
        Pc = Buf(fw, "Pc", [128, 4, 128])
        Pp = Buf(fw, "Pp", [128, 4, 128])
        P0 = Buf(fw, "P0", [128, 4, 128])
        ut = [Buf(fw, f"ut{i}", [128, 512]) for i in range(3)]
        pl = Buf(fw, "pl", [128, 4, 128])
        yb = [Buf(fw, f"yb{i}", [128, 4, 128]) for i in range(2)]
        pt_ps = [Buf(fw, f"pt_ps{i}", [128, 4, 128], psum=True) for i in range(2)]
        y_ps = [Buf(fw, f"y_ps{i}", [128, 4, 128], psum=True) for i in range(2)]
        load(fw, "sp", wp, wp[:, :, :], wpool_d)
        load(fw, "sp", psc, psc[:, :], pscale_d)
        load(fw, "sp", Pc, Pc[:, :, :], cur_d)
        load(fw, "sp", Pp, Pp[:, :, :], prev_d)
        load(fw, "sp", P0, P0[:, :, :], cur0_d)
        yTr = yT.rearrange("(c p) t -> p c t", p=128)
        for it in range(T // 128):
            ts = slice(it * 128, (it + 1) * 128)
            U = ut[it % 3]
            Uprev = ut[(it - 1) % 3]
            load(fw, "sp", U, U[:, :], u_tm[ts, :])
            PT = pt_ps[it % 2]
            for g in range(4):
                gs = slice(g * 128, (g + 1) * 128)
                if it == 0:
                    mm_group(fw, PT, PT[:, g, :], [(U[:, gs], P0[:, g, :])], [U.k, P0.k])
                else:
                    mm_group(fw, PT, PT[:, g, :], [(U[:, gs], Pc[:, g, :]), (Uprev[:, gs], Pp[:, g, :])],
                             [U.k, Uprev.k, Pc.k, Pp.k])
            fw.op("act", lambda e, PT=PT: e.copy(pl[:, :, :], PT[:, :, :]), reads=[PT.k], writes=[pl.k])
            YP = y_ps[it % 2]
            for g in range(4):
                mm_group(fw, YP, YP[:, g, :], [(wp[:, g, :], pl[:, g, :])], [wp.k, pl.k])
            YB = yb[it % 2]
            fw.op("dve", lambda e, YB=YB, YP=YP: e.tensor_tensor(
                YB[:, :, :], YP[:, :, :], psc[:, :].unsqueeze(2).to_broadcast([128, 4, 128]), ALU.mult),
                reads=[YP.k, psc.k], writes=[YB.k])
            store(fw, "pool", YB, yTr[:, :, ts], YB[:, :, :])


def conv_phase(fw, xbcT, xcT, xB_tm, cw_d, cb_d, ident_d, NT=512):
    with fw.phase():
        cw = Buf(fw, "cw", [128, 8, 4])
        cb = Buf(fw, "cb", [128, 8])
        idn = Buf(fw, "idn", [128, 128])
        xt = [Buf(fw, f"xt{i}", [128, NT + 3]) for i in range(3)]
        acc = [Buf(fw, f"acc{i}", [128, NT]) for i in range(2)]
        xc = [Buf(fw, f"xc{i}", [128, NT]) for i in range(3)]
        s2 = [Buf(fw, f"s2{i}", [128, 4, 128]) for i in range(3)]
        tr_ps = [Buf(fw, f"tr_ps{i}", [128, 4, 128], psum=True) for i in range(2)]
        load(fw, "sp", cw, cw[:, :, :], cw_d)
        load(fw, "sp", cb, cb[:, :], cb_d)
        load(fw, "sp", idn, idn[:, :], ident_d)
        k = 0
        for it in range(T // NT):
            t0 = it * NT
            for c in range(8):
                X = xt[k % 3]
                A = acc[k % 2]
                XC = xc[k % 3]
                rows = slice(c * 128, (c + 1) * 128)
                if it == 0:
                    fw.op("pool", lambda e, X=X: e.memset(X[:, 0:3], 0.0), writes=[X.k])
                    load(fw, "sp", X, X[:, 3:NT + 3], xbcT[rows, 0:NT])
                else:
                    load(fw, "sp", X, X[:, :], xbcT[rows, t0 - 3:t0 + NT])
                fw.op("dve", lambda e, X=X, A=A, c=c: e.tensor_scalar(
                    A[:, :], X[:, 0:NT], cw[:, c, 0:1], None, ALU.mult), reads=[X.k, cw.k], writes=[A.k])
                for j in range(1, 4):
                    fw.op("dve", lambda e, X=X, A=A, c=c, j=j: e.scalar_tensor_tensor(
                        A[:, :], X[:, j:j + NT], cw[:, c, j:j + 1], A[:, :], ALU.mult, ALU.add),
                        reads=[X.k, cw.k, A.k], writes=[A.k])
                fw.op("act", lambda e, A=A, XC=XC, c=c: e.activation(
                    XC[:, :], A[:, :], AF.Silu, bias=cb[:, c:c + 1]), reads=[A.k, cb.k], writes=[XC.k])
                store(fw, "pool", XC, xcT[rows, t0:t0 + NT], XC[:, :])
                if c < 6:
                    TP = tr_ps[k % 2]
                    for sub in range(4):
                        fw.op("pe", lambda e, TP=TP, XC=XC, sub=sub: e.transpose(
                            TP[:, sub, :], XC[:, sub * 128:(sub + 1) * 128], idn[:, :]),
                            reads=[XC.k, idn.k], writes=[TP.k])
                    S2 = s2[k % 3]
                    fw.op("act", lambda e, TP=TP, S2=S2: e.copy(S2[:, :, :], TP[:, :, :]),
                          reads=[TP.k], writes=[S2.k])
                    store(fw, "sp", S2, xB_tm[t0:t0 + NT, c * 128:(c + 1) * 128].rearrange(
                        "(s p) v -> p s v", p=128), S2[:, :, :])
                k += 1


def ssd_consts():
    t = np.arange(128)[:, None]
    s = np.arange(128)[None, :]
    U = (t > s).astype(np.float32)
    Tri = (t <= s).astype(np.float32)
    NEG = np.where(s >= t, 0.0, -30000.0).astype(np.float32)
    return U, Tri, NEG


def ssd_phase(fw, xcT, xB_tm, z_tm, dt_tm, yT, U_d, Tri_d, NEG_d, ident_d, dtb_d, alog_d, dsk_d, wn_d):
    with fw.phase():
        ones = Buf(fw, "ones", [128, 128])
        epsb = Buf(fw, "epsb", [128, 1])
        U = Buf(fw, "U", [128, 128])
        Tri = Buf(fw, "Tri", [128, 128])
        NEG = Buf(fw, "NEG", [128, 128])
        idn = Buf(fw, "idn", [128, 128])
        dtb = Buf(fw, "dtb", [128, 8])
        An = Buf(fw, "An", [128, 8])
        dsk = Buf(fw, "dsk", [128, 8])
        wn = Buf(fw, "wn", [128, 512])
        BT = [Buf(fw, f"BT{i}", [128, 2, 128]) for i in range(2)]
        CT = [Buf(fw, f"CT{i}", [128, 2, 128]) for i in range(2)]
        XB = [Buf(fw, f"XB{i}", [128, 768]) for i in range(2)]
        Z = [Buf(fw, f"Z{i}", [128, 512]) for i in range(2)]
        DT = [Buf(fw, f"DT{i}", [128, 8]) for i in range(2)]
        dt = Buf(fw, "dt", [128, 8])
        a = Buf(fw, "a", [128, 8])
        e3 = Buf(fw, "e3", [128, 3, 8])
        xdt = Buf(fw, "xdt", [128, 8, 64])
        xdt2 = Buf(fw, "xdt2", [128, 8, 64])
        CBT = Buf(fw, "CBT", [128, 2, 128])
        lh = [Buf(fw, f"lh{i}", [128, 128]) for i in range(8)]
        Lh = [Buf(fw, f"Lh{i}", [128, 512]) for i in range(2)]
        Wh = [Buf(fw, f"Wh{i}", [128, 512]) for i in range(2)]
        yo = Buf(fw, "yo", [128, 8, 64])
        y = Buf(fw, "y", [128, 8, 64])
        sz = Buf(fw, "sz", [128, 512])
        junk = Buf(fw, "junk", [128, 256])
        ssq = Buf(fw, "ssq", [128, 2])
        rs = Buf(fw, "rs", [128, 2])
        yc = [Buf(fw, f"yc{i}", [128, 4, 128]) for i in range(2)]
        ST = [Buf(fw, f"ST{i}", [128, 4, 64]) for i in range(2)]
        sm_ps = Buf(fw, "sm_ps", [128, 3, 8], psum=True)
        cb_ps = Buf(fw, "cb_ps", [128, 2, 128], psum=True)
        dm_ps = [Buf(fw, f"dm_ps{i}", [128, 512], psum=True) for i in range(2)]
        yd_ps = Buf(fw, "yd_ps", [128, 8, 64], psum=True)
        yo_ps = Buf(fw, "yo_ps", [128, 8, 64], psum=True)
        st_ps = Buf(fw, "st_ps", [128, 2, 256], psum=True)
        tr_ps = Buf(fw, "tr_ps", [128, 4, 128], psum=True)
        fw.op("pool", lambda e: e.memset(ones[:, :], 1.0), writes=[ones.k])
        fw.op("pool", lambda e: e.memset(epsb[:, :], EPS), writes=[epsb.k])
        for i in range(2):
            fw.op("pool", lambda e, i=i: e.memset(ST[i][:, :, :], 0.0), writes=[ST[i].k])
        load(fw, "sp", U, U[:, :], U_d)
        load(fw, "sp", Tri, Tri[:, :], Tri_d)
        load(fw, "sp", NEG, NEG[:, :], NEG_d)
        load(fw, "sp", idn, idn[:, :], ident_d)
        load(fw, "sp", dtb, dtb[:, :], dtb_d.partition_broadcast(128))
        load(fw, "sp", An, An[:, :], alog_d.partition_broadcast(128))
        load(fw, "sp", dsk, dsk[:, :], dsk_d.partition_broadcast(128))
        load(fw, "sp", wn, wn[:, :], wn_d.partition_broadcast(128))
        fw.op("act", lambda e: e.activation(An[:, :], An[:, :], AF.Exp), reads=[An.k], writes=[An.k])
        fw.op("dve", lambda e: e.tensor_scalar(An[:, :], An[:, :], -1.0, None, ALU.mult),
              reads=[An.k], writes=[An.k])
        BTr = xcT[512:768].rearrange("(g p) t -> p g t", p=128)
        CTr = xcT[768:1024].rearrange("(g p) t -> p g t", p=128)
        yTr = yT.rearrange("(c p) t -> p c t", p=128)

        def bc8(ap):
            return ap.unsqueeze(2).to_broadcast([128, 8, 64])

        for n in range(T // 128):
            ts = slice(n * 128, (n + 1) * 128)
            b = n % 2
            B_, C_, X_, Z_, D_ = BT[b], CT[b], XB[b], Z[b], DT[b]
            load(fw, "sp", B_, B_[:, :, :], BTr[:, :, ts])
            load(fw, "sp", C_, C_[:, :, :], CTr[:, :, ts])
            load(fw, "pool", X_, X_[:, :], xB_tm[ts, :])
            load(fw, "pool", Z_, Z_[:, :], z_tm[ts, :])
            load(fw, "pool", D_, D_[:, :], dt_tm[ts, :])
            x3 = X_[:, 0:512].rearrange("p (h d) -> p h d", h=8)
            fw.op("dve", lambda e, D_=D_: e.tensor_tensor(dt[:, :], D_[:, :], dtb[:, :], ALU.add),
                  reads=[D_.k, dtb.k], writes=[dt.k])
            fw.op("act", lambda e: e.activation(dt[:, :], dt[:, :], AF.Exp), reads=[dt.k], writes=[dt.k])
            fw.op("act", lambda e: e.activation(dt[:, :], dt[:, :], AF.Ln, bias=1.0), reads=[dt.k], writes=[dt.k])
            fw.op("dve", lambda e: e.tensor_tensor(a[:, :], dt[:, :], An[:, :], ALU.mult),
                  reads=[dt.k, An.k], writes=[a.k])
            mm_group(fw, sm_ps, sm_ps[:, 0, :], [(Tri[:, :], a[:, :])], [Tri.k, a.k])
            mm_group(fw, sm_ps, sm_ps[:, 1, :], [(ones[:, :], a[:, :])], [ones.k, a.k])
            mm_group(fw, sm_ps, sm_ps[:, 2, :], [(U[:, :], a[:, :])], [U.k, a.k])
            fw.op("act", lambda e: e.activation(e3[:, :, :], sm_ps[:, :, :], AF.Exp),
                  reads=[sm_ps.k], writes=[e3.k])
            fw.op("dve", lambda e, x3=x3, X_=X_: e.tensor_tensor(xdt[:, :, :], x3, bc8(dt[:, :]), ALU.mult),
                  reads=[X_.k, dt.k], writes=[xdt.k])
            fw.op("dve", lambda e: e.tensor_tensor(xdt2[:, :, :], xdt[:, :, :], bc8(e3[:, 2, :]), ALU.mult),
                  reads=[xdt.k, e3.k], writes=[xdt2.k])
            for g in range(2):
                mm_group(fw, cb_ps, cb_ps[:, g, :], [(B_[:, g, :], C_[:, g, :])], [B_.k, C_.k])
            fw.op("act", lambda e: e.copy(CBT[:, :, :], cb_ps[:, :, :]), reads=[cb_ps.k], writes=[CBT.k])
            for h in range(8):
                LH = lh[h]
                fw.op("dve", lambda e, LH=LH, h=h: e.tensor_scalar(
                    LH[:, :], U[:, :], a[:, h:h + 1], None, ALU.mult), reads=[U.k, a.k], writes=[LH.k])
            for h in range(8):
                DM = dm_ps[(h // 4) % 2]
                dm = DM[:, (h % 4) * 128:(h % 4 + 1) * 128]
                mm_group(fw, DM, dm, [(lh[h][:, :], Tri[:, :]), (idn[:, :], NEG[:, :])],
                         [lh[h].k, Tri.k, idn.k, NEG.k])
            for hh in range(2):
                DM = dm_ps[hh]
                fw.op("act", lambda e, DM=DM, hh=hh: e.activation(
                    Lh[hh][:, :], DM[:, :], AF.Exp), reads=[DM.k], writes=[Lh[hh].k])
                fw.op("dve", lambda e, hh=hh: e.tensor_tensor(
                    Wh[hh][:, :].rearrange("p (h l) -> p h l", h=4),
                    Lh[hh][:, :].rearrange("p (h l) -> p h l", h=4),
                    CBT[:, hh, :].unsqueeze(1).to_broadcast([128, 4, 128]), ALU.mult),
                    reads=[Lh[hh].k, CBT.k], writes=[Wh[hh].k])
            for h in range(8):
                WW = Wh[h // 4]
                mm_group(fw, yd_ps, yd_ps[:, h, :], [(WW[:, (h % 4) * 128:(h % 4 + 1) * 128], xdt[:, h, :])],
                         [WW.k, xdt.k])
            for g in range(2):
                mm_group(fw, yo_ps, yo_ps[:, g * 4:(g + 1) * 4, :].rearrange("p h d -> p (h d)"),
                         [(C_[:, g, :], ST[g][:, :, :].rearrange("p h d -> p (h d)"))], [C_.k, ST[g].k])
            fw.op("dve", lambda e: e.tensor_tensor(yo[:, :, :], yo_ps[:, :, :], bc8(e3[:, 0, :]), ALU.mult),
                  reads=[yo_ps.k, e3.k], writes=[yo.k])
            fw.op("dve", lambda e: e.tensor_tensor(y[:, :, :], yd_ps[:, :, :], yo[:, :, :], ALU.add),
                  reads=[yd_ps.k, yo.k], writes=[y.k])
            fw.op("dve", lambda e, x3=x3, X_=X_: e.tensor_tensor(yo[:, :, :], x3, bc8(dsk[:, :]), ALU.mult),
                  reads=[X_.k, dsk.k], writes=[yo.k])
            fw.op("dve", lambda e: e.tensor_tensor(y[:, :, :], y[:, :, :], yo[:, :, :], ALU.add),
                  reads=[y.k, yo.k], writes=[y.k])
            fw.op("act", lambda e, Z_=Z_: e.activation(sz[:, :], Z_[:, :], AF.Silu), reads=[Z_.k], writes=[sz.k])
            y2 = y[:, :, :].rearrange("p h d -> p (h d)")
            fw.op("dve", lambda e, y2=y2: e.tensor_tensor(y2, y2, sz[:, :], ALU.mult),
                  reads=[y.k, sz.k], writes=[y.k])
            fw.op("pool", lambda e: e.memset(ssq[:, :], 0.0), writes=[ssq.k])
            for g in range(2):
                fw.op("act", lambda e, g=g, y2=y2: e.activation(
                    junk[:, :], y2[:, g * 256:(g + 1) * 256], AF.Square, accum_out=ssq[:, g:g + 1]),
                    reads=[y.k], writes=[junk.k, ssq.k])
            rstd_from_ss(fw, rs, ssq, 256, 2, epsb)
            for g in range(2):
                fw.op("dve", lambda e, g=g, y2=y2: e.scalar_tensor_tensor(
                    y2[:, g * 256:(g + 1) * 256], y2[:, g * 256:(g + 1) * 256], rs[:, g:g + 1],
                    wn[:, g * 256:(g + 1) * 256], ALU.mult, ALU.mult),
                    reads=[y.k, rs.k, wn.k], writes=[y.k])
            for c in range(4):
                fw.op("pe", lambda e, c=c, y2=y2: e.transpose(tr_ps[:, c, :], y2[:, c * 128:(c + 1) * 128],
                                                               idn[:, :]),
                      reads=[y.k, idn.k], writes=[tr_ps.k])
            YC = yc[b]
            fw.op("act", lambda e, YC=YC: e.copy(YC[:, :, :], tr_ps[:, :, :]), reads=[tr_ps.k], writes=[YC.k])
            store(fw, "sp", YC, yTr[:, :, ts], YC[:, :, :])
            for g in range(2):
                mm_group(fw, st_ps, st_ps[:, g, :],
                         [(X_[:, 512 + g * 128:512 + (g + 1) * 128],
                           xdt2[:, g * 4:(g + 1) * 4, :].rearrange("p h d -> p (h d)"))], [X_.k, xdt2.k])
            for g in range(2):
                fw.op("dve", lambda e, g=g: e.tensor_tensor(
                    ST[g][:, :, :], ST[g][:, :, :],
                    e3[:, 1, g * 4:(g + 1) * 4].unsqueeze(2).to_broadcast([128, 4, 64]), ALU.mult),
                    reads=[ST[g].k, e3.k], writes=[ST[g].k])
                fw.op("dve", lambda e, g=g: e.tensor_tensor(
                    ST[g][:, :, :].rearrange("p h d -> p (h d)"),
                    ST[g][:, :, :].rearrange("p h d -> p (h d)"), st_ps[:, g, :], ALU.add),
                    reads=[ST[g].k, st_ps.k], writes=[ST[g].k])


def qknorm_phase(fw, qT, kT, qnT, knT, wq_d, wk_d, bones_d, NT=512):
    with fw.phase():
        bo = Buf(fw, "bo", [128, 128])
        epsb = Buf(fw, "epsb", [128, 1])
        wq = Buf(fw, "wq", [128, 1])
        wk = Buf(fw, "wk", [128, 1])
        xt = [Buf(fw, f"xt{i}", [128, NT]) for i in range(3)]
        sq = [Buf(fw, f"sq{i}", [128, NT]) for i in range(2)]
        rstd = [Buf(fw, f"rstd{i}", [128, NT]) for i in range(2)]
        xo = [Buf(fw, f"xo{i}", [128, NT]) for i in range(3)]
        ss_ps = [Buf(fw, f"ss_ps{i}", [128, NT], psum=True) for i in range(2)]
        fw.op("pool", lambda e: e.memset(epsb[:, :], EPS), writes=[epsb.k])
        load(fw, "sp", bo, bo[:, :], bones_d)
        load(fw, "sp", wq, wq[:, :], wq_d)
        load(fw, "sp", wk, wk[:, :], wk_d)
        fw.op("dve", lambda e: e.tensor_scalar(wq[:, :], wq[:, :], 0.125, None, ALU.mult),
              reads=[wq.k], writes=[wq.k])
        k = 0
        for (src, dst, w) in ((qT, qnT, wq), (kT, knT, wk)):
            for it in range(T // NT):
                ts = slice(it * NT, (it + 1) * NT)
                for c in range(4):
                    X, S_, R_, XO, SS = xt[k % 3], sq[k % 2], rstd[k % 2], xo[k % 3], ss_ps[k % 2]
                    k += 1
                    rows = slice(c * 128, (c + 1) * 128)
                    load(fw, "sp", X, X[:, :], src[rows, ts])
                    fw.op("act", lambda e, X=X, S_=S_: e.activation(S_[:, :], X[:, :], AF.Square),
                          reads=[X.k], writes=[S_.k])
                    mm_group(fw, SS, SS[:, :], [(bo[:, :], S_[:, :])], [bo.k, S_.k])
                    rstd_from_ss(fw, R_, SS, 64, NT, epsb)
                    fw.op("dve", lambda e, X=X, XO=XO, R_=R_, w=w: e.scalar_tensor_tensor(
                        XO[:, :], X[:, :], w[:, 0:1], R_[:, :], ALU.mult, ALU.mult),
                        reads=[X.k, w.k, R_.k], writes=[XO.k])
                    store(fw, "pool", XO, dst[rows, ts], XO[:, :])


def t5_consts():
    k = np.arange(128)[:, None]
    q = np.arange(128)[None, :]
    E = np.zeros((33, 2, 128, 128), np.float32)
    for blk, off in ((0, 0), (1, 128)):
        n = q - k + off
        valid = n >= 0
        nn = np.maximum(n, 0)
        nf = np.maximum(nn, 1).astype(np.float32)
        large = 16 + (np.log(nf / np.float32(16)) / np.float32(np.log(128 / 16)) * np.float32(16)).astype(np.int32)
        large = np.minimum(large, 31)
        bucket = np.where(nn < 16, nn, large)
        for b in range(32):
            E[b, blk] = ((bucket == b) & valid).astype(np.float32)
        E[32, blk] = (~valid).astype(np.float32)
    return E.reshape(33, 2 * 16384)


def bias_phase(fw, rel_bias_d, Ecat_d, BnT):
    with fw.phase():
        tab = Buf(fw, "tab", [64, 4])
        t31 = Buf(fw, "t31", [32, 4])
        Ec = [Buf(fw, f"Ec{i}", [33, 4096]) for i in range(2)]
        out = [Buf(fw, f"out{i}", [4, 4096]) for i in range(2)]
        b_ps = [Buf(fw, f"b_ps{i}", [4, 512], psum=True) for i in range(2)]
        fw.op("pool", lambda e: e.memset(tab[:, :], -30000.0), writes=[tab.k])
        load(fw, "sp", tab, tab[0:32, :], rel_bias_d)
        load(fw, "sp", t31, t31[:, :], rel_bias_d[31:32, :].partition_broadcast(32))
        fw.op("dve", lambda e: e.tensor_tensor(tab[0:32, :], tab[0:32, :], t31[:, :], ALU.subtract),
              reads=[tab.k, t31.k], writes=[tab.k])
        k = 0
        for pc in range(8):
            E_, O_ = Ec[pc % 2], out[pc % 2]
            load(fw, "sp", E_, E_[:, :], Ecat_d[:, pc * 4096:(pc + 1) * 4096])
            for j in range(8):
                P_ = b_ps[k % 2]
                k += 1
                mm_group(fw, P_, P_[:, :], [(tab[0:33, :], E_[0:33, j * 512:(j + 1) * 512])], [tab.k, E_.k])
                fw.op("dve", lambda e, P_=P_, O_=O_, j=j: e.tensor_copy(O_[:, j * 512:(j + 1) * 512], P_[:, :]),
                      reads=[P_.k], writes=[O_.k])
            store(fw, "pool", O_, BnT[:, pc * 4096:(pc + 1) * 4096], O_[:, :])


def attn_phase(fw, qnT, knT, v_tm, BnT, yT, ident_d, lq1, lk1, lq2, lk2, subln_d, lambda_init):
    NQ = 512
    with fw.phase():
        idn = Buf(fw, "idn", [128, 128])
        epsb = Buf(fw, "epsb", [128, 1])
        lv = Buf(fw, "lv", [128, 4, 64])
        lt = Buf(fw, "lt", [128, 2, 64])
        ls = Buf(fw, "ls", [128, 2])
        lam = Buf(fw, "lam", [128, 1])
        subw = Buf(fw, "subw", [128, 128])
        qn = Buf(fw, "qn", [128, T])
        kn = Buf(fw, "kn", [128, T])
        va = Buf(fw, "va", [128, 32, 129])
        Bn = Buf(fw, "Bn", [128, 2, 128])
        tmp = [Buf(fw, f"tmp{i}", [128, 128]) for i in range(2)]
        PT = [Buf(fw, f"PT{i}", [128, NQ]) for i in range(3)]
        om = [Buf(fw, f"om{i}", [128, 4, 129]) for i in range(2)]
        rr = Buf(fw, "rr", [128, 4, 2])
        t1 = Buf(fw, "t1", [128, 128])
        o = Buf(fw, "o", [128, 4, 128])
        junk = Buf(fw, "junk", [128, 128])
        ssq = Buf(fw, "ssq", [128, 4])
        rs = Buf(fw, "rs", [128, 4])
        yd = [Buf(fw, f"yd{i}", [128, 4, 128]) for i in range(2)]
        s_ps = [Buf(fw, f"s_ps{i}", [128, NQ], psum=True) for i in range(2)]
        o_ps = [Buf(fw, f"o_ps{i}", [128, 512], psum=True) for i in range(4)]
        tr_ps = Buf(fw, "tr_ps", [128, 4, 128], psum=True)
        fw.op("pool", lambda e: e.memset(epsb[:, :], EPS), writes=[epsb.k])
        load(fw, "sp", idn, idn[:, :], ident_d)
        for i, l in enumerate((lq1, lk1, lq2, lk2)):
            load(fw, "sp", lv, lv[:, i, :], l.partition_broadcast(128))
        load(fw, "sp", subw, subw[:, :], subln_d.partition_broadcast(128))
        fw.op("dve", lambda e: e.tensor_scalar(subw[:, :], subw[:, :], 1.0 - lambda_init, None, ALU.mult),
              reads=[subw.k], writes=[subw.k])
        fw.op("dve", lambda e: e.tensor_tensor(lt[:, 0, :], lv[:, 0, :], lv[:, 1, :], ALU.mult),
              reads=[lv.k], writes=[lt.k])
        fw.op("dve", lambda e: e.tensor_tensor(lt[:, 1, :], lv[:, 2, :], lv[:, 3, :], ALU.mult),
              reads=[lv.k, lt.k], writes=[lt.k])
        fw.op("dve", lambda e: e.reduce_sum(ls[:, :], lt[:, :, :], axis=AX.X), reads=[lt.k], writes=[ls.k])
        fw.op("act", lambda e: e.activation(ls[:, :], ls[:, :], AF.Exp), reads=[ls.k], writes=[ls.k])
        fw.op("dve", lambda e: e.tensor_tensor(lam[:, :], ls[:, 0:1], ls[:, 1:2], ALU.subtract),
              reads=[ls.k], writes=[lam.k])
        fw.op("dve", lambda e: e.tensor_scalar(lam[:, :], lam[:, :], float(lambda_init), None, ALU.add),
              reads=[lam.k], writes=[lam.k])
        fw.op("pool", lambda e: e.memset(va[:, :, 128:129], 1.0), writes=[va.k])
        yTr = yT.rearrange("(c p) t -> p c t", p=128)
        pk = 0
        sk = 0
        for h in range(4):
            rows = slice(h * 128, (h + 1) * 128)
            load(fw, "sp", qn, qn[:, :], qnT[rows, :])
            load(fw, "sp", kn, kn[:, :], knT[rows, :])
            load(fw, "pool", va, va[:, :, 0:128],
                 v_tm[:, rows].rearrange("(b p) v -> p b v", p=128))
            load(fw, "pool", Bn, Bn[:, :, :], BnT[h].rearrange("(b k q) -> k b q", b=2, k=128))
            state = {"sk": 0, "pk": 0}

            def emit_S(qg, m, kb):
                mp = slice(m * 64, (m + 1) * 64)
                SP = s_ps[state["sk"] % 2]
                state["sk"] += 1
                mm_group(fw, SP, SP[:, :], [(kn[mp, kb * 128:(kb + 1) * 128],
                                             qn[mp, qg * NQ:(qg + 1) * NQ])], [kn.k, qn.k])
                P_ = PT[state["pk"] % 3]
                state["pk"] += 1
                if kb <= 4 * qg - 2:
                    fw.op("act", lambda e, P_=P_, SP=SP: e.activation(P_[:, :], SP[:, :], AF.Exp),
                          reads=[SP.k], writes=[P_.k])
                    js = list(range(4))
                else:
                    js = []
                    for j in range(4):
                        qb = 4 * qg + j
                        cs = slice(j * 128, (j + 1) * 128)
                        if kb > qb:
                            continue
                        js.append(j)
                        if kb < qb - 1:
                            fw.op("act", lambda e, P_=P_, SP=SP, cs=cs: e.activation(
                                P_[:, cs], SP[:, cs], AF.Exp), reads=[SP.k], writes=[P_.k])
                        else:
                            TM = tmp[j % 2]
                            bi = 0 if kb == qb else 1
                            fw.op("dve", lambda e, TM=TM, SP=SP, cs=cs, bi=bi: e.tensor_tensor(
                                TM[:, :], SP[:, cs], Bn[:, bi, :], ALU.add),
                                reads=[SP.k, Bn.k], writes=[TM.k])
                            fw.op("act", lambda e, P_=P_, TM=TM, cs=cs: e.activation(
                                P_[:, cs], TM[:, :], AF.Exp), reads=[TM.k], writes=[P_.k])
                return P_, js

            def emit_V(qg, m, kb, P_, js):
                for j in js:
                    qb = 4 * qg + j
                    OP = o_ps[j]
                    fw.op("pe", lambda e, OP=OP, P_=P_, j=j, kb=kb, qb=qb: e.matmul(
                        OP[:, 0:129], mm(P_[:, j * 128:(j + 1) * 128]), mm(va[:, kb, :]),
                        start=(kb == 0), stop=(kb == qb)),
                        reads=[P_.k, va.k], writes=[OP.k], pe_acc=(kb > 0))

            items = [(qg, m, kb) for qg in range(T // NQ) for m in range(2) for kb in range(4 * qg + 4)]
            nxt = emit_S(*items[0])
            for ii, (qg, m, kb) in enumerate(items):
                cur_ = nxt
                if ii + 1 < len(items):
                    nxt = emit_S(*items[ii + 1])
                emit_V(qg, m, kb, *cur_)
                if kb != 4 * qg + 3:
                    continue
                OM = om[m]
                for j in range(4):
                    if j % 2 == 0:
                        fw.op("act", lambda e, OM=OM, j=j: e.copy(OM[:, j, :], o_ps[j][:, 0:129]),
                              reads=[o_ps[j].k], writes=[OM.k])
                    else:
                        fw.op("dve", lambda e, OM=OM, j=j: e.tensor_copy(OM[:, j, :], o_ps[j][:, 0:129]),
                              reads=[o_ps[j].k], writes=[OM.k])
                if m == 0:
                    continue
                YD = yd[qg % 2]
                fw.op("dve", lambda e: e.reciprocal(rr[:, :, 0:1], om[0][:, :, 128:129]),
                      reads=[om[0].k], writes=[rr.k])
                fw.op("dve", lambda e: e.reciprocal(rr[:, :, 1:2], om[1][:, :, 128:129]),
                      reads=[om[1].k, rr.k], writes=[rr.k])
                fw.op("pool", lambda e: e.memset(ssq[:, :], 0.0), writes=[ssq.k])
                for j in range(4):
                    fw.op("dve", lambda e, j=j: e.tensor_scalar(
                        t1[:, :], om[1][:, j, 0:128], rr[:, j, 1:2], lam[:, 0:1], ALU.mult, ALU.mult),
                        reads=[om[1].k, rr.k, lam.k], writes=[t1.k])
                    fw.op("dve", lambda e, j=j: e.scalar_tensor_tensor(
                        o[:, j, :], om[0][:, j, 0:128], rr[:, j, 0:1], t1[:, :], ALU.mult, ALU.subtract),
                        reads=[om[0].k, rr.k, t1.k], writes=[o.k])
                    fw.op("act", lambda e, j=j: e.activation(junk[:, :], o[:, j, :], AF.Square,
                                                             accum_out=ssq[:, j:j + 1]),
                          reads=[o.k], writes=[junk.k, ssq.k])
                rstd_from_ss(fw, rs, ssq, 128, 4, epsb)
                for j in range(4):
                    fw.op("dve", lambda e, j=j: e.scalar_tensor_tensor(
                        o[:, j, :], o[:, j, :], rs[:, j:j + 1], subw[:, :], ALU.mult, ALU.mult),
                        reads=[o.k, rs.k, subw.k], writes=[o.k])
                    fw.op("pe", lambda e, j=j: e.transpose(tr_ps[:, j, :], o[:, j, :], idn[:, :]),
                          reads=[o.k, idn.k], writes=[tr_ps.k])
                fw.op("act", lambda e, YD=YD: e.copy(YD[:, :, :], tr_ps[:, :, :]), reads=[tr_ps.k], writes=[YD.k])
                store(fw, "sp", YD, yTr[:, h, qg * NQ:(qg + 1) * NQ].rearrange("p (j q) -> p j q", j=4),
                      YD[:, :, :])


def _lay_w(W):
    n = W.shape[1]
    return np.ascontiguousarray(W.reshape(8, 128, n).transpose(1, 0, 2))


def _lay_g(g):
    return np.ascontiguousarray(g.reshape(8, 128).T)


def _lay_gu(W):
    return np.ascontiguousarray(W.reshape(8, 128, NFC, 128).transpose(2, 1, 0, 3))


def _lay_d(W):
    return np.ascontiguousarray(W.reshape(NFC, 128, 8, 128).transpose(2, 1, 0, 3))


def host_layout(inputs):
    f = lambda a: np.ascontiguousarray(np.asarray(a, dtype=np.float32))
    sh = {}
    for i in range(2):
        for j in range(2):
            sh[f"wg{i}{j}"] = _lay_gu(f(inputs["ffn_w_gate"][i, j]))
            sh[f"wu{i}{j}"] = _lay_gu(f(inputs["ffn_w_up"][i, j]))
            sh[f"wd{i}{j}"] = _lay_d(f(inputs["ffn_w_down"][i, j]))
            sh[f"fg{i}{j}"] = _lay_g(f(inputs["ffn_norm"][i, j]))
        sh[f"mg{i}"] = _lay_g(f(inputs["mix_norm"][i]))
    Mc, M3, maskA = gla_consts()
    cur, prev, cur0 = pool_consts()
    U, Tri, NEG = ssd_consts()
    sh["ev_w_in"] = _lay_w(f(inputs["ev_w_in"][0]))
    sh["Mc"], sh["M3"], sh["mA"] = Mc, M3, maskA
    sh["wgk1"] = f(np.concatenate([inputs["ev_w_gk_up"][0], inputs["ev_b_gk"][0][None]], 0))
    sh["gnorm"] = f(inputs["ev_gla_norm"][0][:, None])
    sh["wpool"] = f(np.asarray(inputs["ev_w_pool"][0]).transpose(1, 0, 2))
    sh["pscale"] = f(np.asarray(inputs["ev_pool_scale"][0]).reshape(4, 128).T)
    sh["cur"] = f(cur.transpose(1, 0, 2))
    sh["prev"] = f(prev.transpose(1, 0, 2))
    sh["cur0"] = f(cur0.transpose(1, 0, 2))
    sh["ev_w_out"] = _lay_w(f(inputs["ev_w_out"][0]))
    sh["od_w_in"] = _lay_w(f(inputs["od_w_in"][0]))
    sh["U"], sh["Tri"], sh["NEG"] = U, Tri, NEG
    sh["ident"] = np.eye(128, dtype=np.float32)
    sh["cw"] = f(np.asarray(inputs["od_conv_w"][0]).reshape(4, 8, 128).transpose(2, 1, 0))
    sh["cb"] = f(np.asarray(inputs["od_conv_b"][0]).reshape(8, 128).T)
    sh["dtb"] = f(inputs["od_dt_bias"])
    sh["alog"] = f(inputs["od_a_log"])
    sh["dsk"] = f(inputs["od_d_skip"])
    sh["wn"] = f(inputs["od_ssd_norm"])
    sh["wq"] = f(np.tile(np.asarray(inputs["od_q_norm"][0]), 2)[:, None])
    sh["wk"] = f(np.tile(np.asarray(inputs["od_k_norm"][0]), 2)[:, None])
    sh["bones"] = np.kron(np.eye(2), np.ones((64, 64))).astype(np.float32)
    sh["relb"] = f(inputs["rel_bias"])
    sh["Ecat"] = t5_consts()
    for nm in ("q1", "k1", "q2", "k2"):
        sh["l" + nm] = f(inputs["od_lambda_" + nm])
    sh["subln"] = f(inputs["od_subln"])
    sh["od_w_out"] = _lay_w(f(inputs["od_w_out"][0]))
    return sh


def build_program(shared_shapes):
    import math
    nc = bass.Bass("TRN2", target_bir_lowering=False)
    A = {}
    A["xin"] = nc.dram_tensor("xin", [D, T], F32, kind="ExternalInput").ap()
    for k, shp in shared_shapes.items():
        A[k] = nc.dram_tensor(k, list(shp), F32, kind="ExternalInput").ap()
    xout = nc.dram_tensor("xout", [D, T], F32, kind="ExternalOutput").ap()

    def Sx(name, shape):
        return nc.dram_tensor(name, list(shape), F32).ap()

    xa, xb = Sx("xa", [D, T]), Sx("xb", [D, T])
    yT = Sx("yT", [1024, T])
    qT, kT = Sx("qT", [512, T]), Sx("kT", [512, T])
    gT, lrT = Sx("gT", [512, T]), Sx("lrT", [16, T])
    k_tm, v_tm, u_tm = Sx("k_tm", [T, 256]), Sx("v_tm", [T, 512]), Sx("u_tm", [T, 512])
    xbcT, xcT, xB_tm = Sx("xbcT", [1024, T]), Sx("xcT", [1024, T]), Sx("xB_tm", [T, 768])
    qnT, knT = Sx("qnT", [512, T]), Sx("knT", [512, T])
    z_tm, dt_tm, BnT = Sx("z_tm", [T, 512]), Sx("dt_tm", [T, 8]), Sx("BnT", [4, 32768])
    with contextlib.ExitStack() as st:
        fw = FW(nc, st)
        ffn_phase(fw, A["xin"], xa, A["wg00"], A["wu00"], A["wd00"], A["fg00"])
        fm = [(0, 128, qT[0:128]), (128, 128, qT[128:256]), (256, 128, kT[0:128]), (384, 128, kT[128:256])]
        fm += [(1024 + i * 128, 128, gT[i * 128:(i + 1) * 128]) for i in range(4)]
        fm += [(1536, 16, lrT)]
        tm = [(256, 256, k_tm), (512, 512, v_tm), (1552, 512, u_tm)]
        inproj_phase(fw, xa, A["ev_w_in"], 2064, A["mg0"], fm, tm)
        gla_phase(fw, qT[0:256], kT[0:256], gT, lrT, k_tm, v_tm, yT[0:512], A["Mc"], A["M3"], A["mA"],
                  A["wgk1"], A["gnorm"])
        pool_phase(fw, u_tm, yT[512:1024], A["wpool"], A["pscale"], A["cur"], A["prev"], A["cur0"])
        outproj_phase(fw, xa, xb, yT, A["ev_w_out"])
        ffn_phase(fw, xb, xa, A["wg01"], A["wu01"], A["wd01"], A["fg01"])
        ffn_phase(fw, xa, xb, A["wg10"], A["wu10"], A["wd10"], A["fg10"])
        fm = [(512 + i * 128, 128, xbcT[i * 128:(i + 1) * 128]) for i in range(8)]
        fm += [(1544 + i * 128, 128, qT[i * 128:(i + 1) * 128]) for i in range(4)]
        fm += [(2056 + i * 128, 128, kT[i * 128:(i + 1) * 128]) for i in range(4)]
        tm = [(0, 512, z_tm), (1536, 8, dt_tm), (2568, 512, v_tm)]
        inproj_phase(fw, xb, A["od_w_in"], 3080, A["mg1"], fm, tm)
        conv_phase(fw, xbcT, xcT, xB_tm, A["cw"], A["cb"], A["ident"])
        ssd_phase(fw, xcT, xB_tm, z_tm, dt_tm, yT[0:512], A["U"], A["Tri"], A["NEG"], A["ident"],
                  A["dtb"], A["alog"], A["dsk"], A["wn"])
        qknorm_phase(fw, qT, kT, qnT, knT, A["wq"], A["wk"], A["bones"])
        bias_phase(fw, A["relb"], A["Ecat"], BnT)
        attn_phase(fw, qnT, knT, v_tm, BnT, yT[512:1024], A["ident"], A["lq1"], A["lk1"], A["lq2"],
                   A["lk2"], A["subln"], 0.8 - 0.6 * math.exp(-0.3 * 1))
        outproj_phase(fw, xb, xa, yT, A["od_w_out"])
        ffn_phase(fw, xa, xout, A["wg11"], A["wu11"], A["wd11"], A["fg11"])
    return nc


def kernel(**inputs):
    x = np.asarray(inputs["x"], dtype=np.float32)
    sh = host_layout(inputs)
    nc = build_program({k: v.shape for k, v in sh.items()})
    in_maps = []
    for b in range(8):
        m = dict(sh)
        m["xin"] = np.ascontiguousarray(x[b].T)
        in_maps.append(m)
    res = run_bass_kernel_spmd(nc, in_maps, core_ids=list(range(8)))
    out = np.stack([np.ascontiguousarray(res.results[b]["xout"].T) for b in range(8)], 0)
    return out.astype(np.float32)
```

```python
import contextlib
import numpy as np
import concourse.bass as bass
import concourse.mybir as mybir
from concourse.bass_utils import run_bass_kernel_spmd

F32 = mybir.dt.float32
F32R = mybir.dt.float32r
ALU = mybir.AluOpType
AF = mybir.ActivationFunctionType
AX = mybir.AxisListType

T = 4096
D = 1024
DFF = 2816
NFC = DFF // 128
EPS = 1e-6
MM_FAST = False


def mm(ap):
    return ap.bitcast(F32R) if MM_FAST else ap


class Trk:
    __slots__ = ("w", "r", "sem", "name")

    def __init__(self, name=""):
        self.w = None
        self.r = []
        self.sem = None
        self.name = name


class Op:
    __slots__ = ("eng", "fn", "deps", "flag", "sem", "val", "dma", "n")

    def __init__(self, eng, fn, dma=False, n=1):
        self.eng = eng
        self.fn = fn
        self.deps = []
        self.flag = False
        self.sem = None
        self.val = 0
        self.dma = dma
        self.n = n


EPOCH = 6000
ENGS = ("pe", "act", "dve", "pool", "sp")


class FW:
    def __init__(self, nc, stack, n_dma_sems=48):
        self.nc = nc
        self.stack = stack
        self.eng_obj = {"pe": nc.tensor, "act": nc.scalar, "dve": nc.vector,
                        "pool": nc.gpsimd, "sp": nc.sync}
        self.eng_sems = {e: [] for e in ENGS}
        self.eng_cnt = {e: 0 for e in ENGS}
        self.free_dma = {}
        self.dma_cnt = {}
        self.n_sem = 0
        self.ops = []
        self.phase_dma_sems = set()
        self.waited = {e: {} for e in ENGS}
        self.phase_stack = None
        self.n_total = 0

    def new_sem(self, name):
        self.n_sem += 1
        return self.stack.enter_context(self.nc.semaphore(f"{name}_{self.n_sem}"))

    def sb(self, name, shape, dtype=F32):
        self.n_sem += 1
        name = f"{name}_{self.n_sem}"
        t = self.phase_stack.enter_context(self.nc.sbuf_tensor(name, list(shape), dtype))
        return t

    def ps(self, name, shape, dtype=F32):
        self.n_sem += 1
        name = f"{name}_{self.n_sem}"
        t = self.phase_stack.enter_context(self.nc.psum_tensor(name, list(shape), dtype))
        return t

    def _dep(self, op, reads, writes):
        deps = op.deps
        for t in reads:
            if t.w is not None:
                deps.append(t.w)
        for t in writes:
            if t.w is not None:
                deps.append(t.w)
            deps.extend(t.r)
        for t in reads:
            if not op.dma:
                t.r = [x for x in t.r if x.dma or x.eng != op.eng]
            t.r.append(op)
        for t in writes:
            t.w = op
            t.r = []

    def op(self, eng, fn, reads=(), writes=(), pe_acc=False):
        o = Op(eng, fn)
        self._dep(o, reads, writes)
        if pe_acc:
            o.deps = [d for d in o.deps if d.eng != "pe" or d.dma]
        self.ops.append(o)
        return o

    def dma(self, q, fns, sbuf_trk, reads=(), writes=()):
        if not isinstance(fns, (list, tuple)):
            fns = [fns]
        o = Op(q, fns, dma=True, n=len(fns))
        self._dep(o, reads, writes)
        cls = "sw" if q == "pool" else "hw"
        if sbuf_trk.sem is None:
            sbuf_trk.sem = {}
        if cls not in sbuf_trk.sem:
            fl = self.free_dma.setdefault(cls, [])
            if not fl:
                s = self.new_sem("dq" + cls)
                self.dma_cnt[s] = 0
                fl.append(s)
            sbuf_trk.sem[cls] = fl.pop()
            self.phase_dma_sems.add((cls, sbuf_trk.sem[cls]))
        o.sem = sbuf_trk.sem[cls]
        self.dma_cnt[o.sem] += 16 * len(fns)
        o.val = self.dma_cnt[o.sem]
        self.ops.append(o)
        return o

    def emit(self):
        nc = self.nc
        ops = self.ops
        for o in ops:
            for d in o.deps:
                if not d.dma:
                    d.flag = True
        last = {}
        for o in ops:
            if not o.dma:
                last[o.eng] = o
        for o in last.values():
            o.flag = True
        for o in ops:
            if o.dma or not o.flag:
                continue
            c = self.eng_cnt[o.eng]
            ep = c // EPOCH
            sems = self.eng_sems[o.eng]
            while len(sems) <= ep:
                sems.append(self.new_sem("e" + o.eng))
            o.sem = sems[ep]
            o.val = c % EPOCH + 1
            self.eng_cnt[o.eng] = c + 1
        targets = []
        for e, o in last.items():
            targets.append((o.sem, o.val))
        for (_c, s) in self.phase_dma_sems:
            targets.append((s, self.dma_cnt[s]))
        by_eng = {e: [] for e in ENGS}
        for o in ops:
            by_eng[o.eng].append(o)
        waited = self.waited

        def run(ename):
            def body(eng):
                wd = waited[ename]
                for o in by_eng[ename]:
                    for d in o.deps:
                        if d.sem is None:
                            continue
                        if wd.get(d.sem, 0) >= d.val:
                            continue
                        if d.eng == ename and not d.dma and ename == "pe":
                            pass
                        eng.wait_ge(d.sem, d.val)
                        wd[d.sem] = d.val
                    if o.dma:
                        for f in o.fn:
                            f(eng).then_inc(o.sem, 16)
                    else:
                        ins = o.fn(eng)
                        if o.flag:
                            ins.then_inc(o.sem, 1)
                for (s, v) in targets:
                    if wd.get(s, 0) < v:
                        eng.wait_ge(s, v)
                        wd[s] = v
            return body

        with nc.Block() as block:
            block.sync(run("sp"))
            block.scalar(run("act"))
            block.vector(run("dve"))
            block.gpsimd(run("pool"))
            block.tensor(run("pe"))
        self.n_total += len(ops)
        for (_c, s) in self.phase_dma_sems:
            if self.dma_cnt[s] < 24000:
                self.free_dma[_c].append(s)
        self.phase_dma_sems = set()
        self.ops = []

    @contextlib.contextmanager
    def phase(self):
        with contextlib.ExitStack() as st:
            self.phase_stack = st
            yield
            self.emit()
        self.phase_stack = None


class Buf:
    def __init__(self, fw, name, shape, dtype=F32, psum=False):
        self.t = fw.ps(name, shape, dtype) if psum else fw.sb(name, shape, dtype)
        self.k = Trk(name)

    def __getitem__(self, idx):
        return self.t[idx]


def rstd_from_ss(fw, out_buf, ss_ps, n, width, epsb):
    fw.op("act", lambda e: e.activation(out_buf[:, :width], ss_ps[:, :width], AF.Sqrt,
                                        bias=epsb[:, 0:1], scale=1.0 / n),
          reads=[ss_ps.k, epsb.k], writes=[out_buf.k])
    fw.op("dve", lambda e: e.reciprocal(out_buf[:, :width], out_buf[:, :width]),
          reads=[out_buf.k], writes=[out_buf.k])


def ffn_phase(fw, xT_in, xT_out, wg, wu, wd, gnorm, NT=512):
    with fw.phase():
        ones = Buf(fw, "ones", [128, 128])
        g_sb = Buf(fw, "g_sb", [128, 8])
        xt = [Buf(fw, f"xt{i}", [128, 8, NT]) for i in range(2)]
        xn = Buf(fw, "xn", [128, 8, NT])
        sq = [Buf(fw, f"sq{i}", [128, NT]) for i in range(2)]
        rstd = Buf(fw, "rstd", [128, NT])
        hid = Buf(fw, "hid", [128, NFC, NT])
        sg = [Buf(fw, f"sg{i}", [128, NT]) for i in range(2)]
        wgb = [Buf(fw, f"wgb{i}", [128, 8, 128]) for i in range(3)]
        wub = [Buf(fw, f"wub{i}", [128, 8, 128]) for i in range(3)]
        wdb = [Buf(fw, f"wdb{i}", [128, NFC, 128]) for i in range(4)]
        xo = [Buf(fw, f"xo{i}", [128, NT]) for i in range(2)]
        ss_ps = Buf(fw, "ss_ps", [128, NT], psum=True)
        gp = [Buf(fw, f"gp{i}", [128, NT], psum=True) for i in range(2)]
        up = [Buf(fw, f"up{i}", [128, NT], psum=True) for i in range(2)]
        op_ = [Buf(fw, f"op{i}", [128, NT], psum=True) for i in range(2)]

        fw.op("pool", lambda e: e.memset(ones[:, :], 1.0), writes=[ones.k])
        epsb = Buf(fw, "epsb", [128, 1])
        fw.op("pool", lambda e: e.memset(epsb[:, :], EPS), writes=[epsb.k])
        fw.dma("sp", lambda e: e.dma_start(out=g_sb[:, :], in_=gnorm), g_sb.k, writes=[g_sb.k])
        xin = xT_in.rearrange("(c p) t -> p c t", p=128)
        xout = xT_out.rearrange("(c p) t -> p c t", p=128)
        ntiles = T // NT
        wi = 0
        di = 0
        for it in range(ntiles):
            X = xt[it % 2]
            ts = slice(it * NT, (it + 1) * NT)
            fw.dma("sp", lambda e, X=X, ts=ts: e.dma_start(out=X[:, :, :], in_=xin[:, :, ts]),
                   X.k, writes=[X.k])
            for c in range(8):
                S = sq[c % 2]
                fw.op("act", lambda e, S=S, X=X, c=c: e.activation(S[:, :], X[:, c, :], AF.Square),
                      reads=[X.k], writes=[S.k])
                fw.op("pe", lambda e, S=S, c=c: e.matmul(ss_ps[:, :], ones[:, :], S[:, :],
                                                          start=(c == 0), stop=(c == 7)),
                      reads=[ones.k, S.k], writes=[ss_ps.k], pe_acc=(c > 0))
            rstd_from_ss(fw, rstd, ss_ps, D, NT, epsb)
            for c in range(8):
                fw.op("dve", lambda e, X=X, c=c: e.scalar_tensor_tensor(
                    xn[:, c, :], X[:, c, :], g_sb[:, c:c + 1], rstd[:, :], ALU.mult, ALU.mult),
                    reads=[X.k, g_sb.k, rstd.k], writes=[xn.k])
            for f in range(NFC):
                WG = wgb[wi % 3]
                WU = wub[wi % 3]
                wi += 1
                fw.dma("sp", lambda e, WG=WG, f=f: e.dma_start(out=WG[:, :, :], in_=wg[f]),
                       WG.k, writes=[WG.k])
                fw.dma("pool", lambda e, WU=WU, f=f: e.dma_start(out=WU[:, :, :], in_=wu[f]),
                       WU.k, writes=[WU.k])
                G = gp[f % 2]
                U = up[f % 2]
                for c in range(8):
                    fw.op("pe", lambda e, W=WG, G=G, c=c: e.matmul(
                        G[:, :], mm(W[:, c, :]), mm(xn[:, c, :]), start=(c == 0), stop=(c == 7)),
                        reads=[WG.k, xn.k], writes=[G.k], pe_acc=(c > 0))
                for c in range(8):
                    fw.op("pe", lambda e, W=WU, U=U, c=c: e.matmul(
                        U[:, :], mm(W[:, c, :]), mm(xn[:, c, :]), start=(c == 0), stop=(c == 7)),
                        reads=[WU.k, xn.k], writes=[U.k], pe_acc=(c > 0))
                SG = sg[f % 2]
                fw.op("act", lambda e, SG=SG, G=G: e.activation(SG[:, :], G[:, :], AF.Silu),
                      reads=[G.k], writes=[SG.k])
                fw.op("dve", lambda e, SG=SG, U=U, f=f: e.tensor_tensor(
                    hid[:, f, :], SG[:, :], U[:, :], ALU.mult),
                    reads=[SG.k, U.k], writes=[hid.k])
            for dc in range(8):
                W = wdb[di % 4]
                di += 1
                fw.dma("sp" if dc % 2 == 0 else "pool",
                       lambda e, W=W, dc=dc: e.dma_start(out=W[:, :, :], in_=wd[dc]),
                       W.k, writes=[W.k])
                O = op_[dc % 2]
                for f in range(NFC):
                    fw.op("pe", lambda e, W=W, O=O, f=f: e.matmul(
                        O[:, :], mm(W[:, f, :]), mm(hid[:, f, :]), start=(f == 0), stop=(f == NFC - 1)),
                        reads=[W.k, hid.k], writes=[O.k], pe_acc=(f > 0))
                XO = xo[dc % 2]
                fw.op("dve", lambda e, XO=XO, O=O, X=X, dc=dc: e.scalar_tensor_tensor(
                    XO[:, :], O[:, :], 0.5, X[:, dc, :], ALU.mult, ALU.add),
                    reads=[O.k, X.k], writes=[XO.k])
                fw.dma("sp", lambda e, XO=XO, dc=dc, ts=ts: e.dma_start(out=xout[:, dc, ts], in_=XO[:, :]),
                       XO.k, reads=[XO.k])


def mm_group(fw, O, out_ap, pairs, reads):
    n = len(pairs)
    for i, (l, r) in enumerate(pairs):
        fw.op("pe", lambda e, l=l, r=r, i=i: e.matmul(out_ap, mm(l), mm(r), start=(i == 0),
                                                       stop=(i == n - 1)),
              reads=reads, writes=[O.k], pe_acc=(i > 0))


def load(fw, q, B, out_ap, in_ap):
    fw.dma(q, lambda e: e.dma_start(out=out_ap, in_=in_ap), B.k, writes=[B.k])


def store(fw, q, B, out_ap, in_ap):
    fw.dma(q, lambda e: e.dma_start(out=out_ap, in_=in_ap), B.k, reads=[B.k])


def norm_tile(fw, X, hT, sq, ss_ps, rstd, ones, g_sb, epsb, NT):
    for c in range(8):
        S = sq[c % 2]
        fw.op("act", lambda e, S=S, c=c: e.activation(S[:, :], X[:, c, :], AF.Square),
              reads=[X.k], writes=[S.k])
        fw.op("pe", lambda e, S=S, c=c: e.matmul(ss_ps[:, :NT], ones[:, :], S[:, :],
                                                  start=(c == 0), stop=(c == 7)),
              reads=[ones.k, S.k], writes=[ss_ps.k], pe_acc=(c > 0))
    rstd_from_ss(fw, rstd, ss_ps, D, NT, epsb)
    for c in range(8):
        fw.op("dve", lambda e, c=c: e.scalar_tensor_tensor(
            hT[:, c, :], X[:, c, :], g_sb[:, c:c + 1], rstd[:, :NT], ALU.mult, ALU.mult),
            reads=[X.k, g_sb.k, rstd.k], writes=[hT.k])


def inproj_phase(fw, xT_in, w_in, ncols, gnorm, fm_groups, tm_groups, NT=512):
    with fw.phase():
        ones = Buf(fw, "ones", [128, 128])
        epsb = Buf(fw, "epsb", [128, 1])
        g_sb = Buf(fw, "g_sb", [128, 8])
        W = Buf(fw, "W", [128, 8, ncols])
        xt = [Buf(fw, f"xt{i}", [128, 8, NT]) for i in range(2)]
        hT = Buf(fw, "hT", [128, 8, NT])
        sq = [Buf(fw, f"sq{i}", [128, NT]) for i in range(2)]
        rstd = Buf(fw, "rstd", [128, NT])
        stf = [Buf(fw, f"stf{i}", [128, NT]) for i in range(3)]
        stt = [Buf(fw, f"stt{i}", [128, 512]) for i in range(3)]
        ss_ps = Buf(fw, "ss_ps", [128, NT], psum=True)
        fp = [Buf(fw, f"fp{i}", [128, NT], psum=True) for i in range(3)]
        tp = [Buf(fw, f"tp{i}", [128, 512], psum=True) for i in range(3)]
        fw.op("pool", lambda e: e.memset(ones[:, :], 1.0), writes=[ones.k])
        fw.op("pool", lambda e: e.memset(epsb[:, :], EPS), writes=[epsb.k])
        load(fw, "sp", g_sb, g_sb[:, :], gnorm)
        fw.dma("pool", [lambda e, c=c: e.dma_start(out=W[:, c, :], in_=w_in[:, c, :]) for c in range(8)],
               W.k, writes=[W.k])
        xin = xT_in.rearrange("(c p) t -> p c t", p=128)
        k = 0
        for it in range(T // NT):
            X = xt[it % 2]
            ts = slice(it * NT, (it + 1) * NT)
            load(fw, "sp", X, X[:, :, :], xin[:, :, ts])
            norm_tile(fw, X, hT, sq, ss_ps, rstd, ones, g_sb, epsb, NT)
            for (c0, wd_, dst) in fm_groups:
                P_ = fp[k % 3]
                S_ = stf[k % 3]
                mm_group(fw, P_, P_[:wd_, :], [(W[:, c, c0:c0 + wd_], hT[:, c, :]) for c in range(8)],
                         [W.k, hT.k])
                eng = "act" if k % 2 == 0 else "dve"
                if eng == "act":
                    fw.op("act", lambda e, P_=P_, S_=S_, wd_=wd_: e.copy(S_[:wd_, :], P_[:wd_, :]),
                          reads=[P_.k], writes=[S_.k])
                else:
                    fw.op("dve", lambda e, P_=P_, S_=S_, wd_=wd_: e.tensor_copy(S_[:wd_, :], P_[:wd_, :]),
                          reads=[P_.k], writes=[S_.k])
                store(fw, "sp", S_, dst[:, ts], S_[:wd_, :])
                k += 1
            for sub in range(NT // 128):
                t0 = it * NT + sub * 128
                for (c0, wd_, dst) in tm_groups:
                    P_ = tp[k % 3]
                    S_ = stt[k % 3]
                    mm_group(fw, P_, P_[:, :wd_],
                             [(hT[:, c, sub * 128:(sub + 1) * 128], W[:, c, c0:c0 + wd_]) for c in range(8)],
                             [W.k, hT.k])
                    if k % 2 == 0:
                        fw.op("act", lambda e, P_=P_, S_=S_, wd_=wd_: e.copy(S_[:, :wd_], P_[:, :wd_]),
                              reads=[P_.k], writes=[S_.k])
                    else:
                        fw.op("dve", lambda e, P_=P_, S_=S_, wd_=wd_: e.tensor_copy(S_[:, :wd_], P_[:, :wd_]),
                              reads=[P_.k], writes=[S_.k])
                    store(fw, "pool", S_, dst[t0:t0 + 128, :], S_[:, :wd_])
                    k += 1


def outproj_phase(fw, xT_in, xT_out, yT, w_out, NT=512):
    with fw.phase():
        W = Buf(fw, "W", [128, 8, 1024])
        xt = [Buf(fw, f"xt{i}", [128, 8, NT]) for i in range(2)]
        yt = [Buf(fw, f"yt{i}", [128, 8, NT]) for i in range(2)]
        xo = [Buf(fw, f"xo{i}", [128, 8, NT]) for i in range(2)]
        op_ = [Buf(fw, f"op{i}", [128, NT], psum=True) for i in range(3)]
        fw.dma("pool", [lambda e, c=c: e.dma_start(out=W[:, c, :], in_=w_out[:, c, :]) for c in range(8)],
               W.k, writes=[W.k])
        xin = xT_in.rearrange("(c p) t -> p c t", p=128)
        yin = yT.rearrange("(c p) t -> p c t", p=128)
        xout = xT_out.rearrange("(c p) t -> p c t", p=128)
        k = 0
        for it in range(T // NT):
            X = xt[it % 2]
            Y = yt[it % 2]
            XO = xo[it % 2]
            ts = slice(it * NT, (it + 1) * NT)
            load(fw, "sp", X, X[:, :, :], xin[:, :, ts])
            load(fw, "pool", Y, Y[:, :, :], yin[:, :, ts])
            for dc in range(8):
                O = op_[k % 3]
                k += 1
                mm_group(fw, O, O[:, :], [(W[:, c, dc * 128:(dc + 1) * 128], Y[:, c, :]) for c in range(8)],
                         [W.k, Y.k])
                fw.op("dve", lambda e, O=O, X=X, XO=XO, dc=dc: e.tensor_tensor(
                    XO[:, dc, :], O[:, :], X[:, dc, :], ALU.add),
                    reads=[O.k, X.k], writes=[XO.k])
            store(fw, "sp", XO, xout[:, :, ts], XO[:, :, :])


def gla_consts():
    s = np.arange(128)[:, None]
    t = np.arange(128)[None, :]
    same = (s // 64) == (t // 64)
    Mc = np.zeros((128, 130), np.float32)
    Mc[:, :128] = np.where(same & (s <= t), -1.0 / 16, 0.0)
    Mc[:64, 128] = -1.0 / 16
    Mc[64:, 129] = -1.0 / 16
    M3 = np.where(same & (s > t), -1.0 / 16, 0.0).astype(np.float32)
    maskA = np.where(same & (s <= t), 1.0, 0.0).astype(np.float32)
    return Mc, M3, maskA


def gla_phase(fw, qT, kT, gT, lrT, k_tm, v_tm, yT, Mc_d, M3_d, maskA_d, wgk1_d, gnorm_d):
    with fw.phase():
        ones = Buf(fw, "ones", [128, 128])
        epsb = Buf(fw, "epsb", [128, 1])
        Mc = Buf(fw, "Mc", [128, 130])
        M3 = Buf(fw, "M3", [128, 128])
        mA = Buf(fw, "mA", [128, 128])
        wgk = Buf(fw, "wgk", [32, 256])
        wn = Buf(fw, "wn", [128, 1])
        qt = [Buf(fw, f"qt{i}", [128, 2, 128]) for i in range(2)]
        kt = [Buf(fw, f"kt{i}", [128, 2, 128]) for i in range(2)]
        gt = [Buf(fw, f"gt{i}", [128, 4, 128]) for i in range(2)]
        lr = [Buf(fw, f"lr{i}", [32, 128]) for i in range(2)]
        ktm = [Buf(fw, f"ktm{i}", [128, 256]) for i in range(2)]
        vtm = [Buf(fw, f"vtm{i}", [128, 512]) for i in range(2)]
        e1 = Buf(fw, "e1", [128, 256])
        sp_ = Buf(fw, "sp_", [128, 256])
        eG = Buf(fw, "eG", [128, 2, 130])
        enG = Buf(fw, "enG", [128, 2, 128])
        eD = Buf(fw, "eD", [128, 256])
        qd = Buf(fw, "qd", [128, 2, 128])
        kd = Buf(fw, "kd", [128, 2, 128])
        kk = Buf(fw, "kk", [128, 256])
        ATm = Buf(fw, "ATm", [128, 4, 128])
        oxs = Buf(fw, "oxs", [128, 4, 128])
        oT = Buf(fw, "oT", [128, 4, 128])
        sqo = Buf(fw, "sqo", [128, 4, 128])
        rstd = Buf(fw, "rstd", [128, 512])
        sg = Buf(fw, "sg", [128, 4, 128])
        ya = [Buf(fw, f"ya{i}", [128, 4, 128]) for i in range(2)]
        S = [Buf(fw, f"S{i}", [128, 2, 128]) for i in range(2)]
        zd_ps = Buf(fw, "zd_ps", [128, 512], psum=True)
        d_ps = Buf(fw, "d_ps", [128, 512], psum=True)
        gt_ps = Buf(fw, "gt_ps", [128, 2, 256], psum=True)
        at_ps = Buf(fw, "at_ps", [128, 4, 128], psum=True)
        oi_ps = Buf(fw, "oi_ps", [128, 4, 128], psum=True)
        ox_ps = Buf(fw, "ox_ps", [128, 4, 128], psum=True)
        st_ps = [Buf(fw, f"st_ps{i}", [128, 2, 256], psum=True) for i in range(2)]
        fw.op("pool", lambda e: e.memset(ones[:, :], 1.0), writes=[ones.k])
        fw.op("pool", lambda e: e.memset(epsb[:, :], EPS), writes=[epsb.k])
        for i in range(2):
            fw.op("pool", lambda e, i=i: e.memset(lr[i][:, :], 1.0), writes=[lr[i].k])
            fw.op("pool", lambda e, i=i: e.memset(S[i][:, :, :], 0.0), writes=[S[i].k])
        load(fw, "sp", Mc, Mc[:, :], Mc_d)
        load(fw, "sp", M3, M3[:, :], M3_d)
        load(fw, "sp", mA, mA[:, :], maskA_d)
        load(fw, "sp", wgk, wgk[0:17, :], wgk1_d)
        load(fw, "sp", wn, wn[:, :], gnorm_d)
        qTr = qT.rearrange("(c p) t -> p c t", p=128)
        kTr = kT.rearrange("(c p) t -> p c t", p=128)
        gTr = gT.rearrange("(c p) t -> p c t", p=128)
        yTr = yT.rearrange("(c p) t -> p c t", p=128)
        for it in range(T // 128):
            ts = slice(it * 128, (it + 1) * 128)
            b = it % 2
            Q, K_, G_, L, KT, VT = qt[b], kt[b], gt[b], lr[b], ktm[b], vtm[b]
            load(fw, "sp", Q, Q[:, :, :], qTr[:, :, ts])
            load(fw, "sp", K_, K_[:, :, :], kTr[:, :, ts])
            load(fw, "sp", G_, G_[:, :, :], gTr[:, :, ts])
            load(fw, "pool", L, L[0:16, :], lrT[:, ts])
            load(fw, "pool", KT, KT[:, :], k_tm[ts, :])
            load(fw, "pool", VT, VT[:, :], v_tm[ts, :])
            mm_group(fw, zd_ps, zd_ps[:, 0:256], [(L[0:17, :], wgk[0:17, :])], [L.k, wgk.k])
            fw.op("act", lambda e: e.activation(e1[:, :], zd_ps[:, 0:256], AF.Exp, scale=-1.0),
                  reads=[zd_ps.k], writes=[e1.k])
            fw.op("act", lambda e: e.activation(sp_[:, :], e1[:, :], AF.Ln, bias=1.0),
                  reads=[e1.k], writes=[sp_.k])
            for c in range(2):
                mm_group(fw, gt_ps, gt_ps[:, c, 0:130], [(sp_[:, c * 128:(c + 1) * 128], Mc[:, :])],
                         [sp_.k, Mc.k])
            mm_group(fw, d_ps, d_ps[:, 0:256], [(M3[:, :], sp_[:, :])], [M3.k, sp_.k])
            fw.op("act", lambda e: e.activation(eG[:, :, :], gt_ps[:, :, 0:130], AF.Exp),
                  reads=[gt_ps.k], writes=[eG.k])
            fw.op("act", lambda e: e.activation(enG[:, :, :], gt_ps[:, :, 0:128], AF.Exp, scale=-1.0),
                  reads=[gt_ps.k], writes=[enG.k])
            fw.op("act", lambda e: e.activation(eD[:, :], d_ps[:, 0:256], AF.Exp),
                  reads=[d_ps.k], writes=[eD.k])
            fw.op("dve", lambda e, Q=Q: e.scalar_tensor_tensor(
                qd[:, :, :], Q[:, :, :], 0.125, eG[:, :, 0:128], ALU.mult, ALU.mult),
                reads=[Q.k, eG.k], writes=[qd.k])
            fw.op("dve", lambda e, K_=K_: e.tensor_tensor(kd[:, :, :], K_[:, :, :], enG[:, :, :], ALU.mult),
                  reads=[K_.k, enG.k], writes=[kd.k])
            fw.op("dve", lambda e, KT=KT: e.tensor_tensor(kk[:, :], KT[:, :], eD[:, :], ALU.mult),
                  reads=[KT.k, eD.k], writes=[kk.k])
            for h in range(4):
                c, pb = h // 2, (h % 2) * 64
                mm_group(fw, at_ps, at_ps[:, h, :], [(kd[pb:pb + 64, c, :], qd[pb:pb + 64, c, :])],
                         [kd.k, qd.k])
            fw.op("dve", lambda e: e.tensor_tensor(
                ATm[:, :, :], at_ps[:, :, :], mA[:, :].unsqueeze(1).to_broadcast([128, 4, 128]), ALU.mult),
                reads=[at_ps.k, mA.k], writes=[ATm.k])
            for h in range(4):
                mm_group(fw, oi_ps, oi_ps[:, h, :], [(VT[:, h * 128:(h + 1) * 128], ATm[:, h, :])],
                         [VT.k, ATm.k])
            for cc in range(2):
                Sc, Sn = S[cc], S[1 - cc]
                for h in range(4):
                    c, pb = h // 2, (h % 2) * 64
                    mm_group(fw, ox_ps, ox_ps[:, h, cc * 64:(cc + 1) * 64],
                             [(Sc[pb:pb + 64, c, :], qd[pb:pb + 64, c, cc * 64:(cc + 1) * 64])],
                             [Sc.k, qd.k])
                STP = st_ps[cc]
                for c in range(2):
                    mm_group(fw, STP, STP[:, c, :],
                             [(kk[cc * 64:(cc + 1) * 64, c * 128:(c + 1) * 128],
                               VT[cc * 64:(cc + 1) * 64, c * 256:(c + 1) * 256])], [kk.k, VT.k])
                for h in range(4):
                    c, pb = h // 2, (h % 2) * 64
                    fw.op("dve", lambda e, Sc=Sc, Sn=Sn, STP=STP, c=c, pb=pb, h=h, cc=cc:
                          e.scalar_tensor_tensor(
                              Sn[pb:pb + 64, c, :], Sc[pb:pb + 64, c, :], eG[pb:pb + 64, c, 128 + cc:129 + cc],
                              STP[pb:pb + 64, c, (h % 2) * 128:(h % 2) * 128 + 128], ALU.mult, ALU.add),
                          reads=[Sc.k, eG.k, STP.k], writes=[Sn.k])
            fw.op("act", lambda e: e.copy(oxs[:, :, :], ox_ps[:, :, :]), reads=[ox_ps.k], writes=[oxs.k])
            fw.op("dve", lambda e: e.tensor_tensor(oT[:, :, :], oi_ps[:, :, :], oxs[:, :, :], ALU.add),
                  reads=[oi_ps.k, oxs.k], writes=[oT.k])
            fw.op("act", lambda e: e.activation(sqo[:, :, :], oT[:, :, :], AF.Square),
                  reads=[oT.k], writes=[sqo.k])
            mm_group(fw, zd_ps, zd_ps[:, :], [(ones[:, :], sqo[:, :, :].rearrange("p h t -> p (h t)"))],
                     [ones.k, sqo.k])
            rstd_from_ss(fw, rstd, zd_ps, 128, 512, epsb)
            fw.op("act", lambda e, G_=G_: e.activation(sg[:, :, :], G_[:, :, :], AF.Silu),
                  reads=[G_.k], writes=[sg.k])
            YA = ya[b]
            fw.op("dve", lambda e, YA=YA: e.scalar_tensor_tensor(
                YA[:, :, :].rearrange("p h t -> p (h t)"), oT[:, :, :].rearrange("p h t -> p (h t)"),
                wn[:, 0:1], rstd[:, :], ALU.mult, ALU.mult),
                reads=[oT.k, wn.k, rstd.k], writes=[YA.k])
            fw.op("dve", lambda e, YA=YA: e.tensor_tensor(YA[:, :, :], YA[:, :, :], sg[:, :, :], ALU.mult),
                  reads=[YA.k, sg.k], writes=[YA.k])
            store(fw, "sp", YA, yTr[:, :, ts], YA[:, :, :])


def pool_consts():
    s = np.arange(128)[:, None]
    t = np.arange(128)[None, :]
    cur = np.zeros((4, 128, 128), np.float32)
    prev = np.zeros((4, 128, 128), np.float32)
    cur0 = np.zeros((4, 128, 128), np.float32)
    for g, w in enumerate((2, 4, 8, 16)):
        d = t - s
        cur[g] = np.where((d >= 0) & (d < w), 1.0 / w, 0.0) - np.eye(128)
        cnt = np.minimum(t + 1, w).astype(np.float64)
        cur0[g] = np.where((d >= 0) & (d < w), 1.0 / cnt, 0.0) - np.eye(128)
        d2 = t + 128 - s
        prev[g] = np.where((d2 >= 0) & (d2 < w), 1.0 / w, 0.0)
    return cur.astype(np.float32), prev.astype(np.float32), cur0.astype(np.float32)


def pool_phase(fw, u_tm, yT, wpool_d, pscale_d, cur_d, prev_d, cur0_d):
    with fw.phase():
        wp = Buf(fw, "wp", [128, 4, 128])
        psc = Buf(fw, "psc", [128, 4])
        Pc = Buf(fw, "Pc", [128, 4, 128])
        Pp = Buf(fw, "Pp", [128, 4, 128])
        P0 = Buf(fw, "P0", [128, 4, 128])
        ut = [Buf(fw, f"ut{i}", [128, 512]) for i in range(3)]
        pl = Buf(fw, "pl", [128, 4, 128])
        yb = [Buf(fw, f"yb{i}", [128, 4, 128]) for i in range(2)]
        pt_ps = [Buf(fw, f"pt_ps{i}", [128, 4, 128], psum=True) for i in range(2)]
        y_ps = [Buf(fw, f"y_ps{i}", [128, 4, 128], psum=True) for i in range(2)]
        load(fw, "sp", wp, wp[:, :, :], wpool_d)
        load(fw, "sp", psc, psc[:, :], pscale_d)
        load(fw, "sp", Pc, Pc[:, :, :], cur_d)
        load(fw, "sp", Pp, Pp[:, :, :], prev_d)
        load(fw, "sp", P0, P0[:, :, :], cur0_d)
        yTr = yT.rearrange("(c p) t -> p c t", p=128)
        for it in range(T // 128):
            ts = slice(it * 128, (it + 1) * 128)
            U = ut[it % 3]
            Uprev = ut[(it - 1) % 3]
            load(fw, "sp", U, U[:, :], u_tm[ts, :])
            PT = pt_ps[it % 2]
            for g in range(4):
                gs = slice(g * 128, (g + 1) * 128)
                if it == 0:
                    mm_group(fw, PT, PT[:, g, :], [(U[:, gs], P0[:, g, :])], [U.k, P0.k])
                else:
                    mm_group(fw, PT, PT[:, g, :], [(U[:, gs], Pc[:, g, :]), (Uprev[:, gs], Pp[:, g, :])],
                             [U.k, Uprev.k, Pc.k, Pp.k])
            fw.op("act", lambda e, PT=PT: e.copy(pl[:, :, :], PT[:, :, :]), reads=[PT.k], writes=[pl.k])
            YP = y_ps[it % 2]
            for g in range(4):
                mm_group(fw, YP, YP[:, g, :], [(wp[:, g, :], pl[:, g, :])], [wp.k, pl.k])
            YB = yb[it % 2]
            fw.op("dve", lambda e, YB=YB, YP=YP: e.tensor_tensor(
                YB[:, :, :], YP[:, :, :], psc[:, :].unsqueeze(2).to_broadcast([128, 4, 128]), ALU.mult),
                reads=[YP.k, psc.k], writes=[YB.k])
            store(fw, "pool", YB, yTr[:, :, ts], YB[:, :, :])


def conv_phase(fw, xbcT, xcT, xB_tm, cw_d, cb_d, ident_d, NT=512):
    with fw.phase():
        cw = Buf(fw, "cw", [128, 8, 4])
        cb = Buf(fw, "cb", [128, 8])
        idn = Buf(fw, "idn", [128, 128])
        xt = [Buf(fw, f"xt{i}", [128, NT + 3]) for i in range(3)]
        acc = [Buf(fw, f"acc{i}", [128, NT]) for i in range(2)]
        xc = [Buf(fw, f"xc{i}", [128, NT]) for i in range(3)]
        s2 = [Buf(fw, f"s2{i}", [128, 4, 128]) for i in range(3)]
        tr_ps = [Buf(fw, f"tr_ps{i}", [128, 4, 128], psum=True) for i in range(2)]
        load(fw, "sp", cw, cw[:, :, :], cw_d)
        load(fw, "sp", cb, cb[:, :], cb_d)
        load(fw, "sp", idn, idn[:, :], ident_d)
        k = 0
        for it in range(T // NT):
            t0 = it * NT
            for c in range(8):
                X = xt[k % 3]
                A = acc[k % 2]
                XC = xc[k % 3]
                rows = slice(c * 128, (c + 1) * 128)
                if it == 0:
                    fw.op("pool", lambda e, X=X: e.memset(X[:, 0:3], 0.0), writes=[X.k])
                    load(fw, "sp", X, X[:, 3:NT + 3], xbcT[rows, 0:NT])
                else:
                    load(fw, "sp", X, X[:, :], xbcT[rows, t0 - 3:t0 + NT])
                fw.op("dve", lambda e, X=X, A=A, c=c: e.tensor_scalar(
                    A[:, :], X[:, 0:NT], cw[:, c, 0:1], None, ALU.mult), reads=[X.k, cw.k], writes=[A.k])
                for j in range(1, 4):
                    fw.op("dve", lambda e, X=X, A=A, c=c, j=j: e.scalar_tensor_tensor(
                        A[:, :], X[:, j:j + NT], cw[:, c, j:j + 1], A[:, :], ALU.mult, ALU.add),
                        reads=[X.k, cw.k, A.k], writes=[A.k])
                fw.op("act", lambda e, A=A, XC=XC, c=c: e.activation(
                    XC[:, :], A[:, :], AF.Silu, bias=cb[:, c:c + 1]), reads=[A.k, cb.k], writes=[XC.k])
                store(fw, "pool", XC, xcT[rows, t0:t0 + NT], XC[:, :])
                if c < 6:
                    TP = tr_ps[k % 2]
                    for sub in range(4):
                        fw.op("pe", lambda e, TP=TP, XC=XC, sub=sub: e.transpose(
                            TP[:, sub, :], XC[:, sub * 128:(sub + 1) * 128], idn[:, :]),
                            reads=[XC.k, idn.k], writes=[TP.k])
                    S2 = s2[k % 3]
                    fw.op("act", lambda e, TP=TP, S2=S2: e.copy(S2[:, :, :], TP[:, :, :]),
                          reads=[TP.k], writes=[S2.k])
                    store(fw, "sp", S2, xB_tm[t0:t0 + NT, c * 128:(c + 1) * 128].rearrange(
                        "(s p) v -> p s v", p=128), S2[:, :, :])
                k += 1


def ssd_consts():
    t = np.arange(128)[:, None]
    s = np.arange(128)[None, :]
    U = (t > s).astype(np.float32)
    Tri = (t <= s).astype(np.float32)
    NEG = np.where(s >= t, 0.0, -30000.0).astype(np.float32)
    return U, Tri, NEG


def ssd_phase(fw, xcT, xB_tm, z_tm, dt_tm, yT, U_d, Tri_d, NEG_d, ident_d, dtb_d, alog_d, dsk_d, wn_d):
    with fw.phase():
        ones = Buf(fw, "ones", [128, 128])
        epsb = Buf(fw, "epsb", [128, 1])
        U = Buf(fw, "U", [128, 128])
        Tri = Buf(fw, "Tri", [128, 128])
        NEG = Buf(fw, "NEG", [128, 128])
        idn = Buf(fw, "idn", [128, 128])
        dtb = Buf(fw, "dtb", [128, 8])
        An = Buf(fw, "An", [128, 8])
        dsk = Buf(fw, "dsk", [128, 8])
        wn = Buf(fw, "wn", [128, 512])
        BT = [Buf(fw, f"BT{i}", [128, 2, 128]) for i in range(2)]
        CT = [Buf(fw, f"CT{i}", [128, 2, 128]) for i in range(2)]
        XB = [Buf(fw, f"XB{i}", [128, 768]) for i in range(2)]
        Z = [Buf(fw, f"Z{i}", [128, 512]) for i in range(2)]
        DT = [Buf(fw, f"DT{i}", [128, 8]) for i in range(2)]
        dt = Buf(fw, "dt", [128, 8])
        a = Buf(fw, "a", [128, 8])
        e3 = Buf(fw, "e3", [128, 3, 8])
        xdt = Buf(fw, "xdt", [128, 8, 64])
        xdt2 = Buf(fw, "xdt2", [128, 8, 64])
        CBT = Buf(fw, "CBT", [128, 2, 128])
        lh = [Buf(fw, f"lh{i}", [128, 128]) for i in range(8)]
        Lh = [Buf(fw, f"Lh{i}", [128, 512]) for i in range(2)]
        Wh = [Buf(fw, f"Wh{i}", [128, 512]) for i in range(2)]
        yo = Buf(fw, "yo", [128, 8, 64])
        y = Buf(fw, "y", [128, 8, 64])
        sz = Buf(fw, "sz", [128, 512])
        junk = Buf(fw, "junk", [128, 256])
        ssq = Buf(fw, "ssq", [128, 2])
        rs = Buf(fw, "rs", [128, 2])
        yc = [Buf(fw, f"yc{i}", [128, 4, 128]) for i in range(2)]
        ST = [Buf(fw, f"ST{i}", [128, 4, 64]) for i in range(2)]
        sm_ps = Buf(fw, "sm_ps", [128, 3, 8], psum=True)
        cb_ps = Buf(fw, "cb_ps", [128, 2, 128], psum=True)
        dm_ps = [Buf(fw, f"dm_ps{i}", [128, 512], psum=True) for i in range(2)]
        yd_ps = Buf(fw, "yd_ps", [128, 8, 64], psum=True)
        yo_ps = Buf(fw, "yo_ps", [128, 8, 64], psum=True)
        st_ps = Buf(fw, "st_ps", [128, 2, 256], psum=True)
        tr_ps = Buf(fw, "tr_ps", [128, 4, 128], psum=True)
        fw.op("pool", lambda e: e.memset(ones[:, :], 1.0), writes=[ones.k])
        fw.op("pool", lambda e: e.memset(epsb[:, :], EPS), writes=[epsb.k])
        for i in range(2):
            fw.op("pool", lambda e, i=i: e.memset(ST[i][:, :, :], 0.0), writes=[ST[i].k])
        load(fw, "sp", U, U[:, :], U_d)
        load(fw, "sp", Tri, Tri[:, :], Tri_d)
        load(fw, "sp", NEG, NEG[:, :], NEG_d)
        load(fw, "sp", idn, idn[:, :], ident_d)
        load(fw, "sp", dtb, dtb[:, :], dtb_d.partition_broadcast(128))
        load(fw, "sp", An, An[:, :], alog_d.partition_broadcast(128))
        load(fw, "sp", dsk, dsk[:, :], dsk_d.partition_broadcast(128))
        load(fw, "sp", wn, wn[:, :], wn_d.partition_broadcast(128))
        fw.op("act", lambda e: e.activation(An[:, :], An[:, :], AF.Exp), reads=[An.k], writes=[An.k])
        fw.op("dve", lambda e: e.tensor_scalar(An[:, :], An[:, :], -1.0, None, ALU.mult),
              reads=[An.k], writes=[An.k])
        BTr = xcT[512:768].rearrange("(g p) t -> p g t", p=128)
        CTr = xcT[768:1024].rearrange("(g p) t -> p g t", p=128)
        yTr = yT.rearrange("(c p) t -> p c t", p=128)

        def bc8(ap):
            return ap.unsqueeze(2).to_broadcast([128, 8, 64])

        for n in range(T // 128):
            ts = slice(n * 128, (n + 1) * 128)
            b = n % 2
            B_, C_, X_, Z_, D_ = BT[b], CT[b], XB[b], Z[b], DT[b]
            load(fw, "sp", B_, B_[:, :, :], BTr[:, :, ts])
            load(fw, "sp", C_, C_[:, :, :], CTr[:, :, ts])
            load(fw, "pool", X_, X_[:, :], xB_tm[ts, :])
            load(fw, "pool", Z_, Z_[:, :], z_tm[ts, :])
            load(fw, "pool", D_, D_[:, :], dt_tm[ts, :])
            x3 = X_[:, 0:512].rearrange("p (h d) -> p h d", h=8)
            fw.op("dve", lambda e, D_=D_: e.tensor_tensor(dt[:, :], D_[:, :], dtb[:, :], ALU.add),
                  reads=[D_.k, dtb.k], writes=[dt.k])
            fw.op("act", lambda e: e.activation(dt[:, :], dt[:, :], AF.Exp), reads=[dt.k], writes=[dt.k])
            fw.op("act", lambda e: e.activation(dt[:, :], dt[:, :], AF.Ln, bias=1.0), reads=[dt.k], writes=[dt.k])
            fw.op("dve", lambda e: e.tensor_tensor(a[:, :], dt[:, :], An[:, :], ALU.mult),
                  reads=[dt.k, An.k], writes=[a.k])
            mm_group(fw, sm_ps, sm_ps[:, 0, :], [(Tri[:, :], a[:, :])], [Tri.k, a.k])
            mm_group(fw, sm_ps, sm_ps[:, 1, :], [(ones[:, :], a[:, :])], [ones.k, a.k])
            mm_group(fw, sm_ps, sm_ps[:, 2, :], [(U[:, :], a[:, :])], [U.k, a.k])
            fw.op("act", lambda e: e.activation(e3[:, :, :], sm_ps[:, :, :], AF.Exp),
                  reads=[sm_ps.k], writes=[e3.k])
            fw.op("dve", lambda e, x3=x3, X_=X_: e.tensor_tensor(xdt[:, :, :], x3, bc8(dt[:, :]), ALU.mult),
                  reads=[X_.k, dt.k], writes=[xdt.k])
            fw.op("dve", lambda e: e.tensor_tensor(xdt2[:, :, :], xdt[:, :, :], bc8(e3[:, 2, :]), ALU.mult),
                  reads=[xdt.k, e3.k], writes=[xdt2.k])
            for g in range(2):
                mm_group(fw, cb_ps, cb_ps[:, g, :], [(B_[:, g, :], C_[:, g, :])], [B_.k, C_.k])
            fw.op("act", lambda e: e.copy(CBT[:, :, :], cb_ps[:, :, :]), reads=[cb_ps.k], writes=[CBT.k])
            for h in range(8):
                LH = lh[h]
                fw.op("dve", lambda e, LH=LH, h=h: e.tensor_scalar(
                    LH[:, :], U[:, :], a[:, h:h + 1], None, ALU.mult), reads=[U.k, a.k], writes=[LH.k])
            for h in range(8):
                DM = dm_ps[(h // 4) % 2]
                dm = DM[:, (h % 4) * 128:(h % 4 + 1) * 128]
                mm_group(fw, DM, dm, [(lh[h][:, :], Tri[:, :]), (idn[:, :], NEG[:, :])],
                         [lh[h].k, Tri.k, idn.k, NEG.k])
            for hh in range(2):
                DM = dm_ps[hh]
                fw.op("act", lambda e, DM=DM, hh=hh: e.activation(
                    Lh[hh][:, :], DM[:, :], AF.Exp), reads=[DM.k], writes=[Lh[hh].k])
                fw.op("dve", lambda e, hh=hh: e.tensor_tensor(
                    Wh[hh][:, :].rearrange("p (h l) -> p h l", h=4),
                    Lh[hh][:, :].rearrange("p (h l) -> p h l", h=4),
                    CBT[:, hh, :].unsqueeze(1).to_broadcast([128, 4, 128]), ALU.mult),
                    reads=[Lh[hh].k, CBT.k], writes=[Wh[hh].k])
            for h in range(8):
                WW = Wh[h // 4]
                mm_group(fw, yd_ps, yd_ps[:, h, :], [(WW[:, (h % 4) * 128:(h % 4 + 1) * 128], xdt[:, h, :])],
                         [WW.k, xdt.k])
            for g in range(2):
                mm_group(fw, yo_ps, yo_ps[:, g * 4:(g + 1) * 4, :].rearrange("p h d -> p (h d)"),
                         [(C_[:, g, :], ST[g][:, :, :].rearrange("p h d -> p (h d)"))], [C_.k, ST[g].k])
            fw.op("dve", lambda e: e.tensor_tensor(yo[:, :, :], yo_ps[:, :, :], bc8(e3[:, 0, :]), ALU.mult),
                  reads=[yo_ps.k, e3.k], writes=[yo.k])
            fw.op("dve", lambda e: e.tensor_tensor(y[:, :, :], yd_ps[:, :, :], yo[:, :, :], ALU.add),
                  reads=[yd_ps.k, yo.k], writes=[y.k])
            fw.op("dve", lambda e, x3=x3, X_=X_: e.tensor_tensor(yo[:, :, :], x3, bc8(dsk[:, :]), ALU.mult),
                  reads=[X_.k, dsk.k], writes=[yo.k])
            fw.op("dve", lambda e: e.tensor_tensor(y[:, :, :], y[:, :, :], yo[:, :, :], ALU.add),
                  reads=[y.k, yo.k], writes=[y.k])
            fw.op("act", lambda e, Z_=Z_: e.activation(sz[:, :], Z_[:, :], AF.Silu), reads=[Z_.k], writes=[sz.k])
            y2 = y[:, :, :].rearrange("p h d -> p (h d)")
            fw.op("dve", lambda e, y2=y2: e.tensor_tensor(y2, y2, sz[:, :], ALU.mult),
                  reads=[y.k, sz.k], writes=[y.k])
            fw.op("pool", lambda e: e.memset(ssq[:, :], 0.0), writes=[ssq.k])
            for g in range(2):
                fw.op("act", lambda e, g=g, y2=y2: e.activation(
                    junk[:, :], y2[:, g * 256:(g + 1) * 256], AF.Square, accum_out=ssq[:, g:g + 1]),
                    reads=[y.k], writes=[junk.k, ssq.k])
            rstd_from_ss(fw, rs, ssq, 256, 2, epsb)
            for g in range(2):
                fw.op("dve", lambda e, g=g, y2=y2: e.scalar_tensor_tensor(
                    y2[:, g * 256:(g + 1) * 256], y2[:, g * 256:(g + 1) * 256], rs[:, g:g + 1],
                    wn[:, g * 256:(g + 1) * 256], ALU.mult, ALU.mult),
                    reads=[y.k, rs.k, wn.k], writes=[y.k])
            for c in range(4):
                fw.op("pe", lambda e, c=c, y2=y2: e.transpose(tr_ps[:, c, :], y2[:, c * 128:(c + 1) * 128],
                                                               idn[:, :]),
                      reads=[y.k, idn.k], writes=[tr_ps.k])
            YC = yc[b]
            fw.op("act", lambda e, YC=YC: e.copy(YC[:, :, :], tr_ps[:, :, :]), reads=[tr_ps.k], writes=[YC.k])
            store(fw, "sp", YC, yTr[:, :, ts], YC[:, :, :])
            for g in range(2):
                mm_group(fw, st_ps, st_ps[:, g, :],
                         [(X_[:, 512 + g * 128:512 + (g + 1) * 128],
                           xdt2[:, g * 4:(g + 1) * 4, :].rearrange("p h d -> p (h d)"))], [X_.k, xdt2.k])
            for g in range(2):
                fw.op("dve", lambda e, g=g: e.tensor_tensor(
                    ST[g][:, :, :], ST[g][:, :, :],
                    e3[:, 1, g * 4:(g + 1) * 4].unsqueeze(2).to_broadcast([128, 4, 64]), ALU.mult),
                    reads=[ST[g].k, e3.k], writes=[ST[g].k])
                fw.op("dve", lambda e, g=g: e.tensor_tensor(
                    ST[g][:, :, :].rearrange("p h d -> p (h d)"),
                    ST[g][:, :, :].rearrange("p h d -> p (h d)"), st_ps[:, g, :], ALU.add),
                    reads=[ST[g].k, st_ps.k], writes=[ST[g].k])


def qknorm_phase(fw, qT, kT, qnT, knT, wq_d, wk_d, bones_d, NT=512):
    with fw.phase():
        bo = Buf(fw, "bo", [128, 128])
        epsb = Buf(fw, "epsb", [128, 1])
        wq = Buf(fw, "wq", [128, 1])
        wk = Buf(fw, "wk", [128, 1])
        xt = [Buf(fw, f"xt{i}", [128, NT]) for i in range(3)]
        sq = [Buf(fw, f"sq{i}", [128, NT]) for i in range(2)]
        rstd = [Buf(fw, f"rstd{i}", [128, NT]) for i in range(2)]
        xo = [Buf(fw, f"xo{i}", [128, NT]) for i in range(3)]
        ss_ps = [Buf(fw, f"ss_ps{i}", [128, NT], psum=True) for i in range(2)]
        fw.op("pool", lambda e: e.memset(epsb[:, :], EPS), writes=[epsb.k])
        load(fw, "sp", bo, bo[:, :], bones_d)
        load(fw, "sp", wq, wq[:, :], wq_d)
        load(fw, "sp", wk, wk[:, :], wk_d)
        fw.op("dve", lambda e: e.tensor_scalar(wq[:, :], wq[:, :], 0.125, None, ALU.mult),
              reads=[wq.k], writes=[wq.k])
        k = 0
        for (src, dst, w) in ((qT, qnT, wq), (kT, knT, wk)):
            for it in range(T // NT):
                ts = slice(it * NT, (it + 1) * NT)
                for c in range(4):
                    X, S_, R_, XO, SS = xt[k % 3], sq[k % 2], rstd[k % 2], xo[k % 3], ss_ps[k % 2]
                    k += 1
                    rows = slice(c * 128, (c + 1) * 128)
                    load(fw, "sp", X, X[:, :], src[rows, ts])
                    fw.op("act", lambda e, X=X, S_=S_: e.activation(S_[:, :], X[:, :], AF.Square),
                          reads=[X.k], writes=[S_.k])
                    mm_group(fw, SS, SS[:, :], [(bo[:, :], S_[:, :])], [bo.k, S_.k])
                    rstd_from_ss(fw, R_, SS, 64, NT, epsb)
                    fw.op("dve", lambda e, X=X, XO=XO, R_=R_, w=w: e.scalar_tensor_tensor(
                        XO[:, :], X[:, :], w[:, 0:1], R_[:, :], ALU.mult, ALU.mult),
                        reads=[X.k, w.k, R_.k], writes=[XO.k])
                    store(fw, "pool", XO, dst[rows, ts], XO[:, :])


def t5_consts():
    k = np.arange(128)[:, None]
    q = np.arange(128)[None, :]
    E = np.zeros((33, 2, 128, 128), np.float32)
    for blk, off in ((0, 0), (1, 128)):
        n = q - k + off
        valid = n >= 0
        nn = np.maximum(n, 0)
        nf = np.maximum(nn, 1).astype(np.float32)
        large = 16 + (np.log(nf / np.float32(16)) / np.float32(np.log(128 / 16)) * np.float32(16)).astype(np.int32)
        large = np.minimum(large, 31)
        bucket = np.where(nn < 16, nn, large)
        for b in range(32):
            E[b, blk] = ((bucket == b) & valid).astype(np.float32)
        E[32, blk] = (~valid).astype(np.float32)
    return E.reshape(33, 2 * 16384)


def bias_phase(fw, rel_bias_d, Ecat_d, BnT):
    with fw.phase():
        tab = Buf(fw, "tab", [64, 4])
        t31 = Buf(fw, "t31", [32, 4])
        Ec = [Buf(fw, f"Ec{i}", [33, 4096]) for i in range(2)]
        out = [Buf(fw, f"out{i}", [4, 4096]) for i in range(2)]
        b_ps = [Buf(fw, f"b_ps{i}", [4, 512], psum=True) for i in range(2)]
        fw.op("pool", lambda e: e.memset(tab[:, :], -30000.0), writes=[tab.k])
        load(fw, "sp", tab, tab[0:32, :], rel_bias_d)
        load(fw, "sp", t31, t31[:, :], rel_bias_d[31:32, :].partition_broadcast(32))
        fw.op("dve", lambda e: e.tensor_tensor(tab[0:32, :], tab[0:32, :], t31[:, :], ALU.subtract),
              reads=[tab.k, t31.k], writes=[tab.k])
        k = 0
        for pc in range(8):
            E_, O_ = Ec[pc % 2], out[pc % 2]
            load(fw, "sp", E_, E_[:, :], Ecat_d[:, pc * 4096:(pc + 1) * 4096])
            for j in range(8):
                P_ = b_ps[k % 2]
                k += 1
                mm_group(fw, P_, P_[:, :], [(tab[0:33, :], E_[0:33, j * 512:(j + 1) * 512])], [tab.k, E_.k])
                fw.op("dve", lambda e, P_=P_, O_=O_, j=j: e.tensor_copy(O_[:, j * 512:(j + 1) * 512], P_[:, :]),
                      reads=[P_.k], writes=[O_.k])
            store(fw, "pool", O_, BnT[:, pc * 4096:(pc + 1) * 4096], O_[:, :])


def attn_phase(fw, qnT, knT, v_tm, BnT, yT, lq1, lk1, lq2, lk2, subcol_d, lambda_init):
    NQ = 512
    with fw.phase():
        ones = Buf(fw, "ones", [128, 128])
        epsb = Buf(fw, "epsb", [128, 1])
        lv = Buf(fw, "lv", [128, 4, 64])
        lt = Buf(fw, "lt", [128, 2, 64])
        ls = Buf(fw, "ls", [128, 2])
        lam = Buf(fw, "lam", [128, 1])
        subw = Buf(fw, "subw", [128, 1])
        qz = [Buf(fw, f"qz{i}", [128, T]) for i in range(2)]
        kn = Buf(fw, "kn", [128, T])
        va = Buf(fw, "va", [128, 32, 128])
        Bn = Buf(fw, "Bn", [128, 2, 128])
        tmp = [Buf(fw, f"tmp{i}", [128, 128]) for i in range(2)]
        PT = [Buf(fw, f"PT{i}", [128, NQ]) for i in range(3)]
        Pacc = [Buf(fw, f"Pacc{i}", [128, NQ]) for i in range(2)]
        rl = [Buf(fw, f"rl{i}", [128, NQ]) for i in range(2)]
        t0b = Buf(fw, "t0b", [128, NQ])
        t1b = Buf(fw, "t1b", [128, NQ])
        ob = Buf(fw, "ob", [128, NQ])
        sqb = Buf(fw, "sqb", [128, NQ])
        rs = Buf(fw, "rs", [128, NQ])
        yd = [Buf(fw, f"yd{i}", [128, NQ]) for i in range(2)]
        s_ps = [Buf(fw, f"s_ps{i}", [128, NQ], psum=True) for i in range(2)]
        o_ps = [Buf(fw, f"o_ps{i}", [128, NQ], psum=True) for i in range(2)]
        l_ps = Buf(fw, "l_ps", [128, NQ], psum=True)
        ss_ps = Buf(fw, "ss_ps", [128, NQ], psum=True)
        fw.op("pool", lambda e: e.memset(ones[:, :], 1.0), writes=[ones.k])
        fw.op("pool", lambda e: e.memset(epsb[:, :], EPS), writes=[epsb.k])
        for i, l in enumerate((lq1, lk1, lq2, lk2)):
            load(fw, "sp", lv, lv[:, i, :], l.partition_broadcast(128))
        load(fw, "sp", subw, subw[:, :], subcol_d)
        fw.op("dve", lambda e: e.tensor_scalar(subw[:, :], subw[:, :], 1.0 - lambda_init, None, ALU.mult),
              reads=[subw.k], writes=[subw.k])
        fw.op("dve", lambda e: e.tensor_tensor(lt[:, 0, :], lv[:, 0, :], lv[:, 1, :], ALU.mult),
              reads=[lv.k], writes=[lt.k])
        fw.op("dve", lambda e: e.tensor_tensor(lt[:, 1, :], lv[:, 2, :], lv[:, 3, :], ALU.mult),
              reads=[lv.k, lt.k], writes=[lt.k])
        fw.op("dve", lambda e: e.reduce_sum(ls[:, :], lt[:, :, :], axis=AX.X), reads=[lt.k], writes=[ls.k])
        fw.op("act", lambda e: e.activation(ls[:, :], ls[:, :], AF.Exp), reads=[ls.k], writes=[ls.k])
        fw.op("dve", lambda e: e.tensor_tensor(lam[:, :], ls[:, 0:1], ls[:, 1:2], ALU.subtract),
              reads=[ls.k], writes=[lam.k])
        fw.op("dve", lambda e: e.tensor_scalar(lam[:, :], lam[:, :], float(lambda_init), None, ALU.add),
              reads=[lam.k], writes=[lam.k])
        fw.op("pool", lambda e: e.memset(qz[0][64:128, :], 0.0), writes=[qz[0].k])
        fw.op("pool", lambda e: e.memset(qz[1][0:64, :], 0.0), writes=[qz[1].k])
        state = {"sk": 0, "pk": 0}
        for h in range(4):
            rows = slice(h * 128, (h + 1) * 128)
            load(fw, "sp", qz[0], qz[0][0:64, :], qnT[h * 128:h * 128 + 64, :])
            load(fw, "sp", qz[1], qz[1][64:128, :], qnT[h * 128 + 64:h * 128 + 128, :])
            load(fw, "sp", kn, kn[:, :], knT[rows, :])
            load(fw, "pool", va, va[:, :, :], v_tm[:, rows].rearrange("(b p) v -> p b v", p=128))
            load(fw, "pool", Bn, Bn[:, :, :], BnT[h].rearrange("(b k q) -> k b q", b=2, k=128))

            def emit_S(qg, m, kb):
                mp = slice(m * 64, (m + 1) * 64)
                SP = s_ps[state["sk"] % 2]
                state["sk"] += 1
                mm_group(fw, SP, SP[:, :], [(kn[:, kb * 128:(kb + 1) * 128],
                                             qz[m][:, qg * NQ:(qg + 1) * NQ])], [kn.k, qz[m].k])
                P_ = PT[state["pk"] % 3]
                state["pk"] += 1
                if kb <= 4 * qg - 2:
                    fw.op("act", lambda e, P_=P_, SP=SP: e.activation(P_[:, :], SP[:, :], AF.Exp),
                          reads=[SP.k], writes=[P_.k])
                    return P_
                j0 = max(0, kb - 4 * qg)
                if j0 > 0:
                    fw.op("pool", lambda e, P_=P_, j0=j0: e.memset(P_[:, 0:j0 * 128], 0.0), writes=[P_.k])
                for j in range(j0, 4):
                    qb = 4 * qg + j
                    cs = slice(j * 128, (j + 1) * 128)
                    if kb < qb - 1:
                        fw.op("act", lambda e, P_=P_, SP=SP, cs=cs: e.activation(
                            P_[:, cs], SP[:, cs], AF.Exp), reads=[SP.k], writes=[P_.k])
                    else:
                        TM = tmp[j % 2]
                        bi = 0 if kb == qb else 1
                        fw.op("dve", lambda e, TM=TM, SP=SP, cs=cs, bi=bi: e.tensor_tensor(
                            TM[:, :], SP[:, cs], Bn[:, bi, :], ALU.add),
                            reads=[SP.k, Bn.k], writes=[TM.k])
                        fw.op("act", lambda e, P_=P_, TM=TM, cs=cs: e.activation(
                            P_[:, cs], TM[:, :], AF.Exp), reads=[TM.k], writes=[P_.k])
                return P_

            def emit_V(qg, m, kb, P_):
                OP = o_ps[m]
                last = 4 * qg + 3
                fw.op("pe", lambda e, OP=OP, P_=P_, kb=kb, last=last: e.matmul(
                    OP[:, :], mm(va[:, kb, :]), mm(P_[:, :]), start=(kb == 0), stop=(kb == last)),
                    reads=[P_.k, va.k], writes=[OP.k], pe_acc=(kb > 0))
                PA = Pacc[m]
                if kb == 0:
                    fw.op("dve", lambda e, PA=PA, P_=P_: e.tensor_copy(PA[:, :], P_[:, :]),
                          reads=[P_.k], writes=[PA.k])
                else:
                    fw.op("dve", lambda e, PA=PA, P_=P_: e.tensor_tensor(PA[:, :], PA[:, :], P_[:, :], ALU.add),
                          reads=[P_.k, PA.k], writes=[PA.k])

            items = [(qg, m, kb) for qg in range(T // NQ) for m in range(2) for kb in range(4 * qg + 4)]
            nxt = emit_S(*items[0])
            for ii, (qg, m, kb) in enumerate(items):
                cur_ = nxt
                if ii + 1 < len(items):
                    nxt = emit_S(*items[ii + 1])
                emit_V(qg, m, kb, cur_)
                if kb != 4 * qg + 3:
                    continue
                mm_group(fw, l_ps, l_ps[:, :], [(ones[:, :], Pacc[m][:, :])], [ones.k, Pacc[m].k])
                fw.op("dve", lambda e, m=m: e.reciprocal(rl[m][:, :], l_ps[:, :]),
                      reads=[l_ps.k], writes=[rl[m].k])
                if m == 0:
                    fw.op("dve", lambda e: e.tensor_tensor(t0b[:, :], o_ps[0][:, :], rl[0][:, :], ALU.mult),
                          reads=[o_ps[0].k, rl[0].k], writes=[t0b.k])
                    continue
                YD = yd[qg % 2]
                fw.op("dve", lambda e: e.scalar_tensor_tensor(
                    t1b[:, :], o_ps[1][:, :], lam[:, 0:1], rl[1][:, :], ALU.mult, ALU.mult),
                    reads=[o_ps[1].k, lam.k, rl[1].k], writes=[t1b.k])
                fw.op("dve", lambda e: e.tensor_tensor(ob[:, :], t0b[:, :], t1b[:, :], ALU.subtract),
                      reads=[t0b.k, t1b.k], writes=[ob.k])
                fw.op("act", lambda e: e.activation(sqb[:, :], ob[:, :], AF.Square),
                      reads=[ob.k], writes=[sqb.k])
                mm_group(fw, ss_ps, ss_ps[:, :], [(ones[:, :], sqb[:, :])], [ones.k, sqb.k])
                rstd_from_ss(fw, rs, ss_ps, 128, NQ, epsb)
                fw.op("dve", lambda e, YD=YD: e.scalar_tensor_tensor(
                    YD[:, :], ob[:, :], subw[:, 0:1], rs[:, :], ALU.mult, ALU.mult),
                    reads=[ob.k, subw.k, rs.k], writes=[YD.k])
                store(fw, "sp", YD, yT[rows, qg * NQ:(qg + 1) * NQ], YD[:, :])


def _lay_w(W):
    n = W.shape[1]
    return np.ascontiguousarray(W.reshape(8, 128, n).transpose(1, 0, 2))


def _lay_g(g):
    return np.ascontiguousarray(g.reshape(8, 128).T)


def _lay_gu(W):
    return np.ascontiguousarray(W.reshape(8, 128, NFC, 128).transpose(2, 1, 0, 3))


def _lay_d(W):
    return np.ascontiguousarray(W.reshape(NFC, 128, 8, 128).transpose(2, 1, 0, 3))


def host_layout(inputs):
    f = lambda a: np.ascontiguousarray(np.asarray(a, dtype=np.float32))
    sh = {}
    for i in range(2):
        for j in range(2):
            sh[f"wg{i}{j}"] = _lay_gu(f(inputs["ffn_w_gate"][i, j]))
            sh[f"wu{i}{j}"] = _lay_gu(f(inputs["ffn_w_up"][i, j]))
            sh[f"wd{i}{j}"] = _lay_d(f(inputs["ffn_w_down"][i, j]))
            sh[f"fg{i}{j}"] = _lay_g(f(inputs["ffn_norm"][i, j]))
        sh[f"mg{i}"] = _lay_g(f(inputs["mix_norm"][i]))
    Mc, M3, maskA = gla_consts()
    cur, prev, cur0 = pool_consts()
    U, Tri, NEG = ssd_consts()
    sh["ev_w_in"] = _lay_w(f(inputs["ev_w_in"][0]))
    sh["Mc"], sh["M3"], sh["mA"] = Mc, M3, maskA
    sh["wgk1"] = f(np.concatenate([inputs["ev_w_gk_up"][0], inputs["ev_b_gk"][0][None]], 0))
    sh["gnorm"] = f(inputs["ev_gla_norm"][0][:, None])
    sh["wpool"] = f(np.asarray(inputs["ev_w_pool"][0]).transpose(1, 0, 2))
    sh["pscale"] = f(np.asarray(inputs["ev_pool_scale"][0]).reshape(4, 128).T)
    sh["cur"] = f(cur.transpose(1, 0, 2))
    sh["prev"] = f(prev.transpose(1, 0, 2))
    sh["cur0"] = f(cur0.transpose(1, 0, 2))
    sh["ev_w_out"] = _lay_w(f(inputs["ev_w_out"][0]))
    sh["od_w_in"] = _lay_w(f(inputs["od_w_in"][0]))
    sh["U"], sh["Tri"], sh["NEG"] = U, Tri, NEG
    sh["ident"] = np.eye(128, dtype=np.float32)
    sh["cw"] = f(np.asarray(inputs["od_conv_w"][0]).reshape(4, 8, 128).transpose(2, 1, 0))
    sh["cb"] = f(np.asarray(inputs["od_conv_b"][0]).reshape(8, 128).T)
    sh["dtb"] = f(inputs["od_dt_bias"])
    sh["alog"] = f(inputs["od_a_log"])
    sh["dsk"] = f(inputs["od_d_skip"])
    sh["wn"] = f(inputs["od_ssd_norm"])
    sh["wq"] = f(np.tile(np.asarray(inputs["od_q_norm"][0]), 2)[:, None])
    sh["wk"] = f(np.tile(np.asarray(inputs["od_k_norm"][0]), 2)[:, None])
    sh["bones"] = np.kron(np.eye(2), np.ones((64, 64))).astype(np.float32)
    sh["relb"] = f(inputs["rel_bias"])
    sh["Ecat"] = t5_consts()
    for nm in ("q1", "k1", "q2", "k2"):
        sh["l" + nm] = f(inputs["od_lambda_" + nm])
    sh["subcol"] = f(np.asarray(inputs["od_subln"][0])[:, None])
    sh["od_w_out"] = _lay_w(f(inputs["od_w_out"][0]))
    return sh


def build_program(shared_shapes):
    import math
    nc = bass.Bass("TRN2", target_bir_lowering=False)
    A = {}
    A["xin"] = nc.dram_tensor("xin", [D, T], F32, kind="ExternalInput").ap()
    for k, shp in shared_shapes.items():
        A[k] = nc.dram_tensor(k, list(shp), F32, kind="ExternalInput").ap()
    xout = nc.dram_tensor("xout", [D, T], F32, kind="ExternalOutput").ap()

    def Sx(name, shape):
        return nc.dram_tensor(name, list(shape), F32).ap()

    xa, xb = Sx("xa", [D, T]), Sx("xb", [D, T])
    yT = Sx("yT", [1024, T])
    qT, kT = Sx("qT", [512, T]), Sx("kT", [512, T])
    gT, lrT = Sx("gT", [512, T]), Sx("lrT", [16, T])
    k_tm, v_tm, u_tm = Sx("k_tm", [T, 256]), Sx("v_tm", [T, 512]), Sx("u_tm", [T, 512])
    xbcT, xcT, xB_tm = Sx("xbcT", [1024, T]), Sx("xcT", [1024, T]), Sx("xB_tm", [T, 768])
    qnT, knT = Sx("qnT", [512, T]), Sx("knT", [512, T])
    z_tm, dt_tm, BnT = Sx("z_tm", [T, 512]), Sx("dt_tm", [T, 8]), Sx("BnT", [4, 32768])
    with contextlib.ExitStack() as st:
        fw = FW(nc, st)
        ffn_phase(fw, A["xin"], xa, A["wg00"], A["wu00"], A["wd00"], A["fg00"])
        fm = [(0, 128, qT[0:128]), (128, 128, qT[128:256]), (256, 128, kT[0:128]), (384, 128, kT[128:256])]
        fm += [(1024 + i * 128, 128, gT[i * 128:(i + 1) * 128]) for i in range(4)]
        fm += [(1536, 16, lrT)]
        tm = [(256, 256, k_tm), (512, 512, v_tm), (1552, 512, u_tm)]
        inproj_phase(fw, xa, A["ev_w_in"], 2064, A["mg0"], fm, tm)
        gla_phase(fw, qT[0:256], kT[0:256], gT, lrT, k_tm, v_tm, yT[0:512], A["Mc"], A["M3"], A["mA"],
                  A["wgk1"], A["gnorm"])
        pool_phase(fw, u_tm, yT[512:1024], A["wpool"], A["pscale"], A["cur"], A["prev"], A["cur0"])
        outproj_phase(fw, xa, xb, yT, A["ev_w_out"])
        ffn_phase(fw, xb, xa, A["wg01"], A["wu01"], A["wd01"], A["fg01"])
        ffn_phase(fw, xa, xb, A["wg10"], A["wu10"], A["wd10"], A["fg10"])
        fm = [(512 + i * 128, 128, xbcT[i * 128:(i + 1) * 128]) for i in range(8)]
        fm += [(1544 + i * 128, 128, qT[i * 128:(i + 1) * 128]) for i in range(4)]
        fm += [(2056 + i * 128, 128, kT[i * 128:(i + 1) * 128]) for i in range(4)]
        tm = [(0, 512, z_tm), (1536, 8, dt_tm), (2568, 512, v_tm)]
        inproj_phase(fw, xb, A["od_w_in"], 3080, A["mg1"], fm, tm)
        conv_phase(fw, xbcT, xcT, xB_tm, A["cw"], A["cb"], A["ident"])
        ssd_phase(fw, xcT, xB_tm, z_tm, dt_tm, yT[0:512], A["U"], A["Tri"], A["NEG"], A["ident"],
                  A["dtb"], A["alog"], A["dsk"], A["wn"])
        qknorm_phase(fw, qT, kT, qnT, knT, A["wq"], A["wk"], A["bones"])
        bias_phase(fw, A["relb"], A["Ecat"], BnT)
        attn_phase(fw, qnT, knT, v_tm, BnT, yT[512:1024], A["lq1"], A["lk1"], A["lq2"],
                   A["lk2"], A["subcol"], 0.8 - 0.6 * math.exp(-0.3 * 1))
        outproj_phase(fw, xb, xa, yT, A["od_w_out"])
        ffn_phase(fw, xa, xout, A["wg11"], A["wu11"], A["wd11"], A["fg11"])
    return nc


def kernel(**inputs):
    x = np.asarray(inputs["x"], dtype=np.float32)
    sh = host_layout(inputs)
    nc = build_program({k: v.shape for k, v in sh.items()})
    in_maps = []
    for b in range(8):
        m = dict(sh)
        m["xin"] = np.ascontiguousarray(x[b].T)
        in_maps.append(m)
    res = run_bass_kernel_spmd(nc, in_maps, core_ids=list(range(8)))
    out = np.stack([np.ascontiguousarray(res.results[b]["xout"].T) for b in range(8)], 0)
    return out.astype(np.float32)
```

```python
import contextlib
import numpy as np
import concourse.bass as bass
import concourse.mybir as mybir
from concourse.bass_utils import run_bass_kernel_spmd

F32 = mybir.dt.float32
F32R = mybir.dt.float32r
ALU = mybir.AluOpType
AF = mybir.ActivationFunctionType
AX = mybir.AxisListType

T = 4096
D = 1024
DFF = 2816
NFC = DFF // 128
EPS = 1e-6
MM_FAST = False


def mm(ap):
    return ap.bitcast(F32R) if MM_FAST else ap


class Trk:
    __slots__ = ("w", "r", "sem", "name")

    def __init__(self, name=""):
        self.w = None
        self.r = []
        self.sem = None
        self.name = name


class Op:
    __slots__ = ("eng", "fn", "deps", "flag", "sem", "val", "dma", "n")

    def __init__(self, eng, fn, dma=False, n=1):
        self.eng = eng
        self.fn = fn
        self.deps = []
        self.flag = False
        self.sem = None
        self.val = 0
        self.dma = dma
        self.n = n


EPOCH = 6000
ENGS = ("pe", "act", "dve", "pool", "sp")


class FW:
    def __init__(self, nc, stack, n_dma_sems=48):
        self.nc = nc
        self.stack = stack
        self.eng_obj = {"pe": nc.tensor, "act": nc.scalar, "dve": nc.vector,
                        "pool": nc.gpsimd, "sp": nc.sync}
        self.eng_sems = {e: [] for e in ENGS}
        self.eng_cnt = {e: 0 for e in ENGS}
        self.free_dma = {}
        self.dma_cnt = {}
        self.n_sem = 0
        self.ops = []
        self.phase_dma_sems = set()
        self.waited = {e: {} for e in ENGS}
        self.phase_stack = None
        self.n_total = 0

    def new_sem(self, name):
        self.n_sem += 1
        return self.stack.enter_context(self.nc.semaphore(f"{name}_{self.n_sem}"))

    def sb(self, name, shape, dtype=F32):
        self.n_sem += 1
        name = f"{name}_{self.n_sem}"
        t = self.phase_stack.enter_context(self.nc.sbuf_tensor(name, list(shape), dtype))
        return t

    def ps(self, name, shape, dtype=F32):
        self.n_sem += 1
        name = f"{name}_{self.n_sem}"
        t = self.phase_stack.enter_context(self.nc.psum_tensor(name, list(shape), dtype))
        return t

    def _dep(self, op, reads, writes):
        deps = op.deps
        for t in reads:
            if t.w is not None:
                deps.append(t.w)
        for t in writes:
            if t.w is not None:
                deps.append(t.w)
            deps.extend(t.r)
        for t in reads:
            if not op.dma:
                t.r = [x for x in t.r if x.dma or x.eng != op.eng]
            t.r.append(op)
        for t in writes:
            t.w = op
            t.r = []

    def op(self, eng, fn, reads=(), writes=(), pe_acc=False):
        o = Op(eng, fn)
        self._dep(o, reads, writes)
        if pe_acc:
            o.deps = [d for d in o.deps if d.eng != "pe" or d.dma]
        self.ops.append(o)
        return o

    def dma(self, q, fns, sbuf_trk, reads=(), writes=()):
        if not isinstance(fns, (list, tuple)):
            fns = [fns]
        o = Op(q, fns, dma=True, n=len(fns))
        self._dep(o, reads, writes)
        cls = "sw" if q == "pool" else "hw"
        if sbuf_trk.sem is None:
            sbuf_trk.sem = {}
        if cls not in sbuf_trk.sem:
            fl = self.free_dma.setdefault(cls, [])
            if not fl:
                s = self.new_sem("dq" + cls)
                self.dma_cnt[s] = 0
                fl.append(s)
            sbuf_trk.sem[cls] = fl.pop()
            self.phase_dma_sems.add((cls, sbuf_trk.sem[cls]))
        o.sem = sbuf_trk.sem[cls]
        self.dma_cnt[o.sem] += 16 * len(fns)
        o.val = self.dma_cnt[o.sem]
        self.ops.append(o)
        return o

    def emit(self):
        nc = self.nc
        ops = self.ops
        for o in ops:
            for d in o.deps:
                if not d.dma:
                    d.flag = True
        last = {}
        for o in ops:
            if not o.dma:
                last[o.eng] = o
        for o in last.values():
            o.flag = True
        for o in ops:
            if o.dma or not o.flag:
                continue
            c = self.eng_cnt[o.eng]
            ep = c // EPOCH
            sems = self.eng_sems[o.eng]
            while len(sems) <= ep:
                sems.append(self.new_sem("e" + o.eng))
            o.sem = sems[ep]
            o.val = c % EPOCH + 1
            self.eng_cnt[o.eng] = c + 1
        targets = []
        for e, o in last.items():
            targets.append((o.sem, o.val))
        for (_c, s) in self.phase_dma_sems:
            targets.append((s, self.dma_cnt[s]))
        by_eng = {e: [] for e in ENGS}
        for o in ops:
            by_eng[o.eng].append(o)
        waited = self.waited

        def run(ename):
            def body(eng):
                wd = waited[ename]
                for o in by_eng[ename]:
                    for d in o.deps:
                        if d.sem is None:
                            continue
                        if wd.get(d.sem, 0) >= d.val:
                            continue
                        if d.eng == ename and not d.dma and ename == "pe":
                            pass
                        eng.wait_ge(d.sem, d.val)
                        wd[d.sem] = d.val
                    if o.dma:
                        for f in o.fn:
                            f(eng).then_inc(o.sem, 16)
                    else:
                        ins = o.fn(eng)
                        if o.flag:
                            ins.then_inc(o.sem, 1)
                for (s, v) in targets:
                    if wd.get(s, 0) < v:
                        eng.wait_ge(s, v)
                        wd[s] = v
            return body

        with nc.Block() as block:
            block.sync(run("sp"))
            block.scalar(run("act"))
            block.vector(run("dve"))
            block.gpsimd(run("pool"))
            block.tensor(run("pe"))
        self.n_total += len(ops)
        for (_c, s) in self.phase_dma_sems:
            if self.dma_cnt[s] < 24000:
                self.free_dma[_c].append(s)
        self.phase_dma_sems = set()
        self.ops = []

    @contextlib.contextmanager
    def phase(self):
        with contextlib.ExitStack() as st:
            self.phase_stack = st
            yield
            self.emit()
        self.phase_stack = None


class Buf:
    def __init__(self, fw, name, shape, dtype=F32, psum=False):
        self.t = fw.ps(name, shape, dtype) if psum else fw.sb(name, shape, dtype)
        self.k = Trk(name)

    def __getitem__(self, idx):
        return self.t[idx]


def rstd_from_ss(fw, out_buf, ss_ps, n, width, epsb):
    fw.op("act", lambda e: e.activation(out_buf[:, :width], ss_ps[:, :width], AF.Sqrt,
                                        bias=epsb[:, 0:1], scale=1.0 / n),
          reads=[ss_ps.k, epsb.k], writes=[out_buf.k])
    fw.op("dve", lambda e: e.reciprocal(out_buf[:, :width], out_buf[:, :width]),
          reads=[out_buf.k], writes=[out_buf.k])


def ffn_phase(fw, xT_in, xT_out, wg, wu, wd, gnorm, NT=512):
    with fw.phase():
        ones = Buf(fw, "ones", [128, 128])
        g_sb = Buf(fw, "g_sb", [128, 8])
        xt = [Buf(fw, f"xt{i}", [128, 8, NT]) for i in range(2)]
        xn = Buf(fw, "xn", [128, 8, NT])
        sq = [Buf(fw, f"sq{i}", [128, NT]) for i in range(2)]
        rstd = Buf(fw, "rstd", [128, NT])
        hid = Buf(fw, "hid", [128, NFC, NT])
        sg = [Buf(fw, f"sg{i}", [128, NT]) for i in range(2)]
        wgb = [Buf(fw, f"wgb{i}", [128, 8, 128]) for i in range(3)]
        wub = [Buf(fw, f"wub{i}", [128, 8, 128]) for i in range(3)]
        wdb = [Buf(fw, f"wdb{i}", [128, NFC, 128]) for i in range(4)]
        xo = [Buf(fw, f"xo{i}", [128, NT]) for i in range(2)]
        ss_ps = Buf(fw, "ss_ps", [128, NT], psum=True)
        gp = [Buf(fw, f"gp{i}", [128, NT], psum=True) for i in range(2)]
        up = [Buf(fw, f"up{i}", [128, NT], psum=True) for i in range(2)]
        op_ = [Buf(fw, f"op{i}", [128, NT], psum=True) for i in range(2)]

        fw.op("pool", lambda e: e.memset(ones[:, :], 1.0), writes=[ones.k])
        epsb = Buf(fw, "epsb", [128, 1])
        fw.op("pool", lambda e: e.memset(epsb[:, :], EPS), writes=[epsb.k])
        fw.dma("sp", lambda e: e.dma_start(out=g_sb[:, :], in_=gnorm), g_sb.k, writes=[g_sb.k])
        xin = xT_in.rearrange("(c p) t -> p c t", p=128)
        xout = xT_out.rearrange("(c p) t -> p c t", p=128)
        ntiles = T // NT
        wi = 0
        di = 0

        def load_x(it):
            X = xt[it % 2]
            ts = slice(it * NT, (it + 1) * NT)
            fw.dma("pool", lambda e, X=X, ts=ts: e.dma_start(out=X[:, :, :], in_=xin[:, :, ts]),
                   X.k, writes=[X.k])

        def load_wd(dcount):
            W = wdb[dcount % 4]
            dc = dcount % 8
            fw.dma("sp", lambda e, W=W, dc=dc: e.dma_start(out=W[:, :, :], in_=wd[dc]),
                   W.k, writes=[W.k])

        load_x(0)
        for it in range(ntiles):
            X = xt[it % 2]
            ts = slice(it * NT, (it + 1) * NT)
            for c in range(8):
                S = sq[c % 2]
                fw.op("act", lambda e, S=S, X=X, c=c: e.activation(S[:, :], X[:, c, :], AF.Square),
                      reads=[X.k], writes=[S.k])
                fw.op("pe", lambda e, S=S, c=c: e.matmul(ss_ps[:, :], ones[:, :], S[:, :],
                                                          start=(c == 0), stop=(c == 7)),
                      reads=[ones.k, S.k], writes=[ss_ps.k], pe_acc=(c > 0))
            rstd_from_ss(fw, rstd, ss_ps, D, NT, epsb)
            for c in range(8):
                fw.op("dve", lambda e, X=X, c=c: e.scalar_tensor_tensor(
                    xn[:, c, :], X[:, c, :], g_sb[:, c:c + 1], rstd[:, :], ALU.mult, ALU.mult),
                    reads=[X.k, g_sb.k, rstd.k], writes=[xn.k])
            if it + 1 < ntiles:
                load_x(it + 1)
            for f in range(NFC):
                WG = wgb[wi % 3]
                WU = wub[wi % 3]
                wi += 1
                fw.dma("sp", lambda e, WG=WG, f=f: e.dma_start(out=WG[:, :, :], in_=wg[f]),
                       WG.k, writes=[WG.k])
                fw.dma("act", lambda e, WU=WU, f=f: e.dma_start(out=WU[:, :, :], in_=wu[f]),
                       WU.k, writes=[WU.k])
                if f in (6, 10, 14, 18):
                    load_wd(it * 8 + (f - 6) // 4)
                G = gp[f % 2]
                U = up[f % 2]
                for c in range(8):
                    fw.op("pe", lambda e, W=WG, G=G, c=c: e.matmul(
                        G[:, :], mm(W[:, c, :]), mm(xn[:, c, :]), start=(c == 0), stop=(c == 7)),
                        reads=[WG.k, xn.k], writes=[G.k], pe_acc=(c > 0))
                for c in range(8):
                    fw.op("pe", lambda e, W=WU, U=U, c=c: e.matmul(
                        U[:, :], mm(W[:, c, :]), mm(xn[:, c, :]), start=(c == 0), stop=(c == 7)),
                        reads=[WU.k, xn.k], writes=[U.k], pe_acc=(c > 0))
                SG = sg[f % 2]
                fw.op("act", lambda e, SG=SG, G=G: e.activation(SG[:, :], G[:, :], AF.Silu),
                      reads=[G.k], writes=[SG.k])
                fw.op("dve", lambda e, SG=SG, U=U, f=f: e.tensor_tensor(
                    hid[:, f, :], SG[:, :], U[:, :], ALU.mult),
                    reads=[SG.k, U.k], writes=[hid.k])
            for dc in range(8):
                W = wdb[(it * 8 + dc) % 4]
                O = op_[dc % 2]
                for f in range(NFC):
                    fw.op("pe", lambda e, W=W, O=O, f=f: e.matmul(
                        O[:, :], mm(W[:, f, :]), mm(hid[:, f, :]), start=(f == 0), stop=(f == NFC - 1)),
                        reads=[W.k, hid.k], writes=[O.k], pe_acc=(f > 0))
                if dc + 4 < 8:
                    load_wd(it * 8 + dc + 4)
                XO = xo[dc % 2]
                fw.op("dve", lambda e, XO=XO, O=O, X=X, dc=dc: e.scalar_tensor_tensor(
                    XO[:, :], O[:, :], 0.5, X[:, dc, :], ALU.mult, ALU.add),
                    reads=[O.k, X.k], writes=[XO.k])
                fw.dma("pool", lambda e, XO=XO, dc=dc, ts=ts: e.dma_start(out=xout[:, dc, ts], in_=XO[:, :]),
                       XO.k, reads=[XO.k])


def mm_group(fw, O, out_ap, pairs, reads):
    n = len(pairs)
    for i, (l, r) in enumerate(pairs):
        fw.op("pe", lambda e, l=l, r=r, i=i: e.matmul(out_ap, mm(l), mm(r), start=(i == 0),
                                                       stop=(i == n - 1)),
              reads=reads, writes=[O.k], pe_acc=(i > 0))


def load(fw, q, B, out_ap, in_ap):
    fw.dma(q, lambda e: e.dma_start(out=out_ap, in_=in_ap), B.k, writes=[B.k])


def store(fw, q, B, out_ap, in_ap):
    fw.dma(q, lambda e: e.dma_start(out=out_ap, in_=in_ap), B.k, reads=[B.k])


def norm_tile(fw, X, hT, sq, ss_ps, rstd, ones, g_sb, epsb, NT):
    for c in range(8):
        S = sq[c % 2]
        fw.op("act", lambda e, S=S, c=c: e.activation(S[:, :], X[:, c, :], AF.Square),
              reads=[X.k], writes=[S.k])
        fw.op("pe", lambda e, S=S, c=c: e.matmul(ss_ps[:, :NT], ones[:, :], S[:, :],
                                                  start=(c == 0), stop=(c == 7)),
              reads=[ones.k, S.k], writes=[ss_ps.k], pe_acc=(c > 0))
    rstd_from_ss(fw, rstd, ss_ps, D, NT, epsb)
    for c in range(8):
        fw.op("dve", lambda e, c=c: e.scalar_tensor_tensor(
            hT[:, c, :], X[:, c, :], g_sb[:, c:c + 1], rstd[:, :NT], ALU.mult, ALU.mult),
            reads=[X.k, g_sb.k, rstd.k], writes=[hT.k])


def inproj_phase(fw, xT_in, w_in, ncols, gnorm, fm_groups, tm_groups, NT=512):
    with fw.phase():
        ones = Buf(fw, "ones", [128, 128])
        epsb = Buf(fw, "epsb", [128, 1])
        g_sb = Buf(fw, "g_sb", [128, 8])
        W = Buf(fw, "W", [128, 8, ncols])
        xt = [Buf(fw, f"xt{i}", [128, 8, NT]) for i in range(2)]
        hT = Buf(fw, "hT", [128, 8, NT])
        sq = [Buf(fw, f"sq{i}", [128, NT]) for i in range(2)]
        rstd = Buf(fw, "rstd", [128, NT])
        stf = [Buf(fw, f"stf{i}", [128, NT]) for i in range(3)]
        stt = [Buf(fw, f"stt{i}", [128, 512]) for i in range(3)]
        ss_ps = Buf(fw, "ss_ps", [128, NT], psum=True)
        fp = [Buf(fw, f"fp{i}", [128, NT], psum=True) for i in range(3)]
        tp = [Buf(fw, f"tp{i}", [128, 512], psum=True) for i in range(3)]
        fw.op("pool", lambda e: e.memset(ones[:, :], 1.0), writes=[ones.k])
        fw.op("pool", lambda e: e.memset(epsb[:, :], EPS), writes=[epsb.k])
        load(fw, "sp", g_sb, g_sb[:, :], gnorm)
        fw.dma("pool", [lambda e, c=c: e.dma_start(out=W[:, c, :], in_=w_in[:, c, :]) for c in range(8)],
               W.k, writes=[W.k])
        xin = xT_in.rearrange("(c p) t -> p c t", p=128)
        k = 0
        for it in range(T // NT):
            X = xt[it % 2]
            ts = slice(it * NT, (it + 1) * NT)
            load(fw, "sp", X, X[:, :, :], xin[:, :, ts])
            norm_tile(fw, X, hT, sq, ss_ps, rstd, ones, g_sb, epsb, NT)
            for (c0, wd_, dst) in fm_groups:
                P_ = fp[k % 3]
                S_ = stf[k % 3]
                mm_group(fw, P_, P_[:wd_, :], [(W[:, c, c0:c0 + wd_], hT[:, c, :]) for c in range(8)],
                         [W.k, hT.k])
                eng = "act" if k % 2 == 0 else "dve"
                if eng == "act":
                    fw.op("act", lambda e, P_=P_, S_=S_, wd_=wd_: e.copy(S_[:wd_, :], P_[:wd_, :]),
                          reads=[P_.k], writes=[S_.k])
                else:
                    fw.op("dve", lambda e, P_=P_, S_=S_, wd_=wd_: e.tensor_copy(S_[:wd_, :], P_[:wd_, :]),
                          reads=[P_.k], writes=[S_.k])
                store(fw, "sp", S_, dst[:, ts], S_[:wd_, :])
                k += 1
            for sub in range(NT // 128):
                t0 = it * NT + sub * 128
                for (c0, wd_, dst) in tm_groups:
                    P_ = tp[k % 3]
                    S_ = stt[k % 3]
                    mm_group(fw, P_, P_[:, :wd_],
                             [(hT[:, c, sub * 128:(sub + 1) * 128], W[:, c, c0:c0 + wd_]) for c in range(8)],
                             [W.k, hT.k])
                    if k % 2 == 0:
                        fw.op("act", lambda e, P_=P_, S_=S_, wd_=wd_: e.copy(S_[:, :wd_], P_[:, :wd_]),
                              reads=[P_.k], writes=[S_.k])
                    else:
                        fw.op("dve", lambda e, P_=P_, S_=S_, wd_=wd_: e.tensor_copy(S_[:, :wd_], P_[:, :wd_]),
                              reads=[P_.k], writes=[S_.k])
                    store(fw, "pool", S_, dst[t0:t0 + 128, :], S_[:, :wd_])
                    k += 1


def outproj_phase(fw, xT_in, xT_out, yT, w_out, NT=512):
    with fw.phase():
        W = Buf(fw, "W", [128, 8, 1024])
        xt = [Buf(fw, f"xt{i}", [128, 8, NT]) for i in range(2)]
        yt = [Buf(fw, f"yt{i}", [128, 8, NT]) for i in range(2)]
        xo = [Buf(fw, f"xo{i}", [128, 8, NT]) for i in range(2)]
        op_ = [Buf(fw, f"op{i}", [128, NT], psum=True) for i in range(3)]
        fw.dma("pool", [lambda e, c=c: e.dma_start(out=W[:, c, :], in_=w_out[:, c, :]) for c in range(8)],
               W.k, writes=[W.k])
        xin = xT_in.rearrange("(c p) t -> p c t", p=128)
        yin = yT.rearrange("(c p) t -> p c t", p=128)
        xout = xT_out.rearrange("(c p) t -> p c t", p=128)
        k = 0
        for it in range(T // NT):
            X = xt[it % 2]
            Y = yt[it % 2]
            XO = xo[it % 2]
            ts = slice(it * NT, (it + 1) * NT)
            load(fw, "sp", X, X[:, :, :], xin[:, :, ts])
            load(fw, "pool", Y, Y[:, :, :], yin[:, :, ts])
            for dc in range(8):
                O = op_[k % 3]
                k += 1
                mm_group(fw, O, O[:, :], [(W[:, c, dc * 128:(dc + 1) * 128], Y[:, c, :]) for c in range(8)],
                         [W.k, Y.k])
                fw.op("dve", lambda e, O=O, X=X, XO=XO, dc=dc: e.tensor_tensor(
                    XO[:, dc, :], O[:, :], X[:, dc, :], ALU.add),
                    reads=[O.k, X.k], writes=[XO.k])
            store(fw, "sp", XO, xout[:, :, ts], XO[:, :, :])


def gla_consts():
    s = np.arange(128)[:, None]
    t = np.arange(128)[None, :]
    same = (s // 64) == (t // 64)
    Mc = np.zeros((128, 130), np.float32)
    Mc[:, :128] = np.where(same & (s <= t), -1.0 / 16, 0.0)
    Mc[:64, 128] = -1.0 / 16
    Mc[64:, 129] = -1.0 / 16
    M3 = np.where(same & (s > t), -1.0 / 16, 0.0).astype(np.float32)
    maskA = np.where(same & (s <= t), 1.0, 0.0).astype(np.float32)
    return Mc, M3, maskA


def gla_phase(fw, qT, kT, gT, lrT, k_tm, v_tm, yT, Mc_d, M3_d, maskA_d, wgk1_d, gnorm_d):
    with fw.phase():
        ones = Buf(fw, "ones", [128, 128])
        epsb = Buf(fw, "epsb", [128, 1])
        Mc = Buf(fw, "Mc", [128, 130])
        M3 = Buf(fw, "M3", [128, 128])
        mA = Buf(fw, "mA", [128, 128])
        wgk = Buf(fw, "wgk", [32, 256])
        wn = Buf(fw, "wn", [128, 1])
        qt = [Buf(fw, f"qt{i}", [128, 2, 128]) for i in range(2)]
        kt = [Buf(fw, f"kt{i}", [128, 2, 128]) for i in range(2)]
        gt = [Buf(fw, f"gt{i}", [128, 4, 128]) for i in range(2)]
        lr = [Buf(fw, f"lr{i}", [32, 128]) for i in range(2)]
        ktm = [Buf(fw, f"ktm{i}", [128, 256]) for i in range(2)]
        vtm = [Buf(fw, f"vtm{i}", [128, 512]) for i in range(2)]
        e1 = Buf(fw, "e1", [128, 256])
        sp_ = Buf(fw, "sp_", [128, 256])
        eG = Buf(fw, "eG", [128, 2, 130])
        enG = Buf(fw, "enG", [128, 2, 128])
        eD = Buf(fw, "eD", [128, 256])
        qd = Buf(fw, "qd", [128, 2, 128])
        kd = Buf(fw, "kd", [128, 2, 128])
        kk = Buf(fw, "kk", [128, 256])
        ATm = Buf(fw, "ATm", [128, 4, 128])
        oxs = Buf(fw, "oxs", [128, 4, 128])
        oT = Buf(fw, "oT", [128, 4, 128])
        sqo = Buf(fw, "sqo", [128, 4, 128])
        rstd = Buf(fw, "rstd", [128, 512])
        sg = Buf(fw, "sg", [128, 4, 128])
        ya = [Buf(fw, f"ya{i}", [128, 4, 128]) for i in range(2)]
        S = [Buf(fw, f"S{i}", [128, 2, 128]) for i in range(2)]
        zd_ps = Buf(fw, "zd_ps", [128, 512], psum=True)
        d_ps = Buf(fw, "d_ps", [128, 512], psum=True)
        gt_ps = Buf(fw, "gt_ps", [128, 2, 256], psum=True)
        at_ps = Buf(fw, "at_ps", [128, 4, 128], psum=True)
        oi_ps = Buf(fw, "oi_ps", [128, 4, 128], psum=True)
        ox_ps = Buf(fw, "ox_ps", [128, 4, 128], psum=True)
        st_ps = [Buf(fw, f"st_ps{i}", [128, 2, 256], psum=True) for i in range(2)]
        fw.op("pool", lambda e: e.memset(ones[:, :], 1.0), writes=[ones.k])
        fw.op("pool", lambda e: e.memset(epsb[:, :], EPS), writes=[epsb.k])
        for i in range(2):
            fw.op("pool", lambda e, i=i: e.memset(lr[i][:, :], 1.0), writes=[lr[i].k])
            fw.op("pool", lambda e, i=i: e.memset(S[i][:, :, :], 0.0), writes=[S[i].k])
        load(fw, "sp", Mc, Mc[:, :], Mc_d)
        load(fw, "sp", M3, M3[:, :], M3_d)
        load(fw, "sp", mA, mA[:, :], maskA_d)
        load(fw, "sp", wgk, wgk[0:17, :], wgk1_d)
        load(fw, "sp", wn, wn[:, :], gnorm_d)
        qTr = qT.rearrange("(c p) t -> p c t", p=128)
        kTr = kT.rearrange("(c p) t -> p c t", p=128)
        gTr = gT.rearrange("(c p) t -> p c t", p=128)
        yTr = yT.rearrange("(c p) t -> p c t", p=128)
        def gla_loads(it):
            ts = slice(it * 128, (it + 1) * 128)
            b = it % 2
            Q, K_, G_, L, KT, VT = qt[b], kt[b], gt[b], lr[b], ktm[b], vtm[b]
            load(fw, "sp", Q, Q[:, :, :], qTr[:, :, ts])
            load(fw, "sp", K_, K_[:, :, :], kTr[:, :, ts])
            load(fw, "sp", G_, G_[:, :, :], gTr[:, :, ts])
            load(fw, "sp", L, L[0:16, :], lrT[:, ts])
            load(fw, "sp", KT, KT[:, :], k_tm[ts, :])
            load(fw, "sp", VT, VT[:, :], v_tm[ts, :])

        gla_loads(0)
        for it in range(T // 128):
            ts = slice(it * 128, (it + 1) * 128)
            b = it % 2
            Q, K_, G_, L, KT, VT = qt[b], kt[b], gt[b], lr[b], ktm[b], vtm[b]
            if it + 1 < T // 128:
                gla_loads(it + 1)
            mm_group(fw, zd_ps, zd_ps[:, 0:256], [(L[0:17, :], wgk[0:17, :])], [L.k, wgk.k])
            fw.op("act", lambda e: e.activation(e1[:, :], zd_ps[:, 0:256], AF.Exp, scale=-1.0),
                  reads=[zd_ps.k], writes=[e1.k])
            fw.op("act", lambda e: e.activation(sp_[:, :], e1[:, :], AF.Ln, bias=1.0),
                  reads=[e1.k], writes=[sp_.k])
            for c in range(2):
                mm_group(fw, gt_ps, gt_ps[:, c, 0:130], [(sp_[:, c * 128:(c + 1) * 128], Mc[:, :])],
                         [sp_.k, Mc.k])
            mm_group(fw, d_ps, d_ps[:, 0:256], [(M3[:, :], sp_[:, :])], [M3.k, sp_.k])
            fw.op("act", lambda e: e.activation(eG[:, :, :], gt_ps[:, :, 0:130], AF.Exp),
                  reads=[gt_ps.k], writes=[eG.k])
            fw.op("act", lambda e: e.activation(enG[:, :, :], gt_ps[:, :, 0:128], AF.Exp, scale=-1.0),
                  reads=[gt_ps.k], writes=[enG.k])
            fw.op("act", lambda e: e.activation(eD[:, :], d_ps[:, 0:256], AF.Exp),
                  reads=[d_ps.k], writes=[eD.k])
            fw.op("dve", lambda e, Q=Q: e.scalar_tensor_tensor(
                qd[:, :, :], Q[:, :, :], 0.125, eG[:, :, 0:128], ALU.mult, ALU.mult),
                reads=[Q.k, eG.k], writes=[qd.k])
            fw.op("dve", lambda e, K_=K_: e.tensor_tensor(kd[:, :, :], K_[:, :, :], enG[:, :, :], ALU.mult),
                  reads=[K_.k, enG.k], writes=[kd.k])
            fw.op("dve", lambda e, KT=KT: e.tensor_tensor(kk[:, :], KT[:, :], eD[:, :], ALU.mult),
                  reads=[KT.k, eD.k], writes=[kk.k])
            for h in range(4):
                c, pb = h // 2, (h % 2) * 64
                mm_group(fw, at_ps, at_ps[:, h, :], [(kd[pb:pb + 64, c, :], qd[pb:pb + 64, c, :])],
                         [kd.k, qd.k])
            fw.op("dve", lambda e: e.tensor_tensor(
                ATm[:, :, :], at_ps[:, :, :], mA[:, :].unsqueeze(1).to_broadcast([128, 4, 128]), ALU.mult),
                reads=[at_ps.k, mA.k], writes=[ATm.k])
            for h in range(4):
                mm_group(fw, oi_ps, oi_ps[:, h, :], [(VT[:, h * 128:(h + 1) * 128], ATm[:, h, :])],
                         [VT.k, ATm.k])
            for cc in range(2):
                Sc, Sn = S[cc], S[1 - cc]
                for h in range(4):
                    c, pb = h // 2, (h % 2) * 64
                    mm_group(fw, ox_ps, ox_ps[:, h, cc * 64:(cc + 1) * 64],
                             [(Sc[pb:pb + 64, c, :], qd[pb:pb + 64, c, cc * 64:(cc + 1) * 64])],
                             [Sc.k, qd.k])
                STP = st_ps[cc]
                for c in range(2):
                    mm_group(fw, STP, STP[:, c, :],
                             [(kk[cc * 64:(cc + 1) * 64, c * 128:(c + 1) * 128],
                               VT[cc * 64:(cc + 1) * 64, c * 256:(c + 1) * 256])], [kk.k, VT.k])
                for h in range(4):
                    c, pb = h // 2, (h % 2) * 64
                    fw.op("dve", lambda e, Sc=Sc, Sn=Sn, STP=STP, c=c, pb=pb, h=h, cc=cc:
                          e.scalar_tensor_tensor(
                              Sn[pb:pb + 64, c, :], Sc[pb:pb + 64, c, :], eG[pb:pb + 64, c, 128 + cc:129 + cc],
                              STP[pb:pb + 64, c, (h % 2) * 128:(h % 2) * 128 + 128], ALU.mult, ALU.add),
                          reads=[Sc.k, eG.k, STP.k], writes=[Sn.k])
            fw.op("act", lambda e: e.copy(oxs[:, :, :], ox_ps[:, :, :]), reads=[ox_ps.k], writes=[oxs.k])
            fw.op("dve", lambda e: e.tensor_tensor(oT[:, :, :], oi_ps[:, :, :], oxs[:, :, :], ALU.add),
                  reads=[oi_ps.k, oxs.k], writes=[oT.k])
            fw.op("act", lambda e: e.activation(sqo[:, :, :], oT[:, :, :], AF.Square),
                  reads=[oT.k], writes=[sqo.k])
            mm_group(fw, zd_ps, zd_ps[:, :], [(ones[:, :], sqo[:, :, :].rearrange("p h t -> p (h t)"))],
                     [ones.k, sqo.k])
            rstd_from_ss(fw, rstd, zd_ps, 128, 512, epsb)
            fw.op("act", lambda e, G_=G_: e.activation(sg[:, :, :], G_[:, :, :], AF.Silu),
                  reads=[G_.k], writes=[sg.k])
            YA = ya[b]
            fw.op("dve", lambda e, YA=YA: e.scalar_tensor_tensor(
                YA[:, :, :].rearrange("p h t -> p (h t)"), oT[:, :, :].rearrange("p h t -> p (h t)"),
                wn[:, 0:1], rstd[:, :], ALU.mult, ALU.mult),
                reads=[oT.k, wn.k, rstd.k], writes=[YA.k])
            fw.op("dve", lambda e, YA=YA: e.tensor_tensor(YA[:, :, :], YA[:, :, :], sg[:, :, :], ALU.mult),
                  reads=[YA.k, sg.k], writes=[YA.k])
            store(fw, "pool", YA, yTr[:, :, ts], YA[:, :, :])


def pool_consts():
    s = np.arange(128)[:, None]
    t = np.arange(128)[None, :]
    cur = np.zeros((4, 128, 128), np.float32)
    prev = np.zeros((4, 128, 128), np.float32)
    cur0 = np.zeros((4, 128, 128), np.float32)
    for g, w in enumerate((2, 4, 8, 16)):
        d = t - s
        cur[g] = np.where((d >= 0) & (d < w), 1.0 / w, 0.0) - np.eye(128)
        cnt = np.minimum(t + 1, w).astype(np.float64)
        cur0[g] = np.where((d >= 0) & (d < w), 1.0 / cnt, 0.0) - np.eye(128)
        d2 = t + 128 - s
        prev[g] = np.where((d2 >= 0) & (d2 < w), 1.0 / w, 0.0)
    return cur.astype(np.float32), prev.astype(np.float32), cur0.astype(np.float32)


def pool_phase(fw, u_tm, yT, wpool_d, pscale_d, cur_d, prev_d, cur0_d):
    with fw.phase():
        wp = Buf(fw, "wp", [128, 4, 128])
        psc = Buf(fw, "psc", [128, 4])
        Pc = Buf(fw, "Pc", [128, 4, 128])
        Pp = Buf(fw, "Pp", [128, 4, 128])
        P0 = Buf(fw, "P0", [128, 4, 128])
        ut = [Buf(fw, f"ut{i}", [128, 512]) for i in range(3)]
        pl = Buf(fw, "pl", [128, 4, 128])
        yb = [Buf(fw, f"yb{i}", [128, 4, 128]) for i in range(2)]
        pt_ps = [Buf(fw, f"pt_ps{i}", [128, 4, 128], psum=True) for i in range(2)]
        y_ps = [Buf(fw, f"y_ps{i}", [128, 4, 128], psum=True) for i in range(2)]
        load(fw, "sp", wp, wp[:, :, :], wpool_d)
        load(fw, "sp", psc, psc[:, :], pscale_d)
        load(fw, "sp", Pc, Pc[:, :, :], cur_d)
        load(fw, "sp", Pp, Pp[:, :, :], prev_d)
        load(fw, "sp", P0, P0[:, :, :], cur0_d)
        yTr = yT.rearrange("(c p) t -> p c t", p=128)
        for it in range(T // 128):
            ts = slice(it * 128, (it + 1) * 128)
            U = ut[it % 3]
            Uprev = ut[(it - 1) % 3]
            if it == 0:
                load(fw, "sp", U, U[:, :], u_tm[ts, :])
            if it + 1 < T // 128:
                Un = ut[(it + 1) % 3]
                load(fw, "sp", Un, Un[:, :], u_tm[(it + 1) * 128:(it + 2) * 128, :])
            PT = pt_ps[it % 2]
            for g in range(4):
                gs = slice(g * 128, (g + 1) * 128)
                if it == 0:
                    mm_group(fw, PT, PT[:, g, :], [(U[:, gs], P0[:, g, :])], [U.k, P0.k])
                else:
                    mm_group(fw, PT, PT[:, g, :], [(U[:, gs], Pc[:, g, :]), (Uprev[:, gs], Pp[:, g, :])],
                             [U.k, Uprev.k, Pc.k, Pp.k])
            fw.op("act", lambda e, PT=PT: e.copy(pl[:, :, :], PT[:, :, :]), reads=[PT.k], writes=[pl.k])
            YP = y_ps[it % 2]
            for g in range(4):
                mm_group(fw, YP, YP[:, g, :], [(wp[:, g, :], pl[:, g, :])], [wp.k, pl.k])
            YB = yb[it % 2]
            fw.op("dve", lambda e, YB=YB, YP=YP: e.tensor_tensor(
                YB[:, :, :], YP[:, :, :], psc[:, :].unsqueeze(2).to_broadcast([128, 4, 128]), ALU.mult),
                reads=[YP.k, psc.k], writes=[YB.k])
            store(fw, "pool", YB, yTr[:, :, ts], YB[:, :, :])


def conv_phase(fw, xbcT, xcT, xB_tm, cw_d, cb_d, ident_d, NT=512):
    with fw.phase():
        cw = Buf(fw, "cw", [128, 8, 4])
        cb = Buf(fw, "cb", [128, 8])
        idn = Buf(fw, "idn", [128, 128])
        xt = [Buf(fw, f"xt{i}", [128, NT + 3]) for i in range(3)]
        acc = [Buf(fw, f"acc{i}", [128, NT]) for i in range(2)]
        xc = [Buf(fw, f"xc{i}", [128, NT]) for i in range(3)]
        s2 = [Buf(fw, f"s2{i}", [128, 4, 128]) for i in range(3)]
        tr_ps = [Buf(fw, f"tr_ps{i}", [128, 4, 128], psum=True) for i in range(2)]
        load(fw, "sp", cw, cw[:, :, :], cw_d)
        load(fw, "sp", cb, cb[:, :], cb_d)
        load(fw, "sp", idn, idn[:, :], ident_d)
        k = 0
        for it in range(T // NT):
            t0 = it * NT
            for c in range(8):
                X = xt[k % 3]
                A = acc[k % 2]
                XC = xc[k % 3]
                rows = slice(c * 128, (c + 1) * 128)
                if it == 0:
                    fw.op("pool", lambda e, X=X: e.memset(X[:, 0:3], 0.0), writes=[X.k])
                    load(fw, "sp", X, X[:, 3:NT + 3], xbcT[rows, 0:NT])
                else:
                    load(fw, "sp", X, X[:, :], xbcT[rows, t0 - 3:t0 + NT])
                fw.op("dve", lambda e, X=X, A=A, c=c: e.tensor_scalar(
                    A[:, :], X[:, 0:NT], cw[:, c, 0:1], None, ALU.mult), reads=[X.k, cw.k], writes=[A.k])
                for j in range(1, 4):
                    fw.op("dve", lambda e, X=X, A=A, c=c, j=j: e.scalar_tensor_tensor(
                        A[:, :], X[:, j:j + NT], cw[:, c, j:j + 1], A[:, :], ALU.mult, ALU.add),
                        reads=[X.k, cw.k, A.k], writes=[A.k])
                fw.op("act", lambda e, A=A, XC=XC, c=c: e.activation(
                    XC[:, :], A[:, :], AF.Silu, bias=cb[:, c:c + 1]), reads=[A.k, cb.k], writes=[XC.k])
                store(fw, "pool", XC, xcT[rows, t0:t0 + NT], XC[:, :])
                if c < 6:
                    TP = tr_ps[k % 2]
                    for sub in range(4):
                        fw.op("pe", lambda e, TP=TP, XC=XC, sub=sub: e.transpose(
                            TP[:, sub, :], XC[:, sub * 128:(sub + 1) * 128], idn[:, :]),
                            reads=[XC.k, idn.k], writes=[TP.k])
                    S2 = s2[k % 3]
                    fw.op("act", lambda e, TP=TP, S2=S2: e.copy(S2[:, :, :], TP[:, :, :]),
                          reads=[TP.k], writes=[S2.k])
                    store(fw, "sp", S2, xB_tm[t0:t0 + NT, c * 128:(c + 1) * 128].rearrange(
                        "(s p) v -> p s v", p=128), S2[:, :, :])
                k += 1


def ssd_consts():
    t = np.arange(128)[:, None]
    s = np.arange(128)[None, :]
    U = (t > s).astype(np.float32)
    Tri = (t <= s).astype(np.float32)
    NEG = np.where(s >= t, 0.0, -30000.0).astype(np.float32)
    return U, Tri, NEG


def ssd_phase(fw, xcT, xB_tm, z_tm, dt_tm, yT, U_d, Tri_d, NEG_d, ident_d, dtb_d, alog_d, dsk_d, wn_d):
    with fw.phase():
        ones = Buf(fw, "ones", [128, 128])
        epsb = Buf(fw, "epsb", [128, 1])
        U = Buf(fw, "U", [128, 128])
        Tri = Buf(fw, "Tri", [128, 128])
        NEG = Buf(fw, "NEG", [128, 128])
        idn = Buf(fw, "idn", [128, 128])
        dtb = Buf(fw, "dtb", [128, 8])
        An = Buf(fw, "An", [128, 8])
        dsk = Buf(fw, "dsk", [128, 8])
        wn = Buf(fw, "wn", [128, 512])
        BT = [Buf(fw, f"BT{i}", [128, 2, 128]) for i in range(2)]
        CT = [Buf(fw, f"CT{i}", [128, 2, 128]) for i in range(2)]
        XB = [Buf(fw, f"XB{i}", [128, 768]) for i in range(2)]
        Z = [Buf(fw, f"Z{i}", [128, 512]) for i in range(2)]
        DT = [Buf(fw, f"DT{i}", [128, 8]) for i in range(2)]
        dt = Buf(fw, "dt", [128, 8])
        a = Buf(fw, "a", [128, 8])
        e3 = Buf(fw, "e3", [128, 3, 8])
        xdt = Buf(fw, "xdt", [128, 8, 64])
        xdt2 = Buf(fw, "xdt2", [128, 8, 64])
        CBT = Buf(fw, "CBT", [128, 2, 128])
        lh = [Buf(fw, f"lh{i}", [128, 128]) for i in range(8)]
        Lh = [Buf(fw, f"Lh{i}", [128, 512]) for i in range(2)]
        Wh = [Buf(fw, f"Wh{i}", [128, 512]) for i in range(2)]
        yo = Buf(fw, "yo", [128, 8, 64])
        y = Buf(fw, "y", [128, 8, 64])
        sz = Buf(fw, "sz", [128, 512])
        junk = Buf(fw, "junk", [128, 256])
        ssq = Buf(fw, "ssq", [128, 2])
        rs = Buf(fw, "rs", [128, 2])
        yc = [Buf(fw, f"yc{i}", [128, 4, 128]) for i in range(2)]
        ST = [Buf(fw, f"ST{i}", [128, 4, 64]) for i in range(2)]
        sm_ps = Buf(fw, "sm_ps", [128, 3, 8], psum=True)
        cb_ps = Buf(fw, "cb_ps", [128, 2, 128], psum=True)
        dm_ps = [Buf(fw, f"dm_ps{i}", [128, 512], psum=True) for i in range(2)]
        yd_ps = Buf(fw, "yd_ps", [128, 8, 64], psum=True)
        yo_ps = Buf(fw, "yo_ps", [128, 8, 64], psum=True)
        st_ps = Buf(fw, "st_ps", [128, 2, 256], psum=True)
        tr_ps = Buf(fw, "tr_ps", [128, 4, 128], psum=True)
        fw.op("pool", lambda e: e.memset(ones[:, :], 1.0), writes=[ones.k])
        fw.op("pool", lambda e: e.memset(epsb[:, :], EPS), writes=[epsb.k])
        for i in range(2):
            fw.op("pool", lambda e, i=i: e.memset(ST[i][:, :, :], 0.0), writes=[ST[i].k])
        load(fw, "sp", U, U[:, :], U_d)
        load(fw, "sp", Tri, Tri[:, :], Tri_d)
        load(fw, "sp", NEG, NEG[:, :], NEG_d)
        load(fw, "sp", idn, idn[:, :], ident_d)
        load(fw, "sp", dtb, dtb[:, :], dtb_d.partition_broadcast(128))
        load(fw, "sp", An, An[:, :], alog_d.partition_broadcast(128))
        load(fw, "sp", dsk, dsk[:, :], dsk_d.partition_broadcast(128))
        load(fw, "sp", wn, wn[:, :], wn_d.partition_broadcast(128))
        fw.op("act", lambda e: e.activation(An[:, :], An[:, :], AF.Exp), reads=[An.k], writes=[An.k])
        fw.op("dve", lambda e: e.tensor_scalar(An[:, :], An[:, :], -1.0, None, ALU.mult),
              reads=[An.k], writes=[An.k])
        BTr = xcT[512:768].rearrange("(g p) t -> p g t", p=128)
        CTr = xcT[768:1024].rearrange("(g p) t -> p g t", p=128)
        yTr = yT.rearrange("(c p) t -> p c t", p=128)

        def bc8(ap):
            return ap.unsqueeze(2).to_broadcast([128, 8, 64])

        def ssd_loads(n):
            ts = slice(n * 128, (n + 1) * 128)
            b = n % 2
            B_, C_, X_, Z_, D_ = BT[b], CT[b], XB[b], Z[b], DT[b]
            load(fw, "sp", B_, B_[:, :, :], BTr[:, :, ts])
            load(fw, "sp", C_, C_[:, :, :], CTr[:, :, ts])
            load(fw, "sp", X_, X_[:, :], xB_tm[ts, :])
            load(fw, "sp", Z_, Z_[:, :], z_tm[ts, :])
            load(fw, "sp", D_, D_[:, :], dt_tm[ts, :])

        ssd_loads(0)
        for n in range(T // 128):
            ts = slice(n * 128, (n + 1) * 128)
            b = n % 2
            B_, C_, X_, Z_, D_ = BT[b], CT[b], XB[b], Z[b], DT[b]
            if n + 1 < T // 128:
                ssd_loads(n + 1)
            x3 = X_[:, 0:512].rearrange("p (h d) -> p h d", h=8)
            fw.op("dve", lambda e, D_=D_: e.tensor_tensor(dt[:, :], D_[:, :], dtb[:, :], ALU.add),
                  reads=[D_.k, dtb.k], writes=[dt.k])
            fw.op("act", lambda e: e.activation(dt[:, :], dt[:, :], AF.Exp), reads=[dt.k], writes=[dt.k])
            fw.op("act", lambda e: e.activation(dt[:, :], dt[:, :], AF.Ln, bias=1.0), reads=[dt.k], writes=[dt.k])
            fw.op("dve", lambda e: e.tensor_tensor(a[:, :], dt[:, :], An[:, :], ALU.mult),
                  reads=[dt.k, An.k], writes=[a.k])
            mm_group(fw, sm_ps, sm_ps[:, 0, :], [(Tri[:, :], a[:, :])], [Tri.k, a.k])
            mm_group(fw, sm_ps, sm_ps[:, 1, :], [(ones[:, :], a[:, :])], [ones.k, a.k])
            mm_group(fw, sm_ps, sm_ps[:, 2, :], [(U[:, :], a[:, :])], [U.k, a.k])
            fw.op("act", lambda e: e.activation(e3[:, :, :], sm_ps[:, :, :], AF.Exp),
                  reads=[sm_ps.k], writes=[e3.k])
            fw.op("dve", lambda e, x3=x3, X_=X_: e.tensor_tensor(xdt[:, :, :], x3, bc8(dt[:, :]), ALU.mult),
                  reads=[X_.k, dt.k], writes=[xdt.k])
            fw.op("dve", lambda e: e.tensor_tensor(xdt2[:, :, :], xdt[:, :, :], bc8(e3[:, 2, :]), ALU.mult),
                  reads=[xdt.k, e3.k], writes=[xdt2.k])
            for g in range(2):
                mm_group(fw, cb_ps, cb_ps[:, g, :], [(B_[:, g, :], C_[:, g, :])], [B_.k, C_.k])
            fw.op("act", lambda e: e.copy(CBT[:, :, :], cb_ps[:, :, :]), reads=[cb_ps.k], writes=[CBT.k])
            for h in range(8):
                LH = lh[h]
                fw.op("dve", lambda e, LH=LH, h=h: e.tensor_scalar(
                    LH[:, :], U[:, :], a[:, h:h + 1], None, ALU.mult), reads=[U.k, a.k], writes=[LH.k])
            for h in range(8):
                DM = dm_ps[(h // 4) % 2]
                dm = DM[:, (h % 4) * 128:(h % 4 + 1) * 128]
                mm_group(fw, DM, dm, [(lh[h][:, :], Tri[:, :]), (idn[:, :], NEG[:, :])],
                         [lh[h].k, Tri.k, idn.k, NEG.k])
            for hh in range(2):
                DM = dm_ps[hh]
                fw.op("act", lambda e, DM=DM, hh=hh: e.activation(
                    Lh[hh][:, :], DM[:, :], AF.Exp), reads=[DM.k], writes=[Lh[hh].k])
                fw.op("dve", lambda e, hh=hh: e.tensor_tensor(
                    Wh[hh][:, :].rearrange("p (h l) -> p h l", h=4),
                    Lh[hh][:, :].rearrange("p (h l) -> p h l", h=4),
                    CBT[:, hh, :].unsqueeze(1).to_broadcast([128, 4, 128]), ALU.mult),
                    reads=[Lh[hh].k, CBT.k], writes=[Wh[hh].k])
            for h in range(8):
                WW = Wh[h // 4]
                mm_group(fw, yd_ps, yd_ps[:, h, :], [(WW[:, (h % 4) * 128:(h % 4 + 1) * 128], xdt[:, h, :])],
                         [WW.k, xdt.k])
            for g in range(2):
                mm_group(fw, yo_ps, yo_ps[:, g * 4:(g + 1) * 4, :].rearrange("p h d -> p (h d)"),
                         [(C_[:, g, :], ST[g][:, :, :].rearrange("p h d -> p (h d)"))], [C_.k, ST[g].k])
            fw.op("dve", lambda e: e.tensor_tensor(yo[:, :, :], yo_ps[:, :, :], bc8(e3[:, 0, :]), ALU.mult),
                  reads=[yo_ps.k, e3.k], writes=[yo.k])
            fw.op("dve", lambda e: e.tensor_tensor(y[:, :, :], yd_ps[:, :, :], yo[:, :, :], ALU.add),
                  reads=[yd_ps.k, yo.k], writes=[y.k])
            fw.op("dve", lambda e, x3=x3, X_=X_: e.tensor_tensor(yo[:, :, :], x3, bc8(dsk[:, :]), ALU.mult),
                  reads=[X_.k, dsk.k], writes=[yo.k])
            fw.op("dve", lambda e: e.tensor_tensor(y[:, :, :], y[:, :, :], yo[:, :, :], ALU.add),
                  reads=[y.k, yo.k], writes=[y.k])
            fw.op("act", lambda e, Z_=Z_: e.activation(sz[:, :], Z_[:, :], AF.Silu), reads=[Z_.k], writes=[sz.k])
            y2 = y[:, :, :].rearrange("p h d -> p (h d)")
            fw.op("dve", lambda e, y2=y2: e.tensor_tensor(y2, y2, sz[:, :], ALU.mult),
                  reads=[y.k, sz.k], writes=[y.k])
            fw.op("pool", lambda e: e.memset(ssq[:, :], 0.0), writes=[ssq.k])
            for g in range(2):
                fw.op("act", lambda e, g=g, y2=y2: e.activation(
                    junk[:, :], y2[:, g * 256:(g + 1) * 256], AF.Square, accum_out=ssq[:, g:g + 1]),
                    reads=[y.k], writes=[junk.k, ssq.k])
            rstd_from_ss(fw, rs, ssq, 256, 2, epsb)
            for g in range(2):
                fw.op("dve", lambda e, g=g, y2=y2: e.scalar_tensor_tensor(
                    y2[:, g * 256:(g + 1) * 256], y2[:, g * 256:(g + 1) * 256], rs[:, g:g + 1],
                    wn[:, g * 256:(g + 1) * 256], ALU.mult, ALU.mult),
                    reads=[y.k, rs.k, wn.k], writes=[y.k])
            for c in range(4):
                fw.op("pe", lambda e, c=c, y2=y2: e.transpose(tr_ps[:, c, :], y2[:, c * 128:(c + 1) * 128],
                                                               idn[:, :]),
                      reads=[y.k, idn.k], writes=[tr_ps.k])
            YC = yc[b]
            fw.op("act", lambda e, YC=YC: e.copy(YC[:, :, :], tr_ps[:, :, :]), reads=[tr_ps.k], writes=[YC.k])
            store(fw, "pool", YC, yTr[:, :, ts], YC[:, :, :])
            for g in range(2):
                mm_group(fw, st_ps, st_ps[:, g, :],
                         [(X_[:, 512 + g * 128:512 + (g + 1) * 128],
                           xdt2[:, g * 4:(g + 1) * 4, :].rearrange("p h d -> p (h d)"))], [X_.k, xdt2.k])
            for g in range(2):
                fw.op("dve", lambda e, g=g: e.tensor_tensor(
                    ST[g][:, :, :], ST[g][:, :, :],
                    e3[:, 1, g * 4:(g + 1) * 4].unsqueeze(2).to_broadcast([128, 4, 64]), ALU.mult),
                    reads=[ST[g].k, e3.k], writes=[ST[g].k])
                fw.op("dve", lambda e, g=g: e.tensor_tensor(
                    ST[g][:, :, :].rearrange("p h d -> p (h d)"),
                    ST[g][:, :, :].rearrange("p h d -> p (h d)"), st_ps[:, g, :], ALU.add),
                    reads=[ST[g].k, st_ps.k], writes=[ST[g].k])


def qknorm_phase(fw, qT, kT, qnT, knT, wq_d, wk_d, bones_d, NT=512):
    with fw.phase():
        bo = Buf(fw, "bo", [128, 128])
        epsb = Buf(fw, "epsb", [128, 1])
        wq = Buf(fw, "wq", [128, 1])
        wk = Buf(fw, "wk", [128, 1])
        xt = [Buf(fw, f"xt{i}", [128, NT]) for i in range(3)]
        sq = [Buf(fw, f"sq{i}", [128, NT]) for i in range(2)]
        rstd = [Buf(fw, f"rstd{i}", [128, NT]) for i in range(2)]
        xo = [Buf(fw, f"xo{i}", [128, NT]) for i in range(3)]
        ss_ps = [Buf(fw, f"ss_ps{i}", [128, NT], psum=True) for i in range(2)]
        fw.op("pool", lambda e: e.memset(epsb[:, :], EPS), writes=[epsb.k])
        load(fw, "sp", bo, bo[:, :], bones_d)
        load(fw, "sp", wq, wq[:, :], wq_d)
        load(fw, "sp", wk, wk[:, :], wk_d)
        fw.op("dve", lambda e: e.tensor_scalar(wq[:, :], wq[:, :], 0.125, None, ALU.mult),
              reads=[wq.k], writes=[wq.k])
        k = 0
        for (src, dst, w) in ((qT, qnT, wq), (kT, knT, wk)):
            for it in range(T // NT):
                ts = slice(it * NT, (it + 1) * NT)
                for c in range(4):
                    X, S_, R_, XO, SS = xt[k % 3], sq[k % 2], rstd[k % 2], xo[k % 3], ss_ps[k % 2]
                    k += 1
                    rows = slice(c * 128, (c + 1) * 128)
                    load(fw, "sp", X, X[:, :], src[rows, ts])
                    fw.op("act", lambda e, X=X, S_=S_: e.activation(S_[:, :], X[:, :], AF.Square),
                          reads=[X.k], writes=[S_.k])
                    mm_group(fw, SS, SS[:, :], [(bo[:, :], S_[:, :])], [bo.k, S_.k])
                    rstd_from_ss(fw, R_, SS, 64, NT, epsb)
                    fw.op("dve", lambda e, X=X, XO=XO, R_=R_, w=w: e.scalar_tensor_tensor(
                        XO[:, :], X[:, :], w[:, 0:1], R_[:, :], ALU.mult, ALU.mult),
                        reads=[X.k, w.k, R_.k], writes=[XO.k])
                    store(fw, "pool", XO, dst[rows, ts], XO[:, :])


def t5_consts():
    k = np.arange(128)[:, None]
    q = np.arange(128)[None, :]
    E = np.zeros((33, 2, 128, 128), np.float32)
    for blk, off in ((0, 0), (1, 128)):
        n = q - k + off
        valid = n >= 0
        nn = np.maximum(n, 0)
        nf = np.maximum(nn, 1).astype(np.float32)
        large = 16 + (np.log(nf / np.float32(16)) / np.float32(np.log(128 / 16)) * np.float32(16)).astype(np.int32)
        large = np.minimum(large, 31)
        bucket = np.where(nn < 16, nn, large)
        for b in range(32):
            E[b, blk] = ((bucket == b) & valid).astype(np.float32)
        E[32, blk] = (~valid).astype(np.float32)
    return E.reshape(33, 2 * 16384)


def bias_phase(fw, rel_bias_d, Ecat_d, BnT):
    with fw.phase():
        tab = Buf(fw, "tab", [64, 4])
        t31 = Buf(fw, "t31", [32, 4])
        Ec = [Buf(fw, f"Ec{i}", [33, 4096]) for i in range(2)]
        out = [Buf(fw, f"out{i}", [4, 4096]) for i in range(2)]
        b_ps = [Buf(fw, f"b_ps{i}", [4, 512], psum=True) for i in range(2)]
        fw.op("pool", lambda e: e.memset(tab[:, :], -30000.0), writes=[tab.k])
        load(fw, "sp", tab, tab[0:32, :], rel_bias_d)
        load(fw, "sp", t31, t31[:, :], rel_bias_d[31:32, :].partition_broadcast(32))
        fw.op("dve", lambda e: e.tensor_tensor(tab[0:32, :], tab[0:32, :], t31[:, :], ALU.subtract),
              reads=[tab.k, t31.k], writes=[tab.k])
        k = 0
        for pc in range(8):
            E_, O_ = Ec[pc % 2], out[pc % 2]
            load(fw, "sp", E_, E_[:, :], Ecat_d[:, pc * 4096:(pc + 1) * 4096])
            for j in range(8):
                P_ = b_ps[k % 2]
                k += 1
                mm_group(fw, P_, P_[:, :], [(tab[0:33, :], E_[0:33, j * 512:(j + 1) * 512])], [tab.k, E_.k])
                fw.op("dve", lambda e, P_=P_, O_=O_, j=j: e.tensor_copy(O_[:, j * 512:(j + 1) * 512], P_[:, :]),
                      reads=[P_.k], writes=[O_.k])
            store(fw, "pool", O_, BnT[:, pc * 4096:(pc + 1) * 4096], O_[:, :])


def attn_phase(fw, qnT, knT, v_tm, BnT, yT, lq1, lk1, lq2, lk2, subcol_d, lambda_init):
    NQ = 512
    with fw.phase():
        ones = Buf(fw, "ones", [128, 128])
        epsb = Buf(fw, "epsb", [128, 1])
        lv = Buf(fw, "lv", [128, 4, 64])
        lt = Buf(fw, "lt", [128, 2, 64])
        ls = Buf(fw, "ls", [128, 2])
        lam = Buf(fw, "lam", [128, 1])
        subw = Buf(fw, "subw", [128, 1])
        qz = [Buf(fw, f"qz{i}", [128, T]) for i in range(2)]
        kn = Buf(fw, "kn", [128, T])
        va = Buf(fw, "va", [128, 32, 128])
        Bn = Buf(fw, "Bn", [128, 2, 128])
        tmp = [Buf(fw, f"tmp{i}", [128, 128]) for i in range(2)]
        PT = [Buf(fw, f"PT{i}", [128, NQ]) for i in range(3)]
        Pacc = [Buf(fw, f"Pacc{i}", [128, NQ]) for i in range(2)]
        rl = [Buf(fw, f"rl{i}", [128, NQ]) for i in range(2)]
        t0b = Buf(fw, "t0b", [128, NQ])
        t1b = Buf(fw, "t1b", [128, NQ])
        ob = Buf(fw, "ob", [128, NQ])
        sqb = Buf(fw, "sqb", [128, NQ])
        rs = Buf(fw, "rs", [128, NQ])
        yd = [Buf(fw, f"yd{i}", [128, NQ]) for i in range(2)]
        s_ps = [Buf(fw, f"s_ps{i}", [128, NQ], psum=True) for i in range(2)]
        o_ps = [Buf(fw, f"o_ps{i}", [128, NQ], psum=True) for i in range(2)]
        l_ps = Buf(fw, "l_ps", [128, NQ], psum=True)
        ss_ps = Buf(fw, "ss_ps", [128, NQ], psum=True)
        fw.op("pool", lambda e: e.memset(ones[:, :], 1.0), writes=[ones.k])
        fw.op("pool", lambda e: e.memset(epsb[:, :], EPS), writes=[epsb.k])
        for i, l in enumerate((lq1, lk1, lq2, lk2)):
            load(fw, "sp", lv, lv[:, i, :], l.partition_broadcast(128))
        load(fw, "sp", subw, subw[:, :], subcol_d)
        fw.op("dve", lambda e: e.tensor_scalar(subw[:, :], subw[:, :], 1.0 - lambda_init, None, ALU.mult),
              reads=[subw.k], writes=[subw.k])
        fw.op("dve", lambda e: e.tensor_tensor(lt[:, 0, :], lv[:, 0, :], lv[:, 1, :], ALU.mult),
              reads=[lv.k], writes=[lt.k])
        fw.op("dve", lambda e: e.tensor_tensor(lt[:, 1, :], lv[:, 2, :], lv[:, 3, :], ALU.mult),
              reads=[lv.k, lt.k], writes=[lt.k])
        fw.op("dve", lambda e: e.reduce_sum(ls[:, :], lt[:, :, :], axis=AX.X), reads=[lt.k], writes=[ls.k])
        fw.op("act", lambda e: e.activation(ls[:, :], ls[:, :], AF.Exp), reads=[ls.k], writes=[ls.k])
        fw.op("dve", lambda e: e.tensor_tensor(lam[:, :], ls[:, 0:1], ls[:, 1:2], ALU.subtract),
              reads=[ls.k], writes=[lam.k])
        fw.op("dve", lambda e: e.tensor_scalar(lam[:, :], lam[:, :], float(lambda_init), None, ALU.add),
              reads=[lam.k], writes=[lam.k])
        fw.op("pool", lambda e: e.memset(qz[0][64:128, :], 0.0), writes=[qz[0].k])
        fw.op("pool", lambda e: e.memset(qz[1][0:64, :], 0.0), writes=[qz[1].k])
        state = {"sk": 0, "pk": 0}
        for h in range(4):
            rows = slice(h * 128, (h + 1) * 128)
            load(fw, "sp", qz[0], qz[0][0:64, :], qnT[h * 128:h * 128 + 64, :])
            load(fw, "sp", qz[1], qz[1][64:128, :], qnT[h * 128 + 64:h * 128 + 128, :])
            load(fw, "sp", kn, kn[:, :], knT[rows, :])
            load(fw, "pool", va, va[:, :, :], v_tm[:, rows].rearrange("(b p) v -> p b v", p=128))
            load(fw, "pool", Bn, Bn[:, :, :], BnT[h].rearrange("(b k q) -> k b q", b=2, k=128))

            def emit_S(qg, m, kb):
                mp = slice(m * 64, (m + 1) * 64)
                SP = s_ps[state["sk"] % 2]
                state["sk"] += 1
                mm_group(fw, SP, SP[:, :], [(kn[:, kb * 128:(kb + 1) * 128],
                                             qz[m][:, qg * NQ:(qg + 1) * NQ])], [kn.k, qz[m].k])
                P_ = PT[state["pk"] % 3]
                state["pk"] += 1
                if kb <= 4 * qg - 2:
                    fw.op("act", lambda e, P_=P_, SP=SP: e.activation(P_[:, :], SP[:, :], AF.Exp),
                          reads=[SP.k], writes=[P_.k])
                    return P_
                j0 = max(0, kb - 4 * qg)
                if j0 > 0:
                    fw.op("pool", lambda e, P_=P_, j0=j0: e.memset(P_[:, 0:j0 * 128], 0.0), writes=[P_.k])
                for j in range(j0, 4):
                    qb = 4 * qg + j
                    cs = slice(j * 128, (j + 1) * 128)
                    if kb < qb - 1:
                        fw.op("act", lambda e, P_=P_, SP=SP, cs=cs: e.activation(
                            P_[:, cs], SP[:, cs], AF.Exp), reads=[SP.k], writes=[P_.k])
                    else:
                        TM = tmp[j % 2]
                        bi = 0 if kb == qb else 1
                        fw.op("dve", lambda e, TM=TM, SP=SP, cs=cs, bi=bi: e.tensor_tensor(
                            TM[:, :], SP[:, cs], Bn[:, bi, :], ALU.add),
                            reads=[SP.k, Bn.k], writes=[TM.k])
                        fw.op("act", lambda e, P_=P_, TM=TM, cs=cs: e.activation(
                            P_[:, cs], TM[:, :], AF.Exp), reads=[TM.k], writes=[P_.k])
                return P_

            def emit_V(qg, m, kb, P_):
                OP = o_ps[m]
                last = 4 * qg + 3
                fw.op("pe", lambda e, OP=OP, P_=P_, kb=kb, last=last: e.matmul(
                    OP[:, :], mm(va[:, kb, :]), mm(P_[:, :]), start=(kb == 0), stop=(kb == last)),
                    reads=[P_.k, va.k], writes=[OP.k], pe_acc=(kb > 0))
                PA = Pacc[m]
                if kb == 0:
                    fw.op("dve", lambda e, PA=PA, P_=P_: e.tensor_copy(PA[:, :], P_[:, :]),
                          reads=[P_.k], writes=[PA.k])
                else:
                    fw.op("dve", lambda e, PA=PA, P_=P_: e.tensor_tensor(PA[:, :], PA[:, :], P_[:, :], ALU.add),
                          reads=[P_.k, PA.k], writes=[PA.k])

            items = [(qg, m, kb) for qg in range(T // NQ) for m in range(2) for kb in range(4 * qg + 4)]
            nxt = emit_S(*items[0])
            for ii, (qg, m, kb) in enumerate(items):
                cur_ = nxt
                if ii + 1 < len(items):
                    nxt = emit_S(*items[ii + 1])
                emit_V(qg, m, kb, cur_)
                if kb != 4 * qg + 3:
                    continue
                mm_group(fw, l_ps, l_ps[:, :], [(ones[:, :], Pacc[m][:, :])], [ones.k, Pacc[m].k])
                fw.op("dve", lambda e, m=m: e.reciprocal(rl[m][:, :], l_ps[:, :]),
                      reads=[l_ps.k], writes=[rl[m].k])
                if m == 0:
                    fw.op("dve", lambda e: e.tensor_tensor(t0b[:, :], o_ps[0][:, :], rl[0][:, :], ALU.mult),
                          reads=[o_ps[0].k, rl[0].k], writes=[t0b.k])
                    continue
                YD = yd[qg % 2]
                fw.op("dve", lambda e: e.scalar_tensor_tensor(
                    t1b[:, :], o_ps[1][:, :], lam[:, 0:1], rl[1][:, :], ALU.mult, ALU.mult),
                    reads=[o_ps[1].k, lam.k, rl[1].k], writes=[t1b.k])
                fw.op("dve", lambda e: e.tensor_tensor(ob[:, :], t0b[:, :], t1b[:, :], ALU.subtract),
                      reads=[t0b.k, t1b.k], writes=[ob.k])
                fw.op("act", lambda e: e.activation(sqb[:, :], ob[:, :], AF.Square),
                      reads=[ob.k], writes=[sqb.k])
                mm_group(fw, ss_ps, ss_ps[:, :], [(ones[:, :], sqb[:, :])], [ones.k, sqb.k])
                rstd_from_ss(fw, rs, ss_ps, 128, NQ, epsb)
                fw.op("dve", lambda e, YD=YD: e.scalar_tensor_tensor(
                    YD[:, :], ob[:, :], subw[:, 0:1], rs[:, :], ALU.mult, ALU.mult),
                    reads=[ob.k, subw.k, rs.k], writes=[YD.k])
                store(fw, "sp", YD, yT[rows, qg * NQ:(qg + 1) * NQ], YD[:, :])


def _lay_w(W):
    n = W.shape[1]
    return np.ascontiguousarray(W.reshape(8, 128, n).transpose(1, 0, 2))


def _lay_g(g):
    return np.ascontiguousarray(g.reshape(8, 128).T)


def _lay_gu(W):
    return np.ascontiguousarray(W.reshape(8, 128, NFC, 128).transpose(2, 1, 0, 3))


def _lay_d(W):
    return np.ascontiguousarray(W.reshape(NFC, 128, 8, 128).transpose(2, 1, 0, 3))


def host_layout(inputs):
    f = lambda a: np.ascontiguousarray(np.asarray(a, dtype=np.float32))
    sh = {}
    for i in range(2):
        for j in range(2):
            sh[f"wg{i}{j}"] = _lay_gu(f(inputs["ffn_w_gate"][i, j]))
            sh[f"wu{i}{j}"] = _lay_gu(f(inputs["ffn_w_up"][i, j]))
            sh[f"wd{i}{j}"] = _lay_d(f(inputs["ffn_w_down"][i, j]))
            sh[f"fg{i}{j}"] = _lay_g(f(inputs["ffn_norm"][i, j]))
        sh[f"mg{i}"] = _lay_g(f(inputs["mix_norm"][i]))
    Mc, M3, maskA = gla_consts()
    cur, prev, cur0 = pool_consts()
    U, Tri, NEG = ssd_consts()
    sh["ev_w_in"] = _lay_w(f(inputs["ev_w_in"][0]))
    sh["Mc"], sh["M3"], sh["mA"] = Mc, M3, maskA
    sh["wgk1"] = f(np.concatenate([inputs["ev_w_gk_up"][0], inputs["ev_b_gk"][0][None]], 0))
    sh["gnorm"] = f(inputs["ev_gla_norm"][0][:, None])
    sh["wpool"] = f(np.asarray(inputs["ev_w_pool"][0]).transpose(1, 0, 2))
    sh["pscale"] = f(np.asarray(inputs["ev_pool_scale"][0]).reshape(4, 128).T)
    sh["cur"] = f(cur.transpose(1, 0, 2))
    sh["prev"] = f(prev.transpose(1, 0, 2))
    sh["cur0"] = f(cur0.transpose(1, 0, 2))
    sh["ev_w_out"] = _lay_w(f(inputs["ev_w_out"][0]))
    sh["od_w_in"] = _lay_w(f(inputs["od_w_in"][0]))
    sh["U"], sh["Tri"], sh["NEG"] = U, Tri, NEG
    sh["ident"] = np.eye(128, dtype=np.float32)
    sh["cw"] = f(np.asarray(inputs["od_conv_w"][0]).reshape(4, 8, 128).transpose(2, 1, 0))
    sh["cb"] = f(np.asarray(inputs["od_conv_b"][0]).reshape(8, 128).T)
    sh["dtb"] = f(inputs["od_dt_bias"])
    sh["alog"] = f(inputs["od_a_log"])
    sh["dsk"] = f(inputs["od_d_skip"])
    sh["wn"] = f(inputs["od_ssd_norm"])
    sh["wq"] = f(np.tile(np.asarray(inputs["od_q_norm"][0]), 2)[:, None])
    sh["wk"] = f(np.tile(np.asarray(inputs["od_k_norm"][0]), 2)[:, None])
    sh["bones"] = np.kron(np.eye(2), np.ones((64, 64))).astype(np.float32)
    sh["relb"] = f(inputs["rel_bias"])
    sh["Ecat"] = t5_consts()
    for nm in ("q1", "k1", "q2", "k2"):
        sh["l" + nm] = f(inputs["od_lambda_" + nm])
    sh["subcol"] = f(np.asarray(inputs["od_subln"][0])[:, None])
    sh["od_w_out"] = _lay_w(f(inputs["od_w_out"][0]))
    return sh


def build_program(shared_shapes):
    import math
    nc = bass.Bass("TRN2", target_bir_lowering=False)
    A = {}
    A["xin"] = nc.dram_tensor("xin", [D, T], F32, kind="ExternalInput").ap()
    for k, shp in shared_shapes.items():
        A[k] = nc.dram_tensor(k, list(shp), F32, kind="ExternalInput").ap()
    xout = nc.dram_tensor("xout", [D, T], F32, kind="ExternalOutput").ap()

    def Sx(name, shape):
        return nc.dram_tensor(name, list(shape), F32).ap()

    xa, xb = Sx("xa", [D, T]), Sx("xb", [D, T])
    yT = Sx("yT", [1024, T])
    qT, kT = Sx("qT", [512, T]), Sx("kT", [512, T])
    gT, lrT = Sx("gT", [512, T]), Sx("lrT", [16, T])
    k_tm, v_tm, u_tm = Sx("k_tm", [T, 256]), Sx("v_tm", [T, 512]), Sx("u_tm", [T, 512])
    xbcT, xcT, xB_tm = Sx("xbcT", [1024, T]), Sx("xcT", [1024, T]), Sx("xB_tm", [T, 768])
    qnT, knT = Sx("qnT", [512, T]), Sx("knT", [512, T])
    z_tm, dt_tm, BnT = Sx("z_tm", [T, 512]), Sx("dt_tm", [T, 8]), Sx("BnT", [4, 32768])
    with contextlib.ExitStack() as st:
        fw = FW(nc, st)
        ffn_phase(fw, A["xin"], xa, A["wg00"], A["wu00"], A["wd00"], A["fg00"])
        fm = [(0, 128, qT[0:128]), (128, 128, qT[128:256]), (256, 128, kT[0:128]), (384, 128, kT[128:256])]
        fm += [(1024 + i * 128, 128, gT[i * 128:(i + 1) * 128]) for i in range(4)]
        fm += [(1536, 16, lrT)]
        tm = [(256, 256, k_tm), (512, 512, v_tm), (1552, 512, u_tm)]
        inproj_phase(fw, xa, A["ev_w_in"], 2064, A["mg0"], fm, tm)
        gla_phase(fw, qT[0:256], kT[0:256], gT, lrT, k_tm, v_tm, yT[0:512], A["Mc"], A["M3"], A["mA"],
                  A["wgk1"], A["gnorm"])
        pool_phase(fw, u_tm, yT[512:1024], A["wpool"], A["pscale"], A["cur"], A["prev"], A["cur0"])
        outproj_phase(fw, xa, xb, yT, A["ev_w_out"])
        ffn_phase(fw, xb, xa, A["wg01"], A["wu01"], A["wd01"], A["fg01"])
        ffn_phase(fw, xa, xb, A["wg10"], A["wu10"], A["wd10"], A["fg10"])
        fm = [(512 + i * 128, 128, xbcT[i * 128:(i + 1) * 128]) for i in range(8)]
        fm += [(1544 + i * 128, 128, qT[i * 128:(i + 1) * 128]) for i in range(4)]
        fm += [(2056 + i * 128, 128, kT[i * 128:(i + 1) * 128]) for i in range(4)]
        tm = [(0, 512, z_tm), (1536, 8, dt_tm), (2568, 512, v_tm)]
        inproj_phase(fw, xb, A["od_w_in"], 3080, A["mg1"], fm, tm)
        conv_phase(fw, xbcT, xcT, xB_tm, A["cw"], A["cb"], A["ident"])
        ssd_phase(fw, xcT, xB_tm, z_tm, dt_tm, yT[0:512], A["U"], A["Tri"], A["NEG"], A["ident"],
                  A["dtb"], A["alog"], A["dsk"], A["wn"])
        qknorm_phase(fw, qT, kT, qnT, knT, A["wq"], A["wk"], A["bones"])
        bias_phase(fw, A["relb"], A["Ecat"], BnT)
        attn_phase(fw, qnT, knT, v_tm, BnT, yT[512:1024], A["lq1"], A["lk1"], A["lq2"],
                   A["lk2"], A["subcol"], 0.8 - 0.6 * math.exp(-0.3 * 1))
        outproj_phase(fw, xb, xa, yT, A["od_w_out"])
        ffn_phase(fw, xa, xout, A["wg11"], A["wu11"], A["wd11"], A["fg11"])
    return nc


def kernel(**inputs):
    x = np.asarray(inputs["x"], dtype=np.float32)
    sh = host_layout(inputs)
    nc = build_program({k: v.shape for k, v in sh.items()})
    in_maps = []
    for b in range(8):
        m = dict(sh)
        m["xin"] = np.ascontiguousarray(x[b].T)
        in_maps.append(m)
    res = run_bass_kernel_spmd(nc, in_maps, core_ids=list(range(8)))
    out = np.stack([np.ascontiguousarray(res.results[b]["xout"].T) for b in range(8)], 0)
    return out.astype(np.float32)
```

```python
import contextlib
import numpy as np
import concourse.bass as bass
import concourse.mybir as mybir
from concourse.bass_utils import run_bass_kernel_spmd

F32 = mybir.dt.float32
F32R = mybir.dt.float32r
ALU = mybir.AluOpType
AF = mybir.ActivationFunctionType
AX = mybir.AxisListType

T = 4096
D = 1024
DFF = 2816
NFC = DFF // 128
EPS = 1e-6
MM_FAST = False


def mm(ap):
    return ap.bitcast(F32R) if MM_FAST else ap


class Trk:
    __slots__ = ("w", "r", "sem", "name")

    def __init__(self, name=""):
        self.w = None
        self.r = []
        self.sem = None
        self.name = name


class Op:
    __slots__ = ("eng", "fn", "deps", "flag", "sem", "val", "dma", "n")

    def __init__(self, eng, fn, dma=False, n=1):
        self.eng = eng
        self.fn = fn
        self.deps = []
        self.flag = False
        self.sem = None
        self.val = 0
        self.dma = dma
        self.n = n


EPOCH = 6000
ENGS = ("pe", "act", "dve", "pool", "sp")


class FW:
    def __init__(self, nc, stack, n_dma_sems=48):
        self.nc = nc
        self.stack = stack
        self.eng_obj = {"pe": nc.tensor, "act": nc.scalar, "dve": nc.vector,
                        "pool": nc.gpsimd, "sp": nc.sync}
        self.eng_sems = {e: [] for e in ENGS}
        self.eng_cnt = {e: 0 for e in ENGS}
        self.free_dma = {}
        self.dma_cnt = {}
        self.n_sem = 0
        self.ops = []
        self.phase_dma_sems = set()
        self.waited = {e: {} for e in ENGS}
        self.phase_stack = None
        self.n_total = 0

    def new_sem(self, name):
        self.n_sem += 1
        return self.stack.enter_context(self.nc.semaphore(f"{name}_{self.n_sem}"))

    def sb(self, name, shape, dtype=F32):
        self.n_sem += 1
        name = f"{name}_{self.n_sem}"
        t = self.phase_stack.enter_context(self.nc.sbuf_tensor(name, list(shape), dtype))
        return t

    def ps(self, name, shape, dtype=F32):
        self.n_sem += 1
        name = f"{name}_{self.n_sem}"
        t = self.phase_stack.enter_context(self.nc.psum_tensor(name, list(shape), dtype))
        return t

    def _dep(self, op, reads, writes):
        deps = op.deps
        for t in reads:
            if t.w is not None:
                deps.append(t.w)
        for t in writes:
            if t.w is not None:
                deps.append(t.w)
            deps.extend(t.r)
        for t in reads:
            if not op.dma:
                t.r = [x for x in t.r if x.dma or x.eng != op.eng]
            t.r.append(op)
        for t in writes:
            t.w = op
            t.r = []

    def op(self, eng, fn, reads=(), writes=(), pe_acc=False):
        o = Op(eng, fn)
        self._dep(o, reads, writes)
        if pe_acc:
            o.deps = [d for d in o.deps if d.eng != "pe" or d.dma]
        self.ops.append(o)
        return o

    def dma(self, q, fns, sbuf_trk, reads=(), writes=()):
        if not isinstance(fns, (list, tuple)):
            fns = [fns]
        o = Op(q, fns, dma=True, n=len(fns))
        self._dep(o, reads, writes)
        cls = "sw" if q == "pool" else "hw"
        if sbuf_trk.sem is None:
            sbuf_trk.sem = {}
        if cls not in sbuf_trk.sem:
            fl = self.free_dma.setdefault(cls, [])
            if not fl:
                s = self.new_sem("dq" + cls)
                self.dma_cnt[s] = 0
                fl.append(s)
            sbuf_trk.sem[cls] = fl.pop()
            self.phase_dma_sems.add((cls, sbuf_trk.sem[cls]))
        o.sem = sbuf_trk.sem[cls]
        self.dma_cnt[o.sem] += 16 * len(fns)
        o.val = self.dma_cnt[o.sem]
        self.ops.append(o)
        return o

    def interleave(self, a0, b0):
        LA, LB = self.ops[a0:b0], self.ops[b0:]
        pending = set(id(o) for o in LA) | set(id(o) for o in LB)
        out = []
        ia = ib = 0
        ra = len(LA) / max(1, len(LB))
        acc = 0.0

        def ok(o):
            return all(id(d) not in pending for d in o.deps)

        while ia < len(LA) or ib < len(LB):
            want_a = ia < len(LA) and (ib >= len(LB) or acc >= 1.0)
            pick = None
            if want_a and ok(LA[ia]):
                pick = "a"
            elif ib < len(LB) and ok(LB[ib]):
                pick = "b"
            elif ia < len(LA) and ok(LA[ia]):
                pick = "a"
            else:
                raise RuntimeError("interleave: dependency cycle")
            if pick == "a":
                o = LA[ia]
                ia += 1
                acc -= 1.0
            else:
                o = LB[ib]
                ib += 1
                acc += ra
            pending.discard(id(o))
            out.append(o)
        self.ops[a0:] = out

    def emit(self):
        nc = self.nc
        ops = self.ops
        for o in ops:
            for d in o.deps:
                if not d.dma:
                    d.flag = True
        last = {}
        for o in ops:
            if not o.dma:
                last[o.eng] = o
        for o in last.values():
            o.flag = True
        for o in ops:
            if o.dma or not o.flag:
                continue
            c = self.eng_cnt[o.eng]
            ep = c // EPOCH
            sems = self.eng_sems[o.eng]
            while len(sems) <= ep:
                sems.append(self.new_sem("e" + o.eng))
            o.sem = sems[ep]
            o.val = c % EPOCH + 1
            self.eng_cnt[o.eng] = c + 1
        targets = []
        for e, o in last.items():
            targets.append((o.sem, o.val))
        for (_c, s) in self.phase_dma_sems:
            targets.append((s, self.dma_cnt[s]))
        by_eng = {e: [] for e in ENGS}
        for o in ops:
            by_eng[o.eng].append(o)
        waited = self.waited

        def run(ename):
            def body(eng):
                wd = waited[ename]
                for o in by_eng[ename]:
                    for d in o.deps:
                        if d.sem is None:
                            continue
                        if wd.get(d.sem, 0) >= d.val:
                            continue
                        if d.eng == ename and not d.dma and ename == "pe":
                            pass
                        eng.wait_ge(d.sem, d.val)
                        wd[d.sem] = d.val
                    if o.dma:
                        for f in o.fn:
                            f(eng).then_inc(o.sem, 16)
                    else:
                        ins = o.fn(eng)
                        if o.flag:
                            ins.then_inc(o.sem, 1)
                for (s, v) in targets:
                    if wd.get(s, 0) < v:
                        eng.wait_ge(s, v)
                        wd[s] = v
            return body

        with nc.Block() as block:
            block.sync(run("sp"))
            block.scalar(run("act"))
            block.vector(run("dve"))
            block.gpsimd(run("pool"))
            block.tensor(run("pe"))
        self.n_total += len(ops)
        for (_c, s) in self.phase_dma_sems:
            if self.dma_cnt[s] < 24000:
                self.free_dma[_c].append(s)
        self.phase_dma_sems = set()
        self.ops = []

    @contextlib.contextmanager
    def phase(self):
        with contextlib.ExitStack() as st:
            self.phase_stack = st
            yield
            self.emit()
        self.phase_stack = None


class Buf:
    def __init__(self, fw, name, shape, dtype=F32, psum=False):
        self.t = fw.ps(name, shape, dtype) if psum else fw.sb(name, shape, dtype)
        self.k = Trk(name)

    def __getitem__(self, idx):
        return self.t[idx]


def rstd_from_ss(fw, out_buf, ss_ps, n, width, epsb):
    fw.op("act", lambda e: e.activation(out_buf[:, :width], ss_ps[:, :width], AF.Sqrt,
                                        bias=epsb[:, 0:1], scale=1.0 / n),
          reads=[ss_ps.k, epsb.k], writes=[out_buf.k])
    fw.op("dve", lambda e: e.reciprocal(out_buf[:, :width], out_buf[:, :width]),
          reads=[out_buf.k], writes=[out_buf.k])


def ffn_phase(fw, xT_in, xT_out, wg, wu, wd, gnorm, NT=512):
    with fw.phase():
        ones = Buf(fw, "ones", [128, 128])
        g_sb = Buf(fw, "g_sb", [128, 8])
        xt = [Buf(fw, f"xt{i}", [128, 8, NT]) for i in range(2)]
        xn = Buf(fw, "xn", [128, 8, NT])
        sq = [Buf(fw, f"sq{i}", [128, NT]) for i in range(2)]
        rstd = Buf(fw, "rstd", [128, NT])
        hid = Buf(fw, "hid", [128, NFC, NT])
        sg = [Buf(fw, f"sg{i}", [128, NT]) for i in range(2)]
        wgb = [Buf(fw, f"wgb{i}", [128, 8, 128]) for i in range(3)]
        wub = [Buf(fw, f"wub{i}", [128, 8, 128]) for i in range(3)]
        wdb = [Buf(fw, f"wdb{i}", [128, NFC, 128]) for i in range(4)]
        xo = [Buf(fw, f"xo{i}", [128, NT]) for i in range(2)]
        ss_ps = Buf(fw, "ss_ps", [128, NT], psum=True)
        gp = [Buf(fw, f"gp{i}", [128, NT], psum=True) for i in range(2)]
        up = [Buf(fw, f"up{i}", [128, NT], psum=True) for i in range(2)]
        op_ = [Buf(fw, f"op{i}", [128, NT], psum=True) for i in range(2)]

        fw.op("pool", lambda e: e.memset(ones[:, :], 1.0), writes=[ones.k])
        epsb = Buf(fw, "epsb", [128, 1])
        fw.op("pool", lambda e: e.memset(epsb[:, :], EPS), writes=[epsb.k])
        fw.dma("sp", lambda e: e.dma_start(out=g_sb[:, :], in_=gnorm), g_sb.k, writes=[g_sb.k])
        xin = xT_in.rearrange("(c p) t -> p c t", p=128)
        xout = xT_out.rearrange("(c p) t -> p c t", p=128)
        ntiles = T // NT
        wi = 0
        di = 0

        def load_x(it):
            X = xt[it % 2]
            ts = slice(it * NT, (it + 1) * NT)
            fw.dma("pool", lambda e, X=X, ts=ts: e.dma_start(out=X[:, :, :], in_=xin[:, :, ts]),
                   X.k, writes=[X.k])

        def load_wd(dcount):
            W = wdb[dcount % 4]
            dc = dcount % 8
            fw.dma("sp", lambda e, W=W, dc=dc: e.dma_start(out=W[:, :, :], in_=wd[dc]),
                   W.k, writes=[W.k])

        load_x(0)
        for it in range(ntiles):
            X = xt[it % 2]
            ts = slice(it * NT, (it + 1) * NT)
            for c in range(8):
                S = sq[c % 2]
                fw.op("act", lambda e, S=S, X=X, c=c: e.activation(S[:, :], X[:, c, :], AF.Square),
                      reads=[X.k], writes=[S.k])
                fw.op("pe", lambda e, S=S, c=c: e.matmul(ss_ps[:, :], ones[:, :], S[:, :],
                                                          start=(c == 0), stop=(c == 7)),
                      reads=[ones.k, S.k], writes=[ss_ps.k], pe_acc=(c > 0))
            rstd_from_ss(fw, rstd, ss_ps, D, NT, epsb)
            for c in range(8):
                fw.op("dve", lambda e, X=X, c=c: e.scalar_tensor_tensor(
                    xn[:, c, :], X[:, c, :], g_sb[:, c:c + 1], rstd[:, :], ALU.mult, ALU.mult),
                    reads=[X.k, g_sb.k, rstd.k], writes=[xn.k])
            if it + 1 < ntiles:
                load_x(it + 1)
            for f in range(NFC):
                WG = wgb[wi % 3]
                WU = wub[wi % 3]
                wi += 1
                fw.dma("sp", lambda e, WG=WG, f=f: e.dma_start(out=WG[:, :, :], in_=wg[f]),
                       WG.k, writes=[WG.k])
                fw.dma("act", lambda e, WU=WU, f=f: e.dma_start(out=WU[:, :, :], in_=wu[f]),
                       WU.k, writes=[WU.k])
                if f in (6, 10, 14, 18):
                    load_wd(it * 8 + (f - 6) // 4)
                G = gp[f % 2]
                U = up[f % 2]
                for c in range(8):
                    fw.op("pe", lambda e, W=WG, G=G, c=c: e.matmul(
                        G[:, :], mm(W[:, c, :]), mm(xn[:, c, :]), start=(c == 0), stop=(c == 7)),
                        reads=[WG.k, xn.k], writes=[G.k], pe_acc=(c > 0))
                for c in range(8):
                    fw.op("pe", lambda e, W=WU, U=U, c=c: e.matmul(
                        U[:, :], mm(W[:, c, :]), mm(xn[:, c, :]), start=(c == 0), stop=(c == 7)),
                        reads=[WU.k, xn.k], writes=[U.k], pe_acc=(c > 0))
                SG = sg[f % 2]
                fw.op("act", lambda e, SG=SG, G=G: e.activation(SG[:, :], G[:, :], AF.Silu),
                      reads=[G.k], writes=[SG.k])
                fw.op("dve", lambda e, SG=SG, U=U, f=f: e.tensor_tensor(
                    hid[:, f, :], SG[:, :], U[:, :], ALU.mult),
                    reads=[SG.k, U.k], writes=[hid.k])
            for dc in range(8):
                W = wdb[(it * 8 + dc) % 4]
                O = op_[dc % 2]
                for f in range(NFC):
                    fw.op("pe", lambda e, W=W, O=O, f=f: e.matmul(
                        O[:, :], mm(W[:, f, :]), mm(hid[:, f, :]), start=(f == 0), stop=(f == NFC - 1)),
                        reads=[W.k, hid.k], writes=[O.k], pe_acc=(f > 0))
                if dc + 4 < 8:
                    load_wd(it * 8 + dc + 4)
                XO = xo[dc % 2]
                fw.op("dve", lambda e, XO=XO, O=O, X=X, dc=dc: e.scalar_tensor_tensor(
                    XO[:, :], O[:, :], 0.5, X[:, dc, :], ALU.mult, ALU.add),
                    reads=[O.k, X.k], writes=[XO.k])
                fw.dma("pool", lambda e, XO=XO, dc=dc, ts=ts: e.dma_start(out=xout[:, dc, ts], in_=XO[:, :]),
                       XO.k, reads=[XO.k])


def mm_group(fw, O, out_ap, pairs, reads):
    n = len(pairs)
    for i, (l, r) in enumerate(pairs):
        fw.op("pe", lambda e, l=l, r=r, i=i: e.matmul(out_ap, mm(l), mm(r), start=(i == 0),
                                                       stop=(i == n - 1)),
              reads=reads, writes=[O.k], pe_acc=(i > 0))


def load(fw, q, B, out_ap, in_ap):
    fw.dma(q, lambda e: e.dma_start(out=out_ap, in_=in_ap), B.k, writes=[B.k])


def store(fw, q, B, out_ap, in_ap):
    fw.dma(q, lambda e: e.dma_start(out=out_ap, in_=in_ap), B.k, reads=[B.k])


def norm_tile(fw, X, hT, sq, ss_ps, rstd, ones, g_sb, epsb, NT):
    for c in range(8):
        S = sq[c % 2]
        fw.op("act", lambda e, S=S, c=c: e.activation(S[:, :], X[:, c, :], AF.Square),
              reads=[X.k], writes=[S.k])
        fw.op("pe", lambda e, S=S, c=c: e.matmul(ss_ps[:, :NT], ones[:, :], S[:, :],
                                                  start=(c == 0), stop=(c == 7)),
              reads=[ones.k, S.k], writes=[ss_ps.k], pe_acc=(c > 0))
    rstd_from_ss(fw, rstd, ss_ps, D, NT, epsb)
    for c in range(8):
        fw.op("dve", lambda e, c=c: e.scalar_tensor_tensor(
            hT[:, c, :], X[:, c, :], g_sb[:, c:c + 1], rstd[:, :NT], ALU.mult, ALU.mult),
            reads=[X.k, g_sb.k, rstd.k], writes=[hT.k])


def inproj_phase(fw, xT_in, w_in, ncols, gnorm, fm_groups, tm_groups, NT=512):
    with fw.phase():
        ones = Buf(fw, "ones", [128, 128])
        epsb = Buf(fw, "epsb", [128, 1])
        g_sb = Buf(fw, "g_sb", [128, 8])
        W = Buf(fw, "W", [128, 8, ncols])
        xt = [Buf(fw, f"xt{i}", [128, 8, NT]) for i in range(2)]
        hT = Buf(fw, "hT", [128, 8, NT])
        sq = [Buf(fw, f"sq{i}", [128, NT]) for i in range(2)]
        rstd = Buf(fw, "rstd", [128, NT])
        stf = [Buf(fw, f"stf{i}", [128, NT]) for i in range(3)]
        stt = [Buf(fw, f"stt{i}", [128, 512]) for i in range(3)]
        ss_ps = Buf(fw, "ss_ps", [128, NT], psum=True)
        fp = [Buf(fw, f"fp{i}", [128, NT], psum=True) for i in range(3)]
        tp = [Buf(fw, f"tp{i}", [128, 512], psum=True) for i in range(3)]
        fw.op("pool", lambda e: e.memset(ones[:, :], 1.0), writes=[ones.k])
        fw.op("pool", lambda e: e.memset(epsb[:, :], EPS), writes=[epsb.k])
        load(fw, "sp", g_sb, g_sb[:, :], gnorm)
        xin = xT_in.rearrange("(c p) t -> p c t", p=128)
        load(fw, "sp", xt[0], xt[0][:, :, :], xin[:, :, 0:NT])
        wtrk = {}
        for gi, (c0, wd_, dst) in enumerate(list(fm_groups) + list(tm_groups)):
            tk = Trk(f"w{c0}")
            wtrk[c0] = tk
            fw.dma("act" if gi % 2 == 0 else "sp",
                   lambda e, c0=c0, wd_=wd_: e.dma_start(out=W[:, :, c0:c0 + wd_], in_=w_in[:, :, c0:c0 + wd_]),
                   tk, writes=[tk])
        k = 0
        for it in range(T // NT):
            X = xt[it % 2]
            ts = slice(it * NT, (it + 1) * NT)
            if it + 1 < T // NT:
                Xn = xt[(it + 1) % 2]
                load(fw, "sp", Xn, Xn[:, :, :], xin[:, :, (it + 1) * NT:(it + 2) * NT])
            norm_tile(fw, X, hT, sq, ss_ps, rstd, ones, g_sb, epsb, NT)
            for (c0, wd_, dst) in fm_groups:
                P_ = fp[k % 3]
                S_ = stf[k % 3]
                mm_group(fw, P_, P_[:wd_, :], [(W[:, c, c0:c0 + wd_], hT[:, c, :]) for c in range(8)],
                         [wtrk[c0], hT.k])
                eng = "act" if k % 2 == 0 else "dve"
                if eng == "act":
                    fw.op("act", lambda e, P_=P_, S_=S_, wd_=wd_: e.copy(S_[:wd_, :], P_[:wd_, :]),
                          reads=[P_.k], writes=[S_.k])
                else:
                    fw.op("dve", lambda e, P_=P_, S_=S_, wd_=wd_: e.tensor_copy(S_[:wd_, :], P_[:wd_, :]),
                          reads=[P_.k], writes=[S_.k])
                store(fw, "pool", S_, dst[:, ts], S_[:wd_, :])
                k += 1
            for sub in range(NT // 128):
                t0 = it * NT + sub * 128
                for (c0, wd_, dst) in tm_groups:
                    P_ = tp[k % 3]
                    S_ = stt[k % 3]
                    mm_group(fw, P_, P_[:, :wd_],
                             [(hT[:, c, sub * 128:(sub + 1) * 128], W[:, c, c0:c0 + wd_]) for c in range(8)],
                             [wtrk[c0], hT.k])
                    if k % 2 == 0:
                        fw.op("act", lambda e, P_=P_, S_=S_, wd_=wd_: e.copy(S_[:, :wd_], P_[:, :wd_]),
                              reads=[P_.k], writes=[S_.k])
                    else:
                        fw.op("dve", lambda e, P_=P_, S_=S_, wd_=wd_: e.tensor_copy(S_[:, :wd_], P_[:, :wd_]),
                              reads=[P_.k], writes=[S_.k])
                    store(fw, "pool", S_, dst[t0:t0 + 128, :], S_[:, :wd_])
                    k += 1


def outproj_phase(fw, xT_in, xT_out, yT, w_out, NT=512):
    with fw.phase():
        W = Buf(fw, "W", [128, 8, 1024])
        xt = [Buf(fw, f"xt{i}", [128, 8, NT]) for i in range(2)]
        yt = [Buf(fw, f"yt{i}", [128, 8, NT]) for i in range(2)]
        xo = [Buf(fw, f"xo{i}", [128, 8, NT]) for i in range(2)]
        op_ = [Buf(fw, f"op{i}", [128, NT], psum=True) for i in range(3)]
        xin = xT_in.rearrange("(c p) t -> p c t", p=128)
        yin = yT.rearrange("(c p) t -> p c t", p=128)
        xout = xT_out.rearrange("(c p) t -> p c t", p=128)
        load(fw, "sp", yt[0], yt[0][:, :, :], yin[:, :, 0:NT])
        wtrk = []
        for dc in range(8):
            tk = Trk(f"wo{dc}")
            wtrk.append(tk)
            fw.dma("act", lambda e, dc=dc: e.dma_start(out=W[:, :, dc * 128:(dc + 1) * 128],
                                                       in_=w_out[:, :, dc * 128:(dc + 1) * 128]),
                   tk, writes=[tk])
        load(fw, "sp", xt[0], xt[0][:, :, :], xin[:, :, 0:NT])
        k = 0
        for it in range(T // NT):
            X = xt[it % 2]
            Y = yt[it % 2]
            XO = xo[it % 2]
            ts = slice(it * NT, (it + 1) * NT)
            if it + 1 < T // NT:
                tn = slice((it + 1) * NT, (it + 2) * NT)
                load(fw, "sp", yt[(it + 1) % 2], yt[(it + 1) % 2][:, :, :], yin[:, :, tn])
                load(fw, "sp", xt[(it + 1) % 2], xt[(it + 1) % 2][:, :, :], xin[:, :, tn])
            for dc in range(8):
                O = op_[k % 3]
                k += 1
                mm_group(fw, O, O[:, :], [(W[:, c, dc * 128:(dc + 1) * 128], Y[:, c, :]) for c in range(8)],
                         [wtrk[dc], Y.k])
                fw.op("dve", lambda e, O=O, X=X, XO=XO, dc=dc: e.tensor_tensor(
                    XO[:, dc, :], O[:, :], X[:, dc, :], ALU.add),
                    reads=[O.k, X.k], writes=[XO.k])
            store(fw, "pool", XO, xout[:, :, ts], XO[:, :, :])


def gla_consts():
    s = np.arange(128)[:, None]
    t = np.arange(128)[None, :]
    same = (s // 64) == (t // 64)
    Mc = np.zeros((128, 130), np.float32)
    Mc[:, :128] = np.where(same & (s <= t), -1.0 / 16, 0.0)
    Mc[:64, 128] = -1.0 / 16
    Mc[64:, 129] = -1.0 / 16
    M3 = np.where(same & (s > t), -1.0 / 16, 0.0).astype(np.float32)
    maskA = np.where(same & (s <= t), 1.0, 0.0).astype(np.float32)
    return Mc, M3, maskA


def gla_phase(fw, qT, kT, gT, lrT, k_tm, v_tm, yT, Mc_d, M3_d, maskA_d, wgk1_d, gnorm_d):
    with fw.phase():
        ones = Buf(fw, "ones", [128, 128])
        epsb = Buf(fw, "epsb", [128, 1])
        Mc = Buf(fw, "Mc", [128, 130])
        M3 = Buf(fw, "M3", [128, 128])
        mA = Buf(fw, "mA", [128, 128])
        wgk = Buf(fw, "wgk", [32, 256])
        wn = Buf(fw, "wn", [128, 1])
        qt = [Buf(fw, f"qt{i}", [128, 2, 128]) for i in range(2)]
        kt = [Buf(fw, f"kt{i}", [128, 2, 128]) for i in range(2)]
        gt = [Buf(fw, f"gt{i}", [128, 4, 128]) for i in range(2)]
        lr = [Buf(fw, f"lr{i}", [32, 128]) for i in range(2)]
        ktm = [Buf(fw, f"ktm{i}", [128, 256]) for i in range(2)]
        vtm = [Buf(fw, f"vtm{i}", [128, 512]) for i in range(2)]
        e1 = Buf(fw, "e1", [128, 256])
        sp_ = Buf(fw, "sp_", [128, 256])
        eGl = [Buf(fw, f"eG{i}", [128, 2, 130]) for i in range(2)]
        enG = Buf(fw, "enG", [128, 2, 128])
        eD = Buf(fw, "eD", [128, 256])
        qdl = [Buf(fw, f"qd{i}", [128, 2, 128]) for i in range(2)]
        kdl = [Buf(fw, f"kd{i}", [128, 2, 128]) for i in range(2)]
        kkl = [Buf(fw, f"kk{i}", [128, 256]) for i in range(2)]
        ATm = Buf(fw, "ATm", [128, 4, 128])
        oxs = Buf(fw, "oxs", [128, 4, 128])
        oT = Buf(fw, "oT", [128, 4, 128])
        sqo = Buf(fw, "sqo", [128, 4, 128])
        rstd = Buf(fw, "rstd", [128, 512])
        sg = Buf(fw, "sg", [128, 4, 128])
        ya = [Buf(fw, f"ya{i}", [128, 4, 128]) for i in range(2)]
        S = [Buf(fw, f"S{i}", [128, 2, 128]) for i in range(2)]
        zd_ps = Buf(fw, "zd_ps", [128, 512], psum=True)
        d_ps = Buf(fw, "d_ps", [128, 512], psum=True)
        gt_ps = Buf(fw, "gt_ps", [128, 2, 256], psum=True)
        at_ps = Buf(fw, "at_ps", [128, 4, 128], psum=True)
        oi_ps = Buf(fw, "oi_ps", [128, 4, 128], psum=True)
        ox_ps = Buf(fw, "ox_ps", [128, 4, 128], psum=True)
        st_ps = [Buf(fw, f"st_ps{i}", [128, 2, 256], psum=True) for i in range(2)]
        fw.op("pool", lambda e: e.memset(ones[:, :], 1.0), writes=[ones.k])
        fw.op("pool", lambda e: e.memset(epsb[:, :], EPS), writes=[epsb.k])
        for i in range(2):
            fw.op("pool", lambda e, i=i: e.memset(lr[i][:, :], 1.0), writes=[lr[i].k])
            fw.op("pool", lambda e, i=i: e.memset(S[i][:, :, :], 0.0), writes=[S[i].k])
        load(fw, "sp", Mc, Mc[:, :], Mc_d)
        load(fw, "sp", M3, M3[:, :], M3_d)
        load(fw, "sp", mA, mA[:, :], maskA_d)
        load(fw, "sp", wgk, wgk[0:17, :], wgk1_d)
        load(fw, "sp", wn, wn[:, :], gnorm_d)
        qTr = qT.rearrange("(c p) t -> p c t", p=128)
        kTr = kT.rearrange("(c p) t -> p c t", p=128)
        gTr = gT.rearrange("(c p) t -> p c t", p=128)
        yTr = yT.rearrange("(c p) t -> p c t", p=128)
        def gla_loads(it):
            ts = slice(it * 128, (it + 1) * 128)
            b = it % 2
            Q, K_, G_, L, KT, VT = qt[b], kt[b], gt[b], lr[b], ktm[b], vtm[b]
            load(fw, "sp", Q, Q[:, :, :], qTr[:, :, ts])
            load(fw, "sp", K_, K_[:, :, :], kTr[:, :, ts])
            load(fw, "sp", G_, G_[:, :, :], gTr[:, :, ts])
            load(fw, "sp", L, L[0:16, :], lrT[:, ts])
            load(fw, "sp", KT, KT[:, :], k_tm[ts, :])
            load(fw, "sp", VT, VT[:, :], v_tm[ts, :])

        def stageA(it):
            ts = slice(it * 128, (it + 1) * 128)
            b = it % 2
            Q, K_, G_, L, KT, VT = qt[b], kt[b], gt[b], lr[b], ktm[b], vtm[b]
            eG, qd, kd, kk = eGl[b], qdl[b], kdl[b], kkl[b]
            mm_group(fw, zd_ps, zd_ps[:, 0:256], [(L[0:17, :], wgk[0:17, :])], [L.k, wgk.k])
            fw.op("act", lambda e: e.activation(e1[:, :], zd_ps[:, 0:256], AF.Exp, scale=-1.0),
                  reads=[zd_ps.k], writes=[e1.k])
            fw.op("act", lambda e: e.activation(sp_[:, :], e1[:, :], AF.Ln, bias=1.0),
                  reads=[e1.k], writes=[sp_.k])
            for c in range(2):
                mm_group(fw, gt_ps, gt_ps[:, c, 0:130], [(sp_[:, c * 128:(c + 1) * 128], Mc[:, :])],
                         [sp_.k, Mc.k])
            mm_group(fw, zd_ps, zd_ps[:, 256:512], [(M3[:, :], sp_[:, :])], [M3.k, sp_.k])
            fw.op("act", lambda e: e.activation(eG[:, :, :], gt_ps[:, :, 0:130], AF.Exp),
                  reads=[gt_ps.k], writes=[eG.k])
            fw.op("act", lambda e: e.activation(enG[:, :, :], gt_ps[:, :, 0:128], AF.Exp, scale=-1.0),
                  reads=[gt_ps.k], writes=[enG.k])
            fw.op("act", lambda e: e.activation(eD[:, :], zd_ps[:, 256:512], AF.Exp),
                  reads=[zd_ps.k], writes=[eD.k])
            fw.op("dve", lambda e, Q=Q: e.scalar_tensor_tensor(
                qd[:, :, :], Q[:, :, :], 0.125, eG[:, :, 0:128], ALU.mult, ALU.mult),
                reads=[Q.k, eG.k], writes=[qd.k])
            fw.op("dve", lambda e, K_=K_: e.tensor_tensor(kd[:, :, :], K_[:, :, :], enG[:, :, :], ALU.mult),
                  reads=[K_.k, enG.k], writes=[kd.k])
            fw.op("dve", lambda e, KT=KT: e.tensor_tensor(kk[:, :], KT[:, :], eD[:, :], ALU.mult),
                  reads=[KT.k, eD.k], writes=[kk.k])

        def stageB(it):
            ts = slice(it * 128, (it + 1) * 128)
            b = it % 2
            Q, K_, G_, L, KT, VT = qt[b], kt[b], gt[b], lr[b], ktm[b], vtm[b]
            eG, qd, kd, kk = eGl[b], qdl[b], kdl[b], kkl[b]
            for h in range(4):
                c, pb = h // 2, (h % 2) * 64
                mm_group(fw, at_ps, at_ps[:, h, :], [(kd[pb:pb + 64, c, :], qd[pb:pb + 64, c, :])],
                         [kd.k, qd.k])
            fw.op("dve", lambda e: e.tensor_tensor(
                ATm[:, :, :], at_ps[:, :, :], mA[:, :].unsqueeze(1).to_broadcast([128, 4, 128]), ALU.mult),
                reads=[at_ps.k, mA.k], writes=[ATm.k])
            for h in range(4):
                mm_group(fw, oi_ps, oi_ps[:, h, :], [(VT[:, h * 128:(h + 1) * 128], ATm[:, h, :])],
                         [VT.k, ATm.k])
            for cc in range(2):
                Sc, Sn = S[cc], S[1 - cc]
                for h in range(4):
                    c, pb = h // 2, (h % 2) * 64
                    mm_group(fw, ox_ps, ox_ps[:, h, cc * 64:(cc + 1) * 64],
                             [(Sc[pb:pb + 64, c, :], qd[pb:pb + 64, c, cc * 64:(cc + 1) * 64])],
                             [Sc.k, qd.k])
                STP = st_ps[cc]
                for c in range(2):
                    mm_group(fw, STP, STP[:, c, :],
                             [(kk[cc * 64:(cc + 1) * 64, c * 128:(c + 1) * 128],
                               VT[cc * 64:(cc + 1) * 64, c * 256:(c + 1) * 256])], [kk.k, VT.k])
                for h in range(4):
                    c, pb = h // 2, (h % 2) * 64
                    fw.op("dve", lambda e, Sc=Sc, Sn=Sn, STP=STP, c=c, pb=pb, h=h, cc=cc:
                          e.scalar_tensor_tensor(
                              Sn[pb:pb + 64, c, :], Sc[pb:pb + 64, c, :], eG[pb:pb + 64, c, 128 + cc:129 + cc],
                              STP[pb:pb + 64, c, (h % 2) * 128:(h % 2) * 128 + 128], ALU.mult, ALU.add),
                          reads=[Sc.k, eG.k, STP.k], writes=[Sn.k])
            fw.op("act", lambda e: e.copy(oxs[:, :, :], ox_ps[:, :, :]), reads=[ox_ps.k], writes=[oxs.k])
            fw.op("dve", lambda e: e.tensor_tensor(oT[:, :, :], oi_ps[:, :, :], oxs[:, :, :], ALU.add),
                  reads=[oi_ps.k, oxs.k], writes=[oT.k])
            fw.op("act", lambda e: e.activation(sqo[:, :, :], oT[:, :, :], AF.Square),
                  reads=[oT.k], writes=[sqo.k])
            mm_group(fw, d_ps, d_ps[:, :], [(ones[:, :], sqo[:, :, :].rearrange("p h t -> p (h t)"))],
                     [ones.k, sqo.k])
            rstd_from_ss(fw, rstd, d_ps, 128, 512, epsb)
            fw.op("act", lambda e, G_=G_: e.activation(sg[:, :, :], G_[:, :, :], AF.Silu),
                  reads=[G_.k], writes=[sg.k])
            YA = ya[b]
            fw.op("dve", lambda e, YA=YA: e.scalar_tensor_tensor(
                YA[:, :, :].rearrange("p h t -> p (h t)"), oT[:, :, :].rearrange("p h t -> p (h t)"),
                wn[:, 0:1], rstd[:, :], ALU.mult, ALU.mult),
                reads=[oT.k, wn.k, rstd.k], writes=[YA.k])
            fw.op("dve", lambda e, YA=YA: e.tensor_tensor(YA[:, :, :], YA[:, :, :], sg[:, :, :], ALU.mult),
                  reads=[YA.k, sg.k], writes=[YA.k])
            store(fw, "pool", YA, yTr[:, :, ts], YA[:, :, :])

        gla_loads(0)
        stageA(0)
        for it in range(T // 128):
            a0 = len(fw.ops)
            if it + 1 < T // 128:
                gla_loads(it + 1)
                stageA(it + 1)
            b0 = len(fw.ops)
            stageB(it)
            fw.interleave(a0, b0)


def pool_consts():
    s = np.arange(128)[:, None]
    t = np.arange(128)[None, :]
    cur = np.zeros((4, 128, 128), np.float32)
    prev = np.zeros((4, 128, 128), np.float32)
    cur0 = np.zeros((4, 128, 128), np.float32)
    for g, w in enumerate((2, 4, 8, 16)):
        d = t - s
        cur[g] = np.where((d >= 0) & (d < w), 1.0 / w, 0.0) - np.eye(128)
        cnt = np.minimum(t + 1, w).astype(np.float64)
        cur0[g] = np.where((d >= 0) & (d < w), 1.0 / cnt, 0.0) - np.eye(128)
        d2 = t + 128 - s
        prev[g] = np.where((d2 >= 0) & (d2 < w), 1.0 / w, 0.0)
    return cur.astype(np.float32), prev.astype(np.float32), cur0.astype(np.float32)


def pool_phase(fw, u_tm, yT, wpool_d, pscale_d, cur_d, prev_d, cur0_d):
    with fw.phase():
        wp = Buf(fw, "wp", [128, 4, 128])
        psc = Buf(fw, "psc", [128, 4])
        Pc = Buf(fw, "Pc", [128, 4, 128])
        Pp = Buf(fw, "Pp", [128, 4, 128])
        P0 = Buf(fw, "P0", [128, 4, 128])
        ut = [Buf(fw, f"ut{i}", [128, 512]) for i in range(3)]
        pl = Buf(fw, "pl", [128, 4, 128])
        yb = [Buf(fw, f"yb{i}", [128, 4, 128]) for i in range(2)]
        pt_ps = [Buf(fw, f"pt_ps{i}", [128, 4, 128], psum=True) for i in range(2)]
        y_ps = [Buf(fw, f"y_ps{i}", [128, 4, 128], psum=True) for i in range(2)]
        load(fw, "sp", wp, wp[:, :, :], wpool_d)
        load(fw, "sp", psc, psc[:, :], pscale_d)
        load(fw, "sp", Pc, Pc[:, :, :], cur_d)
        load(fw, "sp", Pp, Pp[:, :, :], prev_d)
        load(fw, "sp", P0, P0[:, :, :], cur0_d)
        yTr = yT.rearrange("(c p) t -> p c t", p=128)
        for it in range(T // 128):
            ts = slice(it * 128, (it + 1) * 128)
            U = ut[it % 3]
            Uprev = ut[(it - 1) % 3]
            if it == 0:
                load(fw, "sp", U, U[:, :], u_tm[ts, :])
            if it + 1 < T // 128:
                Un = ut[(it + 1) % 3]
                load(fw, "sp", Un, Un[:, :], u_tm[(it + 1) * 128:(it + 2) * 128, :])
            PT = pt_ps[it % 2]
            for g in range(4):
                gs = slice(g * 128, (g + 1) * 128)
                if it == 0:
                    mm_group(fw, PT, PT[:, g, :], [(U[:, gs], P0[:, g, :])], [U.k, P0.k])
                else:
                    mm_group(fw, PT, PT[:, g, :], [(U[:, gs], Pc[:, g, :]), (Uprev[:, gs], Pp[:, g, :])],
                             [U.k, Uprev.k, Pc.k, Pp.k])
            fw.op("act", lambda e, PT=PT: e.copy(pl[:, :, :], PT[:, :, :]), reads=[PT.k], writes=[pl.k])
            YP = y_ps[it % 2]
            for g in range(4):
                mm_group(fw, YP, YP[:, g, :], [(wp[:, g, :], pl[:, g, :])], [wp.k, pl.k])
            YB = yb[it % 2]
            fw.op("dve", lambda e, YB=YB, YP=YP: e.tensor_tensor(
                YB[:, :, :], YP[:, :, :], psc[:, :].unsqueeze(2).to_broadcast([128, 4, 128]), ALU.mult),
                reads=[YP.k, psc.k], writes=[YB.k])
            store(fw, "pool", YB, yTr[:, :, ts], YB[:, :, :])


def conv_phase(fw, xbcT, xcT, xB_tm, cw_d, cb_d, ident_d, NT=512):
    with fw.phase():
        cw = Buf(fw, "cw", [128, 8, 4])
        cb = Buf(fw, "cb", [128, 8])
        idn = Buf(fw, "idn", [128, 128])
        xt = [Buf(fw, f"xt{i}", [128, NT + 3]) for i in range(3)]
        acc = [Buf(fw, f"acc{i}", [128, NT]) for i in range(2)]
        xc = [Buf(fw, f"xc{i}", [128, NT]) for i in range(3)]
        s2 = [Buf(fw, f"s2{i}", [128, 4, 128]) for i in range(3)]
        tr_ps = [Buf(fw, f"tr_ps{i}", [128, 4, 128], psum=True) for i in range(2)]
        load(fw, "sp", cw, cw[:, :, :], cw_d)
        load(fw, "sp", cb, cb[:, :], cb_d)
        load(fw, "sp", idn, idn[:, :], ident_d)
        k = 0
        for it in range(T // NT):
            t0 = it * NT
            for c in range(8):
                X = xt[k % 3]
                A = acc[k % 2]
                XC = xc[k % 3]
                rows = slice(c * 128, (c + 1) * 128)
                if it == 0:
                    fw.op("pool", lambda e, X=X: e.memset(X[:, 0:3], 0.0), writes=[X.k])
                    load(fw, "sp", X, X[:, 3:NT + 3], xbcT[rows, 0:NT])
                else:
                    load(fw, "sp", X, X[:, :], xbcT[rows, t0 - 3:t0 + NT])
                fw.op("dve", lambda e, X=X, A=A, c=c: e.tensor_scalar(
                    A[:, :], X[:, 0:NT], cw[:, c, 0:1], None, ALU.mult), reads=[X.k, cw.k], writes=[A.k])
                for j in range(1, 4):
                    fw.op("dve", lambda e, X=X, A=A, c=c, j=j: e.scalar_tensor_tensor(
                        A[:, :], X[:, j:j + NT], cw[:, c, j:j + 1], A[:, :], ALU.mult, ALU.add),
                        reads=[X.k, cw.k, A.k], writes=[A.k])
                fw.op("act", lambda e, A=A, XC=XC, c=c: e.activation(
                    XC[:, :], A[:, :], AF.Silu, bias=cb[:, c:c + 1]), reads=[A.k, cb.k], writes=[XC.k])
                store(fw, "pool", XC, xcT[rows, t0:t0 + NT], XC[:, :])
                if c < 6:
                    TP = tr_ps[k % 2]
                    for sub in range(4):
                        fw.op("pe", lambda e, TP=TP, XC=XC, sub=sub: e.transpose(
                            TP[:, sub, :], XC[:, sub * 128:(sub + 1) * 128], idn[:, :]),
                            reads=[XC.k, idn.k], writes=[TP.k])
                    S2 = s2[k % 3]
                    fw.op("act", lambda e, TP=TP, S2=S2: e.copy(S2[:, :, :], TP[:, :, :]),
                          reads=[TP.k], writes=[S2.k])
                    store(fw, "sp", S2, xB_tm[t0:t0 + NT, c * 128:(c + 1) * 128].rearrange(
                        "(s p) v -> p s v", p=128), S2[:, :, :])
                k += 1


def ssd_consts():
    t = np.arange(128)[:, None]
    s = np.arange(128)[None, :]
    U = (t > s).astype(np.float32)
    Tri = (t <= s).astype(np.float32)
    NEG = np.where(s >= t, 0.0, -30000.0).astype(np.float32)
    return U, Tri, NEG


def ssd_phase(fw, xcT, xB_tm, z_tm, dt_tm, yT, U_d, Tri_d, NEG_d, ident_d, dtb_d, alog_d, dsk_d, wn_d):
    with fw.phase():
        ones = Buf(fw, "ones", [128, 128])
        epsb = Buf(fw, "epsb", [128, 1])
        U = Buf(fw, "U", [128, 128])
        Tri = Buf(fw, "Tri", [128, 128])
        NEG = Buf(fw, "NEG", [128, 128])
        idn = Buf(fw, "idn", [128, 128])
        dtb = Buf(fw, "dtb", [128, 8])
        An = Buf(fw, "An", [128, 8])
        dsk = Buf(fw, "dsk", [128, 8])
        wn = Buf(fw, "wn", [128, 512])
        BT = [Buf(fw, f"BT{i}", [128, 2, 128]) for i in range(2)]
        CT = [Buf(fw, f"CT{i}", [128, 2, 128]) for i in range(2)]
        XB = [Buf(fw, f"XB{i}", [128, 768]) for i in range(2)]
        Z = [Buf(fw, f"Z{i}", [128, 512]) for i in range(2)]
        DT = [Buf(fw, f"DT{i}", [128, 8]) for i in range(2)]
        dt = Buf(fw, "dt", [128, 8])
        a = Buf(fw, "a", [128, 8])
        e3 = Buf(fw, "e3", [128, 3, 8])
        xdt = Buf(fw, "xdt", [128, 8, 64])
        xdt2 = Buf(fw, "xdt2", [128, 8, 64])
        CBT = Buf(fw, "CBT", [128, 2, 128])
        lh = [Buf(fw, f"lh{i}", [128, 128]) for i in range(8)]
        Lh = [Buf(fw, f"Lh{i}", [128, 512]) for i in range(2)]
        Wh = [Buf(fw, f"Wh{i}", [128, 512]) for i in range(2)]
        yo = Buf(fw, "yo", [128, 8, 64])
        y = Buf(fw, "y", [128, 8, 64])
        sz = Buf(fw, "sz", [128, 512])
        junk = Buf(fw, "junk", [128, 256])
        ssq = Buf(fw, "ssq", [128, 2])
        rs = Buf(fw, "rs", [128, 2])
        yc = [Buf(fw, f"yc{i}", [128, 4, 128]) for i in range(2)]
        ST = [Buf(fw, f"ST{i}", [128, 4, 64]) for i in range(2)]
        sm_ps = Buf(fw, "sm_ps", [128, 3, 8], psum=True)
        cb_ps = Buf(fw, "cb_ps", [128, 2, 128], psum=True)
        dm_ps = [Buf(fw, f"dm_ps{i}", [128, 512], psum=True) for i in range(2)]
        yd_ps = Buf(fw, "yd_ps", [128, 8, 64], psum=True)
        yo_ps = Buf(fw, "yo_ps", [128, 8, 64], psum=True)
        st_ps = Buf(fw, "st_ps", [128, 2, 256], psum=True)
        tr_ps = Buf(fw, "tr_ps", [128, 4, 128], psum=True)
        fw.op("pool", lambda e: e.memset(ones[:, :], 1.0), writes=[ones.k])
        fw.op("pool", lambda e: e.memset(epsb[:, :], EPS), writes=[epsb.k])
        for i in range(2):
            fw.op("pool", lambda e, i=i: e.memset(ST[i][:, :, :], 0.0), writes=[ST[i].k])
        load(fw, "sp", U, U[:, :], U_d)
        load(fw, "sp", Tri, Tri[:, :], Tri_d)
        load(fw, "sp", NEG, NEG[:, :], NEG_d)
        load(fw, "sp", idn, idn[:, :], ident_d)
        load(fw, "sp", dtb, dtb[:, :], dtb_d.partition_broadcast(128))
        load(fw, "sp", An, An[:, :], alog_d.partition_broadcast(128))
        load(fw, "sp", dsk, dsk[:, :], dsk_d.partition_broadcast(128))
        load(fw, "sp", wn, wn[:, :], wn_d.partition_broadcast(128))
        fw.op("act", lambda e: e.activation(An[:, :], An[:, :], AF.Exp), reads=[An.k], writes=[An.k])
        fw.op("dve", lambda e: e.tensor_scalar(An[:, :], An[:, :], -1.0, None, ALU.mult),
              reads=[An.k], writes=[An.k])
        BTr = xcT[512:768].rearrange("(g p) t -> p g t", p=128)
        CTr = xcT[768:1024].rearrange("(g p) t -> p g t", p=128)
        yTr = yT.rearrange("(c p) t -> p c t", p=128)

        def bc8(ap):
            return ap.unsqueeze(2).to_broadcast([128, 8, 64])

        def ssd_loads(n):
            ts = slice(n * 128, (n + 1) * 128)
            b = n % 2
            B_, C_, X_, Z_, D_ = BT[b], CT[b], XB[b], Z[b], DT[b]
            load(fw, "sp", B_, B_[:, :, :], BTr[:, :, ts])
            load(fw, "sp", C_, C_[:, :, :], CTr[:, :, ts])
            load(fw, "sp", X_, X_[:, :], xB_tm[ts, :])
            load(fw, "sp", Z_, Z_[:, :], z_tm[ts, :])
            load(fw, "sp", D_, D_[:, :], dt_tm[ts, :])

        ssd_loads(0)
        for n in range(T // 128):
            ts = slice(n * 128, (n + 1) * 128)
            b = n % 2
            B_, C_, X_, Z_, D_ = BT[b], CT[b], XB[b], Z[b], DT[b]
            if n + 1 < T // 128:
                ssd_loads(n + 1)
            x3 = X_[:, 0:512].rearrange("p (h d) -> p h d", h=8)
            fw.op("dve", lambda e, D_=D_: e.tensor_tensor(dt[:, :], D_[:, :], dtb[:, :], ALU.add),
                  reads=[D_.k, dtb.k], writes=[dt.k])
            fw.op("act", lambda e: e.activation(dt[:, :], dt[:, :], AF.Exp), reads=[dt.k], writes=[dt.k])
            fw.op("act", lambda e: e.activation(dt[:, :], dt[:, :], AF.Ln, bias=1.0), reads=[dt.k], writes=[dt.k])
            fw.op("dve", lambda e: e.tensor_tensor(a[:, :], dt[:, :], An[:, :], ALU.mult),
                  reads=[dt.k, An.k], writes=[a.k])
            mm_group(fw, sm_ps, sm_ps[:, 0, :], [(Tri[:, :], a[:, :])], [Tri.k, a.k])
            mm_group(fw, sm_ps, sm_ps[:, 1, :], [(ones[:, :], a[:, :])], [ones.k, a.k])
            mm_group(fw, sm_ps, sm_ps[:, 2, :], [(U[:, :], a[:, :])], [U.k, a.k])
            fw.op("act", lambda e: e.activation(e3[:, :, :], sm_ps[:, :, :], AF.Exp),
                  reads=[sm_ps.k], writes=[e3.k])
            fw.op("dve", lambda e, x3=x3, X_=X_: e.tensor_tensor(xdt[:, :, :], x3, bc8(dt[:, :]), ALU.mult),
                  reads=[X_.k, dt.k], writes=[xdt.k])
            fw.op("dve", lambda e: e.tensor_tensor(xdt2[:, :, :], xdt[:, :, :], bc8(e3[:, 2, :]), ALU.mult),
                  reads=[xdt.k, e3.k], writes=[xdt2.k])
            for g in range(2):
                mm_group(fw, cb_ps, cb_ps[:, g, :], [(B_[:, g, :], C_[:, g, :])], [B_.k, C_.k])
            fw.op("act", lambda e: e.copy(CBT[:, :, :], cb_ps[:, :, :]), reads=[cb_ps.k], writes=[CBT.k])
            for h in range(8):
                LH = lh[h]
                fw.op("dve", lambda e, LH=LH, h=h: e.tensor_scalar(
                    LH[:, :], U[:, :], a[:, h:h + 1], None, ALU.mult), reads=[U.k, a.k], writes=[LH.k])
            for h in range(8):
                DM = dm_ps[(h // 4) % 2]
                dm = DM[:, (h % 4) * 128:(h % 4 + 1) * 128]
                mm_group(fw, DM, dm, [(lh[h][:, :], Tri[:, :]), (idn[:, :], NEG[:, :])],
                         [lh[h].k, Tri.k, idn.k, NEG.k])
            for hh in range(2):
                DM = dm_ps[hh]
                fw.op("act", lambda e, DM=DM, hh=hh: e.activation(
                    Lh[hh][:, :], DM[:, :], AF.Exp), reads=[DM.k], writes=[Lh[hh].k])
                fw.op("dve", lambda e, hh=hh: e.tensor_tensor(
                    Wh[hh][:, :].rearrange("p (h l) -> p h l", h=4),
                    Lh[hh][:, :].rearrange("p (h l) -> p h l", h=4),
                    CBT[:, hh, :].unsqueeze(1).to_broadcast([128, 4, 128]), ALU.mult),
                    reads=[Lh[hh].k, CBT.k], writes=[Wh[hh].k])
            for h in range(8):
                WW = Wh[h // 4]
                mm_group(fw, yd_ps, yd_ps[:, h, :], [(WW[:, (h % 4) * 128:(h % 4 + 1) * 128], xdt[:, h, :])],
                         [WW.k, xdt.k])
            for g in range(2):
                mm_group(fw, yo_ps, yo_ps[:, g * 4:(g + 1) * 4, :].rearrange("p h d -> p (h d)"),
                         [(C_[:, g, :], ST[g][:, :, :].rearrange("p h d -> p (h d)"))], [C_.k, ST[g].k])
            fw.op("dve", lambda e: e.tensor_tensor(yo[:, :, :], yo_ps[:, :, :], bc8(e3[:, 0, :]), ALU.mult),
                  reads=[yo_ps.k, e3.k], writes=[yo.k])
            fw.op("dve", lambda e: e.tensor_tensor(y[:, :, :], yd_ps[:, :, :], yo[:, :, :], ALU.add),
                  reads=[yd_ps.k, yo.k], writes=[y.k])
            fw.op("dve", lambda e, x3=x3, X_=X_: e.tensor_tensor(yo[:, :, :], x3, bc8(dsk[:, :]), ALU.mult),
                  reads=[X_.k, dsk.k], writes=[yo.k])
            fw.op("dve", lambda e: e.tensor_tensor(y[:, :, :], y[:, :, :], yo[:, :, :], ALU.add),
                  reads=[y.k, yo.k], writes=[y.k])
            fw.op("act", lambda e, Z_=Z_: e.activation(sz[:, :], Z_[:, :], AF.Silu), reads=[Z_.k], writes=[sz.k])
            y2 = y[:, :, :].rearrange("p h d -> p (h d)")
            fw.op("dve", lambda e, y2=y2: e.tensor_tensor(y2, y2, sz[:, :], ALU.mult),
                  reads=[y.k, sz.k], writes=[y.k])
            fw.op("pool", lambda e: e.memset(ssq[:, :], 0.0), writes=[ssq.k])
            for g in range(2):
                fw.op("act", lambda e, g=g, y2=y2: e.activation(
                    junk[:, :], y2[:, g * 256:(g + 1) * 256], AF.Square, accum_out=ssq[:, g:g + 1]),
                    reads=[y.k], writes=[junk.k, ssq.k])
            rstd_from_ss(fw, rs, ssq, 256, 2, epsb)
            for g in range(2):
                fw.op("dve", lambda e, g=g, y2=y2: e.scalar_tensor_tensor(
                    y2[:, g * 256:(g + 1) * 256], y2[:, g * 256:(g + 1) * 256], rs[:, g:g + 1],
                    wn[:, g * 256:(g + 1) * 256], ALU.mult, ALU.mult),
                    reads=[y.k, rs.k, wn.k], writes=[y.k])
            for c in range(4):
                fw.op("pe", lambda e, c=c, y2=y2: e.transpose(tr_ps[:, c, :], y2[:, c * 128:(c + 1) * 128],
                                                               idn[:, :]),
                      reads=[y.k, idn.k], writes=[tr_ps.k])
            YC = yc[b]
            fw.op("act", lambda e, YC=YC: e.copy(YC[:, :, :], tr_ps[:, :, :]), reads=[tr_ps.k], writes=[YC.k])
            store(fw, "pool", YC, yTr[:, :, ts], YC[:, :, :])
            for g in range(2):
                mm_group(fw, st_ps, st_ps[:, g, :],
                         [(X_[:, 512 + g * 128:512 + (g + 1) * 128],
                           xdt2[:, g * 4:(g + 1) * 4, :].rearrange("p h d -> p (h d)"))], [X_.k, xdt2.k])
            for g in range(2):
                fw.op("dve", lambda e, g=g: e.tensor_tensor(
                    ST[g][:, :, :], ST[g][:, :, :],
                    e3[:, 1, g * 4:(g + 1) * 4].unsqueeze(2).to_broadcast([128, 4, 64]), ALU.mult),
                    reads=[ST[g].k, e3.k], writes=[ST[g].k])
                fw.op("dve", lambda e, g=g: e.tensor_tensor(
                    ST[g][:, :, :].rearrange("p h d -> p (h d)"),
                    ST[g][:, :, :].rearrange("p h d -> p (h d)"), st_ps[:, g, :], ALU.add),
                    reads=[ST[g].k, st_ps.k], writes=[ST[g].k])


def qknorm_phase(fw, qT, kT, qnT, knT, wq_d, wk_d, bones_d, NT=512):
    with fw.phase():
        bo = Buf(fw, "bo", [128, 128])
        epsb = Buf(fw, "epsb", [128, 1])
        wq = Buf(fw, "wq", [128, 1])
        wk = Buf(fw, "wk", [128, 1])
        xt = [Buf(fw, f"xt{i}", [128, NT]) for i in range(3)]
        sq = [Buf(fw, f"sq{i}", [128, NT]) for i in range(2)]
        rstd = [Buf(fw, f"rstd{i}", [128, NT]) for i in range(2)]
        xo = [Buf(fw, f"xo{i}", [128, NT]) for i in range(3)]
        ss_ps = [Buf(fw, f"ss_ps{i}", [128, NT], psum=True) for i in range(2)]
        fw.op("pool", lambda e: e.memset(epsb[:, :], EPS), writes=[epsb.k])
        load(fw, "sp", bo, bo[:, :], bones_d)
        load(fw, "sp", wq, wq[:, :], wq_d)
        load(fw, "sp", wk, wk[:, :], wk_d)
        fw.op("dve", lambda e: e.tensor_scalar(wq[:, :], wq[:, :], 0.125, None, ALU.mult),
              reads=[wq.k], writes=[wq.k])
        k = 0
        for (src, dst, w) in ((qT, qnT, wq), (kT, knT, wk)):
            for it in range(T // NT):
                ts = slice(it * NT, (it + 1) * NT)
                for c in range(4):
                    X, S_, R_, XO, SS = xt[k % 3], sq[k % 2], rstd[k % 2], xo[k % 3], ss_ps[k % 2]
                    k += 1
                    rows = slice(c * 128, (c + 1) * 128)
                    load(fw, "sp", X, X[:, :], src[rows, ts])
                    fw.op("act", lambda e, X=X, S_=S_: e.activation(S_[:, :], X[:, :], AF.Square),
                          reads=[X.k], writes=[S_.k])
                    mm_group(fw, SS, SS[:, :], [(bo[:, :], S_[:, :])], [bo.k, S_.k])
                    rstd_from_ss(fw, R_, SS, 64, NT, epsb)
                    fw.op("dve", lambda e, X=X, XO=XO, R_=R_, w=w: e.scalar_tensor_tensor(
                        XO[:, :], X[:, :], w[:, 0:1], R_[:, :], ALU.mult, ALU.mult),
                        reads=[X.k, w.k, R_.k], writes=[XO.k])
                    store(fw, "pool", XO, dst[rows, ts], XO[:, :])


def t5_consts():
    k = np.arange(128)[:, None]
    q = np.arange(128)[None, :]
    E = np.zeros((33, 2, 128, 128), np.float32)
    for blk, off in ((0, 0), (1, 128)):
        n = q - k + off
        valid = n >= 0
        nn = np.maximum(n, 0)
        nf = np.maximum(nn, 1).astype(np.float32)
        large = 16 + (np.log(nf / np.float32(16)) / np.float32(np.log(128 / 16)) * np.float32(16)).astype(np.int32)
        large = np.minimum(large, 31)
        bucket = np.where(nn < 16, nn, large)
        for b in range(32):
            E[b, blk] = ((bucket == b) & valid).astype(np.float32)
        E[32, blk] = (~valid).astype(np.float32)
    return E.reshape(33, 2 * 16384)


def bias_phase(fw, rel_bias_d, Ecat_d, BnT):
    with fw.phase():
        tab = Buf(fw, "tab", [64, 4])
        t31 = Buf(fw, "t31", [32, 4])
        Ec = [Buf(fw, f"Ec{i}", [33, 4096]) for i in range(2)]
        out = [Buf(fw, f"out{i}", [4, 4096]) for i in range(2)]
        b_ps = [Buf(fw, f"b_ps{i}", [4, 512], psum=True) for i in range(2)]
        fw.op("pool", lambda e: e.memset(tab[:, :], -30000.0), writes=[tab.k])
        load(fw, "sp", tab, tab[0:32, :], rel_bias_d)
        load(fw, "sp", t31, t31[:, :], rel_bias_d[31:32, :].partition_broadcast(32))
        fw.op("dve", lambda e: e.tensor_tensor(tab[0:32, :], tab[0:32, :], t31[:, :], ALU.subtract),
              reads=[tab.k, t31.k], writes=[tab.k])
        k = 0
        for pc in range(8):
            E_, O_ = Ec[pc % 2], out[pc % 2]
            load(fw, "sp", E_, E_[:, :], Ecat_d[:, pc * 4096:(pc + 1) * 4096])
            for j in range(8):
                P_ = b_ps[k % 2]
                k += 1
                mm_group(fw, P_, P_[:, :], [(tab[0:33, :], E_[0:33, j * 512:(j + 1) * 512])], [tab.k, E_.k])
                fw.op("dve", lambda e, P_=P_, O_=O_, j=j: e.tensor_copy(O_[:, j * 512:(j + 1) * 512], P_[:, :]),
                      reads=[P_.k], writes=[O_.k])
            store(fw, "pool", O_, BnT[:, pc * 4096:(pc + 1) * 4096], O_[:, :])


def attn_phase(fw, qnT, knT, v_tm, BnT, yT, lq1, lk1, lq2, lk2, subcol_d, lambda_init):
    NQ = 512
    with fw.phase():
        ones = Buf(fw, "ones", [128, 128])
        epsb = Buf(fw, "epsb", [128, 1])
        lv = Buf(fw, "lv", [128, 4, 64])
        lt = Buf(fw, "lt", [128, 2, 64])
        ls = Buf(fw, "ls", [128, 2])
        lam = Buf(fw, "lam", [128, 1])
        subw = Buf(fw, "subw", [128, 1])
        qz = [Buf(fw, f"qz{i}", [128, T]) for i in range(2)]
        kn = Buf(fw, "kn", [128, T])
        va = Buf(fw, "va", [128, 32, 128])
        Bn = Buf(fw, "Bn", [128, 2, 128])
        tmp = [Buf(fw, f"tmp{i}", [128, 128]) for i in range(2)]
        PT = [Buf(fw, f"PT{i}", [128, NQ]) for i in range(3)]
        Pacc = [Buf(fw, f"Pacc{i}", [128, NQ]) for i in range(2)]
        rl = [Buf(fw, f"rl{i}", [128, NQ]) for i in range(2)]
        t0b = Buf(fw, "t0b", [128, NQ])
        t1b = Buf(fw, "t1b", [128, NQ])
        ob = Buf(fw, "ob", [128, NQ])
        sqb = Buf(fw, "sqb", [128, NQ])
        rs = Buf(fw, "rs", [128, NQ])
        yd = [Buf(fw, f"yd{i}", [128, NQ]) for i in range(2)]
        s_ps = [Buf(fw, f"s_ps{i}", [128, NQ], psum=True) for i in range(2)]
        o_ps = [Buf(fw, f"o_ps{i}", [128, NQ], psum=True) for i in range(2)]
        l_ps = Buf(fw, "l_ps", [128, NQ], psum=True)
        ss_ps = Buf(fw, "ss_ps", [128, NQ], psum=True)
        fw.op("pool", lambda e: e.memset(ones[:, :], 1.0), writes=[ones.k])
        fw.op("pool", lambda e: e.memset(epsb[:, :], EPS), writes=[epsb.k])
        for i, l in enumerate((lq1, lk1, lq2, lk2)):
            load(fw, "sp", lv, lv[:, i, :], l.partition_broadcast(128))
        load(fw, "sp", subw, subw[:, :], subcol_d)
        fw.op("dve", lambda e: e.tensor_scalar(subw[:, :], subw[:, :], 1.0 - lambda_init, None, ALU.mult),
              reads=[subw.k], writes=[subw.k])
        fw.op("dve", lambda e: e.tensor_tensor(lt[:, 0, :], lv[:, 0, :], lv[:, 1, :], ALU.mult),
              reads=[lv.k], writes=[lt.k])
        fw.op("dve", lambda e: e.tensor_tensor(lt[:, 1, :], lv[:, 2, :], lv[:, 3, :], ALU.mult),
              reads=[lv.k, lt.k], writes=[lt.k])
        fw.op("dve", lambda e: e.reduce_sum(ls[:, :], lt[:, :, :], axis=AX.X), reads=[lt.k], writes=[ls.k])
        fw.op("act", lambda e: e.activation(ls[:, :], ls[:, :], AF.Exp), reads=[ls.k], writes=[ls.k])
        fw.op("dve", lambda e: e.tensor_tensor(lam[:, :], ls[:, 0:1], ls[:, 1:2], ALU.subtract),
              reads=[ls.k], writes=[lam.k])
        fw.op("dve", lambda e: e.tensor_scalar(lam[:, :], lam[:, :], float(lambda_init), None, ALU.add),
              reads=[lam.k], writes=[lam.k])
        fw.op("pool", lambda e: e.memset(qz[0][64:128, :], 0.0), writes=[qz[0].k])
        fw.op("pool", lambda e: e.memset(qz[1][0:64, :], 0.0), writes=[qz[1].k])
        state = {"sk": 0, "pk": 0}
        for h in range(4):
            rows = slice(h * 128, (h + 1) * 128)
            load(fw, "sp", qz[0], qz[0][0:64, :], qnT[h * 128:h * 128 + 64, :])
            load(fw, "sp", qz[1], qz[1][64:128, :], qnT[h * 128 + 64:h * 128 + 128, :])
            load(fw, "sp", kn, kn[:, :], knT[rows, :])
            load(fw, "pool", va, va[:, :, :], v_tm[:, rows].rearrange("(b p) v -> p b v", p=128))
            load(fw, "pool", Bn, Bn[:, :, :], BnT[h].rearrange("(b k q) -> k b q", b=2, k=128))

            def emit_S(qg, m, kb):
                mp = slice(m * 64, (m + 1) * 64)
                SP = s_ps[state["sk"] % 2]
                state["sk"] += 1
                mm_group(fw, SP, SP[:, :], [(kn[:, kb * 128:(kb + 1) * 128],
                                             qz[m][:, qg * NQ:(qg + 1) * NQ])], [kn.k, qz[m].k])
                P_ = PT[state["pk"] % 3]
                state["pk"] += 1
                if kb <= 4 * qg - 2:
                    fw.op("act", lambda e, P_=P_, SP=SP: e.activation(P_[:, :], SP[:, :], AF.Exp),
                          reads=[SP.k], writes=[P_.k])
                    return P_
                j0 = max(0, kb - 4 * qg)
                if j0 > 0:
                    fw.op("pool", lambda e, P_=P_, j0=j0: e.memset(P_[:, 0:j0 * 128], 0.0), writes=[P_.k])
                for j in range(j0, 4):
                    qb = 4 * qg + j
                    cs = slice(j * 128, (j + 1) * 128)
                    if kb < qb - 1:
                        fw.op("act", lambda e, P_=P_, SP=SP, cs=cs: e.activation(
                            P_[:, cs], SP[:, cs], AF.Exp), reads=[SP.k], writes=[P_.k])
                    else:
                        TM = tmp[j % 2]
                        bi = 0 if kb == qb else 1
                        fw.op("dve", lambda e, TM=TM, SP=SP, cs=cs, bi=bi: e.tensor_tensor(
                            TM[:, :], SP[:, cs], Bn[:, bi, :], ALU.add),
                            reads=[SP.k, Bn.k], writes=[TM.k])
                        fw.op("act", lambda e, P_=P_, TM=TM, cs=cs: e.activation(
                            P_[:, cs], TM[:, :], AF.Exp), reads=[TM.k], writes=[P_.k])
                return P_

            def emit_V(qg, m, kb, P_):
                OP = o_ps[m]
                last = 4 * qg + 3
                fw.op("pe", lambda e, OP=OP, P_=P_, kb=kb, last=last: e.matmul(
                    OP[:, :], mm(va[:, kb, :]), mm(P_[:, :]), start=(kb == 0), stop=(kb == last)),
                    reads=[P_.k, va.k], writes=[OP.k], pe_acc=(kb > 0))
                PA = Pacc[m]
                if kb == 0:
                    fw.op("dve", lambda e, PA=PA, P_=P_: e.tensor_copy(PA[:, :], P_[:, :]),
                          reads=[P_.k], writes=[PA.k])
                else:
                    fw.op("dve", lambda e, PA=PA, P_=P_: e.tensor_tensor(PA[:, :], PA[:, :], P_[:, :], ALU.add),
                          reads=[P_.k, PA.k], writes=[PA.k])

            items = [(qg, m, kb) for qg in range(T // NQ) for m in range(2) for kb in range(4 * qg + 4)]
            nxt = emit_S(*items[0])
            for ii, (qg, m, kb) in enumerate(items):
                cur_ = nxt
                if ii + 1 < len(items):
                    nxt = emit_S(*items[ii + 1])
                emit_V(qg, m, kb, cur_)
                if kb != 4 * qg + 3:
                    continue
                mm_group(fw, l_ps, l_ps[:, :], [(ones[:, :], Pacc[m][:, :])], [ones.k, Pacc[m].k])
                fw.op("dve", lambda e, m=m: e.reciprocal(rl[m][:, :], l_ps[:, :]),
                      reads=[l_ps.k], writes=[rl[m].k])
                if m == 0:
                    fw.op("dve", lambda e: e.tensor_tensor(t0b[:, :], o_ps[0][:, :], rl[0][:, :], ALU.mult),
                          reads=[o_ps[0].k, rl[0].k], writes=[t0b.k])
                    continue
                YD = yd[qg % 2]
                fw.op("dve", lambda e: e.scalar_tensor_tensor(
                    t1b[:, :], o_ps[1][:, :], lam[:, 0:1], rl[1][:, :], ALU.mult, ALU.mult),
                    reads=[o_ps[1].k, lam.k, rl[1].k], writes=[t1b.k])
                fw.op("dve", lambda e: e.tensor_tensor(ob[:, :], t0b[:, :], t1b[:, :], ALU.subtract),
                      reads=[t0b.k, t1b.k], writes=[ob.k])
                fw.op("act", lambda e: e.activation(sqb[:, :], ob[:, :], AF.Square),
                      reads=[ob.k], writes=[sqb.k])
                mm_group(fw, ss_ps, ss_ps[:, :], [(ones[:, :], sqb[:, :])], [ones.k, sqb.k])
                rstd_from_ss(fw, rs, ss_ps, 128, NQ, epsb)
                fw.op("dve", lambda e, YD=YD: e.scalar_tensor_tensor(
                    YD[:, :], ob[:, :], subw[:, 0:1], rs[:, :], ALU.mult, ALU.mult),
                    reads=[ob.k, subw.k, rs.k], writes=[YD.k])
                store(fw, "sp", YD, yT[rows, qg * NQ:(qg + 1) * NQ], YD[:, :])


def _lay_w(W):
    n = W.shape[1]
    return np.ascontiguousarray(W.reshape(8, 128, n).transpose(1, 0, 2))


def _lay_g(g):
    return np.ascontiguousarray(g.reshape(8, 128).T)


def _lay_gu(W):
    return np.ascontiguousarray(W.reshape(8, 128, NFC, 128).transpose(2, 1, 0, 3))


def _lay_d(W):
    return np.ascontiguousarray(W.reshape(NFC, 128, 8, 128).transpose(2, 1, 0, 3))


def host_layout(inputs):
    f = lambda a: np.ascontiguousarray(np.asarray(a, dtype=np.float32))
    sh = {}
    for i in range(2):
        for j in range(2):
            sh[f"wg{i}{j}"] = _lay_gu(f(inputs["ffn_w_gate"][i, j]))
            sh[f"wu{i}{j}"] = _lay_gu(f(inputs["ffn_w_up"][i, j]))
            sh[f"wd{i}{j}"] = _lay_d(f(inputs["ffn_w_down"][i, j]))
            sh[f"fg{i}{j}"] = _lay_g(f(inputs["ffn_norm"][i, j]))
        sh[f"mg{i}"] = _lay_g(f(inputs["mix_norm"][i]))
    Mc, M3, maskA = gla_consts()
    cur, prev, cur0 = pool_consts()
    U, Tri, NEG = ssd_consts()
    sh["ev_w_in"] = _lay_w(f(inputs["ev_w_in"][0]))
    sh["Mc"], sh["M3"], sh["mA"] = Mc, M3, maskA
    sh["wgk1"] = f(np.concatenate([inputs["ev_w_gk_up"][0], inputs["ev_b_gk"][0][None]], 0))
    sh["gnorm"] = f(inputs["ev_gla_norm"][0][:, None])
    sh["wpool"] = f(np.asarray(inputs["ev_w_pool"][0]).transpose(1, 0, 2))
    sh["pscale"] = f(np.asarray(inputs["ev_pool_scale"][0]).reshape(4, 128).T)
    sh["cur"] = f(cur.transpose(1, 0, 2))
    sh["prev"] = f(prev.transpose(1, 0, 2))
    sh["cur0"] = f(cur0.transpose(1, 0, 2))
    sh["ev_w_out"] = _lay_w(f(inputs["ev_w_out"][0]))
    sh["od_w_in"] = _lay_w(f(inputs["od_w_in"][0]))
    sh["U"], sh["Tri"], sh["NEG"] = U, Tri, NEG
    sh["ident"] = np.eye(128, dtype=np.float32)
    sh["cw"] = f(np.asarray(inputs["od_conv_w"][0]).reshape(4, 8, 128).transpose(2, 1, 0))
    sh["cb"] = f(np.asarray(inputs["od_conv_b"][0]).reshape(8, 128).T)
    sh["dtb"] = f(inputs["od_dt_bias"])
    sh["alog"] = f(inputs["od_a_log"])
    sh["dsk"] = f(inputs["od_d_skip"])
    sh["wn"] = f(inputs["od_ssd_norm"])
    sh["wq"] = f(np.tile(np.asarray(inputs["od_q_norm"][0]), 2)[:, None])
    sh["wk"] = f(np.tile(np.asarray(inputs["od_k_norm"][0]), 2)[:, None])
    sh["bones"] = np.kron(np.eye(2), np.ones((64, 64))).astype(np.float32)
    sh["relb"] = f(inputs["rel_bias"])
    sh["Ecat"] = t5_consts()
    for nm in ("q1", "k1", "q2", "k2"):
        sh["l" + nm] = f(inputs["od_lambda_" + nm])
    sh["subcol"] = f(np.asarray(inputs["od_subln"][0])[:, None])
    sh["od_w_out"] = _lay_w(f(inputs["od_w_out"][0]))
    return sh


def build_program(shared_shapes):
    import math
    nc = bass.Bass("TRN2", target_bir_lowering=False)
    A = {}
    A["xin"] = nc.dram_tensor("xin", [D, T], F32, kind="ExternalInput").ap()
    for k, shp in shared_shapes.items():
        A[k] = nc.dram_tensor(k, list(shp), F32, kind="ExternalInput").ap()
    xout = nc.dram_tensor("xout", [D, T], F32, kind="ExternalOutput").ap()

    def Sx(name, shape):
        return nc.dram_tensor(name, list(shape), F32).ap()

    xa, xb = Sx("xa", [D, T]), Sx("xb", [D, T])
    yT = Sx("yT", [1024, T])
    qT, kT = Sx("qT", [512, T]), Sx("kT", [512, T])
    gT, lrT = Sx("gT", [512, T]), Sx("lrT", [16, T])
    k_tm, v_tm, u_tm = Sx("k_tm", [T, 256]), Sx("v_tm", [T, 512]), Sx("u_tm", [T, 512])
    xbcT, xcT, xB_tm = Sx("xbcT", [1024, T]), Sx("xcT", [1024, T]), Sx("xB_tm", [T, 768])
    qnT, knT = Sx("qnT", [512, T]), Sx("knT", [512, T])
    z_tm, dt_tm, BnT = Sx("z_tm", [T, 512]), Sx("dt_tm", [T, 8]), Sx("BnT", [4, 32768])
    with contextlib.ExitStack() as st:
        fw = FW(nc, st)
        ffn_phase(fw, A["xin"], xa, A["wg00"], A["wu00"], A["wd00"], A["fg00"])
        fm = [(0, 128, qT[0:128]), (128, 128, qT[128:256]), (256, 128, kT[0:128]), (384, 128, kT[128:256])]
        fm += [(1024 + i * 128, 128, gT[i * 128:(i + 1) * 128]) for i in range(4)]
        fm += [(1536, 16, lrT)]
        tm = [(256, 256, k_tm), (512, 512, v_tm), (1552, 512, u_tm)]
        inproj_phase(fw, xa, A["ev_w_in"], 2064, A["mg0"], fm, tm)
        gla_phase(fw, qT[0:256], kT[0:256], gT, lrT, k_tm, v_tm, yT[0:512], A["Mc"], A["M3"], A["mA"],
                  A["wgk1"], A["gnorm"])
        pool_phase(fw, u_tm, yT[512:1024], A["wpool"], A["pscale"], A["cur"], A["prev"], A["cur0"])
        outproj_phase(fw, xa, xb, yT, A["ev_w_out"])
        ffn_phase(fw, xb, xa, A["wg01"], A["wu01"], A["wd01"], A["fg01"])
        ffn_phase(fw, xa, xb, A["wg10"], A["wu10"], A["wd10"], A["fg10"])
        fm = [(512 + i * 128, 128, xbcT[i * 128:(i + 1) * 128]) for i in range(8)]
        fm += [(1544 + i * 128, 128, qT[i * 128:(i + 1) * 128]) for i in range(4)]
        fm += [(2056 + i * 128, 128, kT[i * 128:(i + 1) * 128]) for i in range(4)]
        tm = [(0, 512, z_tm), (1536, 8, dt_tm), (2568, 512, v_tm)]
        inproj_phase(fw, xb, A["od_w_in"], 3080, A["mg1"], fm, tm)
        conv_phase(fw, xbcT, xcT, xB_tm, A["cw"], A["cb"], A["ident"])
        ssd_phase(fw, xcT, xB_tm, z_tm, dt_tm, yT[0:512], A["U"], A["Tri"], A["NEG"], A["ident"],
                  A["dtb"], A["alog"], A["dsk"], A["wn"])
        qknorm_phase(fw, qT, kT, qnT, knT, A["wq"], A["wk"], A["bones"])
        bias_phase(fw, A["relb"], A["Ecat"], BnT)
        attn_phase(fw, qnT, knT, v_tm, BnT, yT[512:1024], A["lq1"], A["lk1"], A["lq2"],
                   A["lk2"], A["subcol"], 0.8 - 0.6 * math.exp(-0.3 * 1))
        outproj_phase(fw, xb, xa, yT, A["od_w_out"])
        ffn_phase(fw, xa, xout, A["wg11"], A["wu11"], A["wd11"], A["fg11"])
    return nc


def kernel(**inputs):
    x = np.asarray(inputs["x"], dtype=np.float32)
    sh = host_layout(inputs)
    nc = build_program({k: v.shape for k, v in sh.items()})
    in_maps = []
    for b in range(8):
        m = dict(sh)
        m["xin"] = np.ascontiguousarray(x[b].T)
        in_maps.append(m)
    res = run_bass_kernel_spmd(nc, in_maps, core_ids=list(range(8)))
    out = np.stack([np.ascontiguousarray(res.results[b]["xout"].T) for b in range(8)], 0)
    return out.astype(np.float32)
```
